# Optimizing a Trainium2 kernel written in Bass

```python
import math
import jax
import jax.numpy as jnp
from jax import lax
import numpy as np

D_MODEL = 2048
BATCH = 16
SEQ = 256
DEPTH = 2
DEC_BATCH = 2
DEC_SEQ = 4096
PAST_LEN = 256

GRID_W = 64
CHUNK = 128
EPS = 1e-6
CONV_A_DIM = D_MODEL // 2
CONV_W = 3
SSD_INNER = D_MODEL
SSD_HEAD_DIM = 64
SSD_HEADS = SSD_INNER // SSD_HEAD_DIM
SSD_GROUPS = 4
SSD_STATE = 128
SSD_CONV_W = 3
SSD_XBC = SSD_INNER + 2 * SSD_GROUPS * SSD_STATE
SGU_DIM = D_MODEL // 2
SGU_GROUPS = 8
N_BRANCH = 3
IN_SPLITS = (CONV_A_DIM, CONV_A_DIM, CONV_A_DIM, SSD_INNER, SSD_XBC, 2 * SSD_HEADS, SGU_DIM, SGU_DIM, N_BRANCH * D_MODEL)
IN_DIM = 3 * CONV_A_DIM + SSD_INNER + SSD_XBC + 2 * SSD_HEADS + 2 * SGU_DIM + N_BRANCH * D_MODEL
E_GROUPS = 4
EXP_PER_GROUP = 4
N_EXP = E_GROUPS * EXP_PER_GROUP
TOP_K_IN_GROUP = 2
D_EXPERT = D_MODEL // 4
ADA_DIM = 6 * D_MODEL

kernel_name = 'hybrid_diffusion_step'


def rmsnorm(x, w):
    xf = x.astype(jnp.float32)
    y = xf * lax.rsqrt(jnp.mean(xf * xf, axis=-1, keepdims=True) + EPS)
    return (y * w.astype(jnp.float32)).astype(x.dtype)


def layernorm(x, g, b):
    xf = x.astype(jnp.float32)
    mu = jnp.mean(xf, axis=-1, keepdims=True)
    xc = xf - mu
    var = jnp.mean(xc * xc, axis=-1, keepdims=True)
    return (xc * lax.rsqrt(var + EPS) * g.astype(jnp.float32) + b.astype(jnp.float32)).astype(x.dtype)


def dwconv(x, w, b=None):
    k = w.shape[0]
    pad = (k - 1) // 2
    y = lax.conv_general_dilated(x, w[:, None, :].astype(x.dtype), window_strides=(1,), padding=[(pad, pad)],
                                 dimension_numbers=('NWC', 'WIO', 'NWC'), feature_group_count=x.shape[-1])
    if b is not None:
        y = y + b
    return y


def grid_pos_code(n_tok, dtype):
    rows = n_tok // GRID_W
    rr, cc = jnp.meshgrid(jnp.arange(rows, dtype=jnp.float32), jnp.arange(GRID_W, dtype=jnp.float32), indexing='ij')
    quarter = D_MODEL // 4
    omega = 1.0 / (10000.0 ** (jnp.arange(quarter, dtype=jnp.float32) / quarter))
    ar = rr.reshape(-1)[:, None] * omega
    ac = cc.reshape(-1)[:, None] * omega
    return jnp.concatenate([jnp.sin(ar), jnp.cos(ar), jnp.sin(ac), jnp.cos(ac)], axis=-1).astype(dtype)


def ssd_chunked(x, dt, a, bmat, cmat, h0):
    b, l, h, p = x.shape
    nc = l // CHUNK
    rep = h // bmat.shape[2]
    bh = jnp.repeat(bmat, rep, axis=2).reshape(b, nc, CHUNK, h, -1)
    ch = jnp.repeat(cmat, rep, axis=2).reshape(b, nc, CHUNK, h, -1)
    xdt = (x * dt[..., None]).reshape(b, nc, CHUNK, h, p)
    da = (dt * a).reshape(b, nc, CHUNK, h).transpose(0, 3, 1, 2)
    cs = jnp.cumsum(da, axis=-1)
    tri = jnp.tril(jnp.ones((CHUNK, CHUNK), dtype=bool))
    lmat = jnp.exp(jnp.where(tri, cs[..., :, None] - cs[..., None, :], -jnp.inf))
    scores = jnp.einsum('bcihn,bcjhn->bhcij', ch, bh) * lmat
    y_diag = jnp.einsum('bhcij,bcjhp->bcihp', scores, xdt)
    decay_st = jnp.exp(cs[..., -1:] - cs)
    st = jnp.einsum('bcqhn,bhcq,bcqhp->bchpn', bh, decay_st, xdt)
    st = jnp.concatenate([h0[:, None], st], axis=1)
    ccs = jnp.cumsum(jnp.pad(cs[..., -1], ((0, 0), (0, 0), (1, 0))), axis=-1)
    tri2 = jnp.tril(jnp.ones((nc + 1, nc + 1), dtype=bool))
    dchunk = jnp.exp(jnp.where(tri2, ccs[..., :, None] - ccs[..., None, :], -jnp.inf))
    hs = jnp.einsum('bhzc,bchpn->bzhpn', dchunk, st)
    y_off = jnp.einsum('bcihn,bchpn,bhci->bcihp', ch, hs[:, :-1], jnp.exp(cs))
    return (y_diag + y_off).reshape(b, l, h, p), hs[:, -1]


def mixer(h, pos, h0, p):
    b, L, _ = h.shape
    if pos is not None:
        h = h + pos
    proj = h @ p['w_in']
    split_points = np.cumsum(np.array(IN_SPLITS))[:-1].tolist()
    a_b, a_c, a_h, z, xbc, dt_raw, s_u, s_v, gates = jnp.split(proj, split_points, axis=-1)
    y_a = (a_b * dwconv(a_c * a_h, p['conv_a_w'])) @ p['w_out_a']
    xbc = jax.nn.silu(dwconv(xbc, p['ssd_conv_w'], p['ssd_conv_b']))
    xs, bm, cm = jnp.split(xbc, [SSD_INNER, SSD_INNER + SSD_GROUPS * SSD_STATE], axis=-1)
    f32 = jnp.float32
    xs4 = xs.reshape(b, L, SSD_HEADS, SSD_HEAD_DIM).astype(f32)
    bm = bm.reshape(b, L, SSD_GROUPS, SSD_STATE).astype(f32)
    cm = cm.reshape(b, L, SSD_GROUPS, SSD_STATE).astype(f32)
    dt = jax.nn.softplus(dt_raw.reshape(b, L, 2, SSD_HEADS).astype(f32) + p['ssd_dt_bias'].astype(f32))
    a = -jnp.exp(p['ssd_a_log'].astype(f32))
    h0f = h0.astype(f32)
    flip = lambda t: jnp.flip(t, axis=1)
    y_f, s_f = ssd_chunked(xs4, dt[:, :, 0], a[0], bm, cm, h0f[:, 0])
    y_r, s_b = ssd_chunked(flip(xs4), flip(dt[:, :, 1]), a[1], flip(bm), flip(cm), h0f[:, 1])
    y_s = y_f + flip(y_r) + p['ssd_d'].astype(f32)[:, None] * xs4
    y_s = y_s.reshape(b, L, SSD_INNER) * jax.nn.silu(z.astype(f32))
    yg = y_s.reshape(b, L, SSD_GROUPS, SSD_INNER // SSD_GROUPS)
    yg = yg * lax.rsqrt(jnp.mean(yg * yg, axis=-1, keepdims=True) + EPS)
    y_s = (yg.reshape(b, L, SSD_INNER) * p['ssd_norm_w'].astype(f32)).astype(h.dtype)
    y_b = y_s @ p['w_out_b']
    u = jax.nn.gelu(s_u)
    v = layernorm(jax.nn.gelu(s_v), p['sgu_ln_g'], p['sgu_ln_b'])
    v = v.reshape(b, L // CHUNK, CHUNK, SGU_GROUPS, SGU_DIM // SGU_GROUPS)
    sp = jnp.einsum('gij,bcjgd->bcigd', p['sgu_w'], v) + p['sgu_b'][:, :, None]
    y_c = (u * sp.reshape(b, L, SGU_DIM)) @ p['w_out_c']
    g_a, g_b, g_c = jnp.split(jax.nn.sigmoid(gates), N_BRANCH, axis=-1)
    out = (g_a * y_a + g_b * y_b + g_c * y_c) @ p['w_o']
    return out, jnp.stack([s_f, s_b], axis=1).astype(h0.dtype)


def hier_moe(h, p):
    b, L, D = h.shape
    t = h.reshape(-1, D)
    pg = jax.nn.softmax((t @ p['router_g_w'] + p['router_g_b']).astype(jnp.float32), axis=-1)
    gi = jnp.argmax(pg, axis=-1)
    pg_sel = jnp.max(pg, axis=-1)
    le = (t @ p['router_e_w'] + p['router_e_b']).astype(jnp.float32).reshape(-1, E_GROUPS, EXP_PER_GROUP)
    le_sel = jnp.einsum('ngk,ng->nk', le, jax.nn.one_hot(gi, E_GROUPS, dtype=jnp.float32))
    pe = jax.nn.softmax(le_sel, axis=-1)
    top_v, top_i = lax.top_k(pe, TOP_K_IN_GROUP)
    wts = pg_sel[:, None] * top_v / jnp.sum(top_v, axis=-1, keepdims=True)
    gidx = gi[:, None] * EXP_PER_GROUP + top_i
    comb = jnp.sum(jax.nn.one_hot(gidx, N_EXP, dtype=jnp.float32) * wts[..., None], axis=1).astype(h.dtype)
    hg = jnp.einsum('nd,edf->nef', t, p['exp_w_gate'])
    hu = jnp.einsum('nd,edf->nef', t, p['exp_w_up'])
    act = jax.nn.silu(hg) * hu * comb[:, :, None]
    return jnp.einsum('nef,efd->nd', act, p['exp_w_down']).reshape(b, L, D)


def layer(x, mod, pos, h0, p):
    sh1, sc1, g1, sh2, sc2, g2 = [m[:, None, :] for m in jnp.split(mod, 6, axis=-1)]
    h = rmsnorm(x, p['norm_mix_w']) * (1 + sc1) + sh1
    mix, st = mixer(h, pos, h0, p)
    x = x + g1 * mix
    h = rmsnorm(x, p['norm_ffn_w']) * (1 + sc2) + sh2
    x = x + g2 * hier_moe(h, p)
    return x, st


def setup_inputs(seed: int = 0) -> dict:
    key = jax.random.key(seed)
    ks = iter(jax.random.split(key, 40))
    f32 = jnp.float32
    D = D_MODEL
    L = DEPTH

    def nrm(shape, scale):
        return scale * jax.random.normal(next(ks), shape, f32)

    x_prompt = nrm((BATCH, SEQ, D), 1.0)
    x_sample = nrm((DEC_BATCH, DEC_SEQ, D), 1.0)
    state_ssd = nrm((DEC_BATCH, DEPTH, 2, SSD_HEADS, SSD_HEAD_DIM, SSD_STATE), 0.1)
    c = nrm((DEC_BATCH, D), 1.0)
    c_ctx = nrm((D,), 1.0)
    norm_mix_w = 1.0 + nrm((L, D), 0.02)
    norm_ffn_w = 1.0 + nrm((L, D), 0.02)
    w_ada = nrm((L, D, ADA_DIM), 0.5 * D ** -0.5)
    b_ada = nrm((L, ADA_DIM), 0.02)
    w_in = nrm((L, D, IN_DIM), D ** -0.5)
    conv_a_w = nrm((L, CONV_W, CONV_A_DIM), CONV_W ** -0.5)
    w_out_a = nrm((L, CONV_A_DIM, D), CONV_A_DIM ** -0.5)
    ssd_conv_w = nrm((L, SSD_CONV_W, SSD_XBC), SSD_CONV_W ** -0.5)
    ssd_conv_b = nrm((L, SSD_XBC), 0.02)
    ssd_a_log = jnp.log(jax.random.uniform(next(ks), (L, 2, SSD_HEADS), f32, 1.0, 16.0))
    dt0 = jnp.exp(jax.random.uniform(next(ks), (L, 2, SSD_HEADS), f32, math.log(1e-3), math.log(1e-1)))
    ssd_dt_bias = dt0 + jnp.log(-jnp.expm1(-dt0))
    ssd_d = 1.0 + nrm((L, SSD_HEADS), 0.02)
    ssd_norm_w = 1.0 + nrm((L, SSD_INNER), 0.02)
    w_out_b = nrm((L, SSD_INNER, D), SSD_INNER ** -0.5)
    sgu_ln_g = 1.0 + nrm((L, SGU_DIM), 0.02)
    sgu_ln_b = nrm((L, SGU_DIM), 0.02)
    sgu_w = nrm((L, SGU_GROUPS, CHUNK, CHUNK), CHUNK ** -0.5)
    sgu_b = 1.0 + nrm((L, CHUNK, SGU_GROUPS), 0.02)
    w_out_c = nrm((L, SGU_DIM, D), SGU_DIM ** -0.5)
    w_o = nrm((L, D, D), D ** -0.5)
    router_g_w = nrm((L, D, E_GROUPS), D ** -0.5)
    router_g_b = nrm((L, E_GROUPS), 0.01)
    router_e_w = nrm((L, D, N_EXP), D ** -0.5)
    router_e_b = nrm((L, N_EXP), 0.01)
    exp_w_gate = nrm((L, N_EXP, D, D_EXPERT), D ** -0.5)
    exp_w_up = nrm((L, N_EXP, D, D_EXPERT), D ** -0.5)
    exp_w_down = nrm((L, N_EXP, D_EXPERT, D), D_EXPERT ** -0.5)
    final_norm_w = 1.0 + nrm((D,), 0.02)
    return {'x_prompt': x_prompt, 'x_sample': x_sample, 'state_ssd': state_ssd, 'c': c, 'c_ctx': c_ctx,
            'norm_mix_w': norm_mix_w, 'norm_ffn_w': norm_ffn_w, 'w_ada': w_ada, 'b_ada': b_ada, 'w_in': w_in,
            'conv_a_w': conv_a_w, 'w_out_a': w_out_a, 'ssd_conv_w': ssd_conv_w, 'ssd_conv_b': ssd_conv_b,
            'ssd_a_log': ssd_a_log, 'ssd_dt_bias': ssd_dt_bias, 'ssd_d': ssd_d, 'ssd_norm_w': ssd_norm_w,
            'w_out_b': w_out_b, 'sgu_ln_g': sgu_ln_g, 'sgu_ln_b': sgu_ln_b, 'sgu_w': sgu_w, 'sgu_b': sgu_b,
            'w_out_c': w_out_c, 'w_o': w_o, 'router_g_w': router_g_w, 'router_g_b': router_g_b,
            'router_e_w': router_e_w, 'router_e_b': router_e_b, 'exp_w_gate': exp_w_gate, 'exp_w_up': exp_w_up,
            'exp_w_down': exp_w_down, 'final_norm_w': final_norm_w}


def reference(x_prompt, x_sample, state_ssd, c, c_ctx, norm_mix_w, norm_ffn_w, w_ada, b_ada, w_in, conv_a_w, w_out_a,
              ssd_conv_w, ssd_conv_b, ssd_a_log, ssd_dt_bias, ssd_d, ssd_norm_w, w_out_b, sgu_ln_g, sgu_ln_b,
              sgu_w, sgu_b, w_out_c, w_o, router_g_w, router_g_b, router_e_w, router_e_b, exp_w_gate, exp_w_up,
              exp_w_down, final_norm_w):
    pos = grid_pos_code(x_sample.shape[1], x_sample.dtype)
    zero_state = jnp.zeros((x_prompt.shape[0], 2, SSD_HEADS, SSD_HEAD_DIM, SSD_STATE), x_prompt.dtype)
    xc = x_prompt
    xl = x_sample
    new_states = []
    for l in range(DEPTH):
        p = dict(norm_mix_w=norm_mix_w[l], norm_ffn_w=norm_ffn_w[l], w_in=w_in[l], conv_a_w=conv_a_w[l],
                 w_out_a=w_out_a[l], ssd_conv_w=ssd_conv_w[l], ssd_conv_b=ssd_conv_b[l], ssd_a_log=ssd_a_log[l],
                 ssd_dt_bias=ssd_dt_bias[l], ssd_d=ssd_d[l], ssd_norm_w=ssd_norm_w[l], w_out_b=w_out_b[l],
                 sgu_ln_g=sgu_ln_g[l], sgu_ln_b=sgu_ln_b[l], sgu_w=sgu_w[l], sgu_b=sgu_b[l], w_out_c=w_out_c[l],
                 w_o=w_o[l], router_g_w=router_g_w[l], router_g_b=router_g_b[l], router_e_w=router_e_w[l],
                 router_e_b=router_e_b[l], exp_w_gate=exp_w_gate[l], exp_w_up=exp_w_up[l],
                 exp_w_down=exp_w_down[l])
        mod_ctx = jax.nn.silu(c_ctx)[None, :] @ w_ada[l] + b_ada[l]
        mod_lat = jax.nn.silu(c) @ w_ada[l] + b_ada[l]
        xc, st = layer(xc, mod_ctx, None, zero_state, p)
        new_states.append(st)
        xl, _ = layer(xl, mod_lat, pos, state_ssd[:, l], p)
    y_prompt = rmsnorm(xc, final_norm_w)
    y_sample = rmsnorm(xl, final_norm_w)
    new_state_ssd = jnp.stack(new_states, axis=1)
    return (y_prompt, y_sample, new_state_ssd)
```

```python
import numpy as np
from contextlib import ExitStack
import concourse.bass as bass
import concourse.mybir as mybir
from concourse.bass_utils import run_bass_kernel_spmd

F32 = mybir.dt.float32
F32R = mybir.dt.float32r
AF = mybir.ActivationFunctionType
ALU = mybir.AluOpType

D = 2048
KD = 16
IN_DIM = 16448
ADA = 6 * D
H = 32
HP = 64
NS = 128
G = 4
NE = 16
DE = 512
EPS = 1e-6
BIG = 30000.0
C_AB, C_AC, C_AH, C_Z, C_XBC, C_DT, C_SU, C_SV, C_GATE = 0, 1024, 2048, 3072, 5120, 8192, 8256, 9280, 10304
ARENA = 24 * 1024
NDS = 96


class Tile:
    def __init__(self, r, f):
        self.r = r
        self.f = f
        self.w = None
        self.rd = {}
        self.dsem = None


class Prog:
    ENG = ['pe', 'act', 'dve', 'pool', 'sp']

    def __init__(self, nc, es, arena, arena_r, psum):
        self.nc = nc
        self.q = {e: [] for e in self.ENG}
        self.csem = {e: es.enter_context(nc.semaphore('c_' + e)) for e in self.ENG}
        self.cnt = {e: 0 for e in self.ENG}
        self.dsems = [es.enter_context(nc.semaphore('d%d' % i)) for i in range(NDS)]
        self.dcnt = [0] * NDS
        self.dfree = list(range(NDS))
        self.seen = {e: {} for e in self.ENG}
        self.arena = arena
        self.arena_r = arena_r
        self.off = 0
        self.off_r = 0
        self.phase_tiles = []
        self.pbanks = [Tile(None, p[:]) for p in psum]
        self.pidx = 0

    def alloc(self, *shape, r=False):
        n = int(np.prod(shape))
        if r:
            assert self.off_r + n <= ARENA, ("arena_r overflow", self.off_r, n)
            r = self.arena_r[:, self.off_r:self.off_r + n]
            f = r.bitcast(F32)
            self.off_r += n
        else:
            assert self.off + n <= ARENA, ("arena overflow", self.off, n)
            f = self.arena[:, self.off:self.off + n]
            r = f
            self.off += n
        if len(shape) == 2:
            r = r.rearrange("p (a b) -> p a b", a=shape[0])
            f = f.rearrange("p (a b) -> p a b", a=shape[0])
        elif len(shape) == 3:
            r = r.rearrange("p (a b c) -> p a b c", a=shape[0], b=shape[1])
            f = f.rearrange("p (a b c) -> p a b c", a=shape[0], b=shape[1])
        t = Tile(r, f)
        self.phase_tiles.append(t)
        return t

    def allocr(self, *shape):
        return self.alloc(*shape, r=True)

    def mark(self):
        return (self.off, len(self.phase_tiles), self.off_r)

    def reset(self, mark):
        self.barrier()
        off, nt, off_r = mark
        for t in self.phase_tiles[nt:]:
            if t.dsem is not None:
                self.dfree.append(t.dsem)
        del self.phase_tiles[nt:]
        self.off = off
        self.off_r = off_r

    def ps(self):
        t = self.pbanks[self.pidx]
        self.pidx = (self.pidx + 1) % len(self.pbanks)
        return t

    def _semobj(self, k):
        return self.csem[k[1]] if k[0] == 'c' else self.dsems[k[1]]

    def _deps(self, eng, reads, writes):
        need = {}

        def add(ev):
            if ev is None:
                return
            k, v = ev
            if k == ('c', 'pe') and eng == 'pe':
                return
            if need.get(k, 0) < v:
                need[k] = v
        for t in reads:
            add(t.w)
        for t in writes:
            add(t.w)
            for k, v in t.rd.items():
                add((k, v))
        waits = []
        for k, v in need.items():
            if self.seen[eng].get(k, 0) >= v:
                continue
            self.seen[eng][k] = v
            waits.append((self._semobj(k), v))
        return waits

    def _commit(self, ev, reads, writes):
        k, v = ev
        for t in reads:
            if t.rd.get(k, 0) < v:
                t.rd[k] = v
        for t in writes:
            t.w = ev
            t.rd = {}

    def op(self, eng, fn, reads=(), writes=()):
        waits = self._deps(eng, reads, writes)
        self.cnt[eng] += 1
        ev = (('c', eng), self.cnt[eng])
        self._commit(ev, reads, writes)
        sem = self.csem[eng]

        def run(e):
            for s, v in waits:
                e.wait_ge(s, v)
            ins = fn(e)
            ins.then_inc(sem, 1)
        self.q[eng].append(run)

    def dma(self, eng, tile, pairs, write, slow=False):
        reads, writes = ((), (tile,)) if write else ((tile,), ())
        waits = self._deps(eng, reads, writes)
        if tile.dsem is None:
            tile.dsem = self.dfree.pop()
        i = tile.dsem
        self.dcnt[i] += 16 * len(pairs)
        ev = (('d', i), self.dcnt[i])
        self._commit(ev, reads, writes)
        sem = self.dsems[i]
        kw = dict(allow_slow_non_contiguous=True) if slow else {}

        def run(e):
            for s, v in waits:
                e.wait_ge(s, v)
            for (o, a) in pairs:
                e.dma_start(out=o, in_=a, **kw).then_inc(sem, 16)
        self.q[eng].append(run)

    def load(self, tile, out_ap, in_ap, eng='sp', slow=False):
        self.dma(eng, tile, [(out_ap, in_ap)], True, slow)

    def store(self, tile, out_ap, in_ap, eng='sp', slow=False):
        self.dma(eng, tile, [(out_ap, in_ap)], False, slow)

    def barrier(self):
        for e in self.ENG:
            waits = []
            for e2 in self.ENG:
                v = self.cnt[e2]
                k = ('c', e2)
                if v and self.seen[e].get(k, 0) < v:
                    self.seen[e][k] = v
                    waits.append((self.csem[e2], v))
            for i in range(NDS):
                v = self.dcnt[i]
                k = ('d', i)
                if v and self.seen[e].get(k, 0) < v:
                    self.seen[e][k] = v
                    waits.append((self.dsems[i], v))
            if waits:
                self.q[e].append(lambda eng, waits=waits: [eng.wait_ge(s, v) for s, v in waits])

    def emit(self, block):
        m = {'pe': block.tensor, 'act': block.scalar, 'dve': block.vector, 'pool': block.gpsimd, 'sp': block.sync}
        for en in self.ENG:
            def f(eng, en=en):
                for run in self.q[en]:
                    run(eng)
            m[en](f)

    def mm(self, pt, out, pairs, reads):
        n = len(pairs)

        def fn(e):
            ins = None
            for i, (l, r) in enumerate(pairs):
                ins = e.matmul(out, l, r, start=(i == 0), stop=(i == n - 1))
            return ins
        self.op('pe', fn, reads, [pt])

    def tr(self, pt, out, in_, ident, reads):
        self.op('pe', lambda e: e.transpose(out, in_, ident), reads, [pt])

    def act(self, out, in_, func, reads, writes, eng='act', **kw):
        self.op('act', lambda e: e.activation(out=out, in_=in_, func=func, **kw), reads, writes)

    def tt(self, eng, out, in0, in1, op, reads, writes):
        self.op(eng, lambda e: e.tensor_tensor(out=out, in0=in0, in1=in1, op=op), reads, writes)

    def ts(self, eng, out, in0, s1, s2, op0, op1, reads, writes):
        if s2 is None:
            self.op(eng, lambda e: e.tensor_scalar(out=out, in0=in0, scalar1=s1, scalar2=None, op0=op0), reads, writes)
        else:
            self.op(eng, lambda e: e.tensor_scalar(out=out, in0=in0, scalar1=s1, scalar2=s2, op0=op0, op1=op1), reads, writes)

    def stt(self, out, in0, scalar, in1, op0, op1, reads, writes):
        self.op('dve', lambda e: e.scalar_tensor_tensor(out=out, in0=in0, scalar=scalar, in1=in1, op0=op0, op1=op1),
                reads, writes)

    def copy(self, eng, out, in_, reads, writes):
        if eng == 'act':
            self.op('act', lambda e: e.activation(out=out, in_=in_, func=AF.Copy), reads, writes)
        else:
            self.op(eng, lambda e: e.tensor_copy(out=out, in_=in_), reads, writes)


def bc(ap, shape):
    return ap.to_broadcast(list(shape))


def build(cfg):
    SEQS = cfg['seqs']
    DEPTH = cfg.get('depth', 2)
    DBG = cfg.get('debug', False)
    NT = sum(l for l, _ in SEQS)
    LS = max([l for l, k in SEQS if k == 0] + [128])
    NPR = sum(1 for _, k in SEQS if k == 1)
    NPROW = NT + 2 * len(SEQS)
    chunks = []
    t0 = 0
    pb = 0
    seqinfo = []
    pri = 0
    for s, (l, k) in enumerate(SEQS):
        nci = l // 128
        first = len(chunks)
        for ci in range(nci):
            chunks.append(dict(s=s, ci=ci, t0=t0 + ci * 128, pr=pb + 1 + ci * 128, kind=k, nci=nci, p0=ci * 128))
        seqinfo.append(dict(first=first, n=nci, kind=k, pb=pb, len=l, t0=t0, pri=(pri if k == 1 else -1)))
        if k == 1:
            pri += 1
        t0 += l
        pb += l + 2
    NCH = len(chunks)

    nc = bass.Bass("TRN2", target_bir_lowering=False)

    def din(name, shape, dt=F32):
        return nc.dram_tensor(name, list(shape), dt, kind="ExternalInput").ap()

    def dscr(name, shape):
        return nc.dram_tensor(name, list(shape), F32, kind=("ExternalOutput" if DBG else "Internal")).ap()

    x_in = din("x_in", [NT, D])
    c2 = din("c2", [2, D])
    h0 = din("h0", [2, 2, H * HP, NS])
    posT = din("posT", [D, LS])
    consts = din("consts", [128, 6, 128])
    w_ada = din("w_ada", [2, D, ADA], F32R)
    b_ada = din("b_ada", [2, ADA])
    w_in = din("w_in", [2, D, IN_DIM], F32R)
    norm_mix_w = din("norm_mix_w", [2, D])
    norm_ffn_w = din("norm_ffn_w", [2, D])
    conv_a_w = din("conv_a_w", [2, 3, 1024])
    w_out_a = din("w_out_a", [2, 1024, D], F32R)
    ssd_conv_w = din("ssd_conv_w", [2, 3, 3072])
    ssd_conv_b = din("ssd_conv_b", [2, 3072])
    ssd_a_log = din("ssd_a_log", [2, 2, H])
    ssd_dt_bias = din("ssd_dt_bias", [2, 2 * H])
    ssd_d = din("ssd_d", [2, H])
    ssd_norm_w = din("ssd_norm_w", [2, D])
    w_out_b = din("w_out_b", [2, D, D], F32R)
    sgu_ln_g = din("sgu_ln_g", [2, 1024])
    sgu_ln_b = din("sgu_ln_b", [2, 1024])
    sgu_w = din("sgu_w", [2, 8, 128, 128])
    sgu_b = din("sgu_b", [2, 128, 8])
    w_out_c = din("w_out_c", [2, 1024, D], F32R)
    w_o = din("w_o", [2, D, D], F32R)
    router_w = din("router_w", [2, D, 20])
    router_b = din("router_b", [2, 20])
    exp_w_gate = din("exp_w_gate", [2, NE, D, DE], F32R)
    exp_w_up = din("exp_w_up", [2, NE, D, DE], F32R)
    exp_w_down = din("exp_w_down", [2, NE, DE, D], F32R)
    final_norm_w = din("final_norm_w", [1, D])

    y_out = nc.dram_tensor("y_out", [NT, D], F32, kind="ExternalOutput").ap()
    st_out = nc.dram_tensor("st_out", [max(NPR, 1), 2, 2, H, HP, NS], F32, kind="ExternalOutput").ap()

    PROJ_A = dscr("PROJ", [NPROW, 8192])
    PROJ_B = dscr("PROJB", [NPROW, IN_DIM - 8192])

    class _Proj:
        def __getitem__(self, key):
            rs, cs = key
            if cs.start >= 8192:
                return PROJ_B[rs, cs.start - 8192:cs.stop - 8192]
            assert cs.stop <= 8192
            return PROJ_A[rs, cs]
    PROJ = _Proj()
    XBC = dscr("XBC", [NT, 3072])
    DT = dscr("DT", [NT, 64])
    U = dscr("U", [NT, 4096])
    YP = dscr("YP", [NT, D])
    XR = dscr("XR", [NT, D])
    MOD = dscr("MOD", [2, ADA])
    XM = dscr("XM", [NT, D])
    COMB = dscr("COMB", [NT, 16])

    es = ExitStack()
    with es:
        arena_t = es.enter_context(nc.sbuf_tensor("arena", [128, ARENA], F32))
        arena_r = es.enter_context(nc.sbuf_tensor("arena_r", [128, ARENA], F32R))
        psum = [es.enter_context(nc.psum_tensor("pb%d" % i, [128, 512], F32)) for i in range(8)]
        block = es.enter_context(nc.Block())
        P = Prog(nc, es, arena_t, arena_r, psum)

        cst = P.alloc(6, 128)
        P.load(cst, cst.f, consts)
        ident, Umat, UTmat, ones, maskF, maskB = [cst.f[:, i, :] for i in range(6)]
        modT = P.alloc(96, 2)
        AB = P.alloc(4, KD, 2)
        nwT = P.alloc(2, KD)
        mk = P.mark()
        zrow = P.alloc(2048)
        P.op('pool', lambda e: e.memset(zrow.f, 0.0), [], [zrow])
        for si in seqinfo:
            for rr in (si['pb'], si['pb'] + si['len'] + 1):
                P.store(zrow, PROJ_A[rr:rr + 1, :].rearrange("o (a b) -> (o a) b", a=4), zrow.f[0:4, :])
        P.reset(mk)

        def wtile_load(t, dst, src):
            P.load(t, dst, src, eng='pool')

        def make_hT(xt, which, ch, hT, col0, tmp, posB=None, hT32=None):
            r = 1 if ch['kind'] == 0 else 0
            Aap = AB.f[:, 2 * which, :, r]
            Bap = AB.f[:, 2 * which + 1, :, r]
            ss = tmp['ss']
            junk = tmp['junk']
            P.op('act', lambda e: e.activation(out=junk.f, in_=xt.f, func=AF.Square, accum_out=ss.f[:, 0:1]),
                 [xt], [junk, ss])
            P.ts('dve', ss.f[:, 1:2], ss.f[:, 0:1], 1.0 / D, EPS, ALU.mult, ALU.add, [ss], [ss])
            P.act(ss.f[:, 2:3], ss.f[:, 1:2], AF.Sqrt, [ss], [ss])
            P.op('dve', lambda e: e.reciprocal(out=ss.f[:, 3:4], in_=ss.f[:, 2:3]), [ss], [ss])
            P.op('act', lambda e: e.activation(out=junk.f, in_=xt.f, func=AF.Copy, scale=ss.f[:, 3:4]), [xt, ss], [junk])
            for q4 in range(4):
                pt = P.ps()
                for q in range(4):
                    k = q4 * 4 + q
                    P.tr(pt, pt.f[:, q * 128:(q + 1) * 128], junk.f[:, k * 128:(k + 1) * 128], ident, [junk, cst])
                for q in range(4):
                    k = q4 * 4 + q
                    in1 = posB.f[:, k, :] if posB is not None else bc(Bap[:, k:k + 1], [128, 128])
                    rds = [pt, AB] + ([posB] if posB is not None else [])
                    P.stt(hT.r[:, k, col0:col0 + 128], pt.f[:, q * 128:(q + 1) * 128], Aap[:, k:k + 1], in1,
                          ALU.mult, ALU.add, rds, [hT])
                    if hT32 is not None:
                        P.stt(hT32.f[:, k, :], pt.f[:, q * 128:(q + 1) * 128], Aap[:, k:k + 1], in1,
                              ALU.mult, ALU.add, rds, [hT32])

        for l in range(DEPTH):
            xsrc = x_in if l == 0 else XR
            mk = P.mark()
            sc = P.alloc(D)
            scT = P.allocr(KD, 2)
            modrow = P.alloc(ADA)
            badats = [P.alloc(512) for _ in range(2)]
            wr = [P.allocr(KD, 512) for _ in range(2)]
            P.load(sc, sc.f[0:2, :], c2)
            P.act(sc.f[0:2, :], sc.f[0:2, :], AF.Silu, [sc], [sc])
            pt = P.ps()
            for k in range(KD):
                P.tr(pt, pt.f[:, 2 * k:2 * k + 2], sc.f[0:2, k * 128:(k + 1) * 128], ident[0:2, 0:2], [sc, cst])
            P.copy('dve', scT.r, pt.f[:, 0:32].rearrange("p (k r) -> p k r", r=2), [pt], [scT])
            for nt in range(ADA // 512):
                w = wr[nt % 2]
                wtile_load(w, w.r, w_ada[l, :, nt * 512:(nt + 1) * 512].rearrange("(k p) n -> p k n", p=128))
                badat = badats[nt % 2]
                P.dma('sp', badat, [(badat.f[0:1, :], b_ada[l:l + 1, nt * 512:(nt + 1) * 512]),
                                    (badat.f[1:2, :], b_ada[l:l + 1, nt * 512:(nt + 1) * 512])], True)
                pt = P.ps()
                P.mm(pt, pt.f[0:2, :], [(scT.r[:, k, :], w.r[:, k, :]) for k in range(KD)], [scT, w])
                P.tt('dve', modrow.f[0:2, nt * 512:(nt + 1) * 512], pt.f[0:2, :], badat.f[0:2, :],
                     ALU.add, [pt, badat], [modrow])
            P.store(modrow, MOD, modrow.f[0:2, :])
            pt = P.ps()
            for c in range(96):
                P.tr(pt, pt.f[:, 2 * c:2 * c + 2], modrow.f[0:2, c * 128:(c + 1) * 128], ident[0:2, 0:2], [modrow, cst])
            P.copy('dve', modT.f, pt.f[:, 0:192].rearrange("p (c r) -> p c r", r=2), [pt], [modT])
            P.load(nwT, nwT.f[:, 0, :], norm_mix_w[l].rearrange("(k p) -> p k", p=128), slow=True)
            P.load(nwT, nwT.f[:, 1, :], norm_ffn_w[l].rearrange("(k p) -> p k", p=128), slow=True)
            for which, (isc, ish) in enumerate(((1, 0), (4, 3))):
                for r in range(2):
                    P.stt(AB.f[:, 2 * which, :, r], modT.f[:, isc * 16:(isc + 1) * 16, r], 1.0, nwT.f[:, which, :],
                          ALU.add, ALU.mult, [modT, nwT], [AB])
                    P.copy('dve', AB.f[:, 2 * which + 1, :, r], modT.f[:, ish * 16:(ish + 1) * 16, r], [modT], [AB])
            P.reset(mk)

            mk = P.mark()
            TS = 4
            hT = P.allocr(KD, TS * 128)
            xts = [P.alloc(D) for _ in range(2)]
            tmp = dict(ss=P.alloc(8), junk=P.alloc(D))
            posb = P.alloc(KD, 128)
            wts = [P.allocr(KD, 512) for _ in range(2)]
            outs = [P.alloc(512) for _ in range(4)]
            oi = 0
            wi = 0
            for g0 in range(0, NCH, TS):
                grp = chunks[g0:g0 + TS]
                for j, ch in enumerate(grp):
                    xt = xts[j % 2]
                    P.load(xt, xt.f, xsrc[ch['t0']:ch['t0'] + 128, :])
                    pB = None
                    if ch['kind'] == 0:
                        P.load(posb, posb.f, posT[:, ch['p0']:ch['p0'] + 128].rearrange("(k p) t -> p k t", p=128))
                        P.tt('pool', posb.f, posb.f, bc(AB.f[:, 1, :, 1:2], [128, KD, 128]), ALU.add, [posb, AB], [posb])
                        pB = posb
                    make_hT(xt, 0, ch, hT, j * 128, tmp, posB=pB)
                ntile = (IN_DIM + 511) // 512
                for nt in range(ntile):
                    ncol = min(512, IN_DIM - nt * 512)
                    w = wts[wi % 2]
                    wi += 1
                    wtile_load(w, w.r[:, :, 0:ncol], w_in[l, :, nt * 512:nt * 512 + ncol].rearrange("(k p) n -> p k n", p=128))
                    for j, ch in enumerate(grp):
                        pt = P.ps()
                        P.mm(pt, pt.f[:, 0:ncol], [(hT.r[:, k, j * 128:(j + 1) * 128], w.r[:, k, 0:ncol]) for k in range(KD)],
                             [hT, w])
                        o = outs[oi % 4]
                        oi += 1
                        P.copy('act' if oi % 2 else 'dve', o.f[:, 0:ncol], pt.f[:, 0:ncol], [pt], [o])
                        P.store(o, PROJ[ch['pr']:ch['pr'] + 128, nt * 512:nt * 512 + ncol], o.f[:, 0:ncol])
            P.reset(mk)
            if cfg.get('stop') == 'proj':
                break

            mk = P.mark()
            cw = P.alloc(3, 3072)
            cb = P.alloc(3072)
            dtb = P.alloc(64)
            dtt = P.alloc(64)
            tin = [P.alloc(3072) for _ in range(3)]
            P.load(cw, cw.f, ssd_conv_w[l].partition_broadcast(128))
            P.load(cb, cb.f, ssd_conv_b[l].partition_broadcast(128))
            P.load(dtb, dtb.f, ssd_dt_bias[l].partition_broadcast(128))
            for ch in chunks:
                pr, t0 = ch['pr'], ch['t0']
                for s3 in range(3):
                    P.load(tin[s3], tin[s3].f, PROJ[pr - 1 + s3:pr - 1 + s3 + 128, C_XBC:C_XBC + 3072])
                    P.tt('pool', tin[s3].f, tin[s3].f, cw.f[:, s3, :], ALU.mult, [tin[s3], cw], [tin[s3]])
                P.tt('dve', tin[0].f, tin[0].f, tin[1].f, ALU.add, [tin[0], tin[1]], [tin[0]])
                P.tt('dve', tin[0].f, tin[0].f, tin[2].f, ALU.add, [tin[0], tin[2]], [tin[0]])
                P.tt('dve', tin[0].f, tin[0].f, cb.f, ALU.add, [tin[0], cb], [tin[0]])
                P.act(tin[1].f, tin[0].f, AF.Silu, [tin[0]], [tin[1]])
                P.store(tin[1], XBC[t0:t0 + 128, :], tin[1].f)
                P.load(dtt, dtt.f, PROJ[pr:pr + 128, C_DT:C_DT + 64])
                P.tt('dve', dtt.f, dtt.f, dtb.f, ALU.add, [dtt, dtb], [dtt])
                P.act(dtt.f, dtt.f, AF.Exp, [dtt], [dtt])
                P.ts('dve', dtt.f, dtt.f, 1.0, None, ALU.add, None, [dtt], [dtt])
                P.act(dtt.f, dtt.f, AF.Ln, [dtt], [dtt])
                P.store(dtt, DT[t0:t0 + 128, :], dtt.f)
            P.reset(mk)

            mk = P.mark()
            caw = P.alloc(3, 1024)
            tab = P.alloc(1024)
            tch = [P.alloc(2048) for _ in range(3)]
            P.load(caw, caw.f, conv_a_w[l].partition_broadcast(128))
            for ch in chunks:
                pr, t0 = ch['pr'], ch['t0']
                P.load(tab, tab.f, PROJ[pr:pr + 128, C_AB:C_AB + 1024])
                for s3 in range(3):
                    t = tch[s3]
                    P.load(t, t.f, PROJ[pr - 1 + s3:pr - 1 + s3 + 128, C_AC:C_AC + 2048])
                    P.tt('pool', t.f[:, 0:1024], t.f[:, 0:1024], t.f[:, 1024:2048], ALU.mult, [t], [t])
                    P.tt('pool', t.f[:, 0:1024], t.f[:, 0:1024], caw.f[:, s3, :], ALU.mult, [t, caw], [t])
                a0 = tch[0]
                P.tt('dve', a0.f[:, 0:1024], a0.f[:, 0:1024], tch[1].f[:, 0:1024], ALU.add, [a0, tch[1]], [a0])
                P.tt('dve', a0.f[:, 0:1024], a0.f[:, 0:1024], tch[2].f[:, 0:1024], ALU.add, [a0, tch[2]], [a0])
                P.tt('dve', a0.f[:, 0:1024], a0.f[:, 0:1024], tab.f, ALU.mult, [a0, tab], [a0])
                P.store(a0, U[t0:t0 + 128, 0:1024], a0.f[:, 0:1024])
            P.reset(mk)

            mk = P.mark()
            lng = P.alloc(1024)
            lnb = P.alloc(1024)
            wT = P.alloc(8, 128)
            wtmp = P.alloc(8, 128)
            sgb = P.alloc(8)
            suv = P.alloc(2048)
            gt_ = P.alloc(2048)
            junkc3 = P.alloc(1024)
            stc3 = P.alloc(8)
            uc = P.alloc(1024)
            P.load(lng, lng.f, sgu_ln_g[l].partition_broadcast(128))
            P.load(lnb, lnb.f, sgu_ln_b[l].partition_broadcast(128))
            P.load(sgb, sgb.f, sgu_b[l])
            P.load(wtmp, wtmp.f, sgu_w[l].rearrange("g i j -> i g j"))
            for hf in range(2):
                pt = P.ps()
                for q in range(4):
                    P.tr(pt, pt.f[:, q * 128:(q + 1) * 128], wtmp.f[:, hf * 4 + q, :], ident, [wtmp, cst])
                P.copy('dve', wT.f[:, hf * 4:(hf + 1) * 4, :], pt.f.rearrange("p (a b) -> p a b", a=4), [pt], [wT])
            for ch in chunks:
                pr, t0 = ch['pr'], ch['t0']
                P.load(suv, suv.f, PROJ[pr:pr + 128, C_SU:C_SU + 2048])
                P.tt('pool', gt_.f, suv.f, suv.f, ALU.mult, [suv], [gt_])
                P.ts('dve', gt_.f, gt_.f, 0.044715, 1.0, ALU.mult, ALU.add, [gt_], [gt_])
                P.tt('dve', gt_.f, gt_.f, suv.f, ALU.mult, [gt_, suv], [gt_])
                P.act(gt_.f, gt_.f, AF.Sigmoid, [gt_], [gt_], scale=1.5957691216057308)
                P.tt('dve', suv.f, suv.f, gt_.f, ALU.mult, [suv, gt_], [suv])
                u_ = suv.f[:, 0:1024]
                v_ = suv.f[:, 1024:2048]
                P.op('dve', lambda e, v_=v_: e.tensor_reduce(out=stc3.f[:, 0:1], in_=v_, axis=mybir.AxisListType.X, op=ALU.add),
                     [suv], [stc3])
                P.ts('dve', stc3.f[:, 1:2], stc3.f[:, 0:1], -1.0 / 1024, None, ALU.mult, None, [stc3], [stc3])
                P.ts('dve', v_, v_, stc3.f[:, 1:2], None, ALU.add, None, [suv, stc3], [suv])
                P.op('act', lambda e, v_=v_: e.activation(out=junkc3.f, in_=v_, func=AF.Square, accum_out=stc3.f[:, 2:3]),
                     [suv], [junkc3, stc3])
                P.ts('dve', stc3.f[:, 3:4], stc3.f[:, 2:3], 1.0 / 1024, EPS, ALU.mult, ALU.add, [stc3], [stc3])
                P.act(stc3.f[:, 4:5], stc3.f[:, 3:4], AF.Sqrt, [stc3], [stc3])
                P.op('dve', lambda e: e.reciprocal(out=stc3.f[:, 5:6], in_=stc3.f[:, 4:5]), [stc3], [stc3])
                P.stt(v_, v_, stc3.f[:, 5:6], lng.f, ALU.mult, ALU.mult, [suv, stc3, lng], [suv])
                P.tt('dve', v_, v_, lnb.f, ALU.add, [suv, lnb], [suv])
                for hf in range(2):
                    pt = P.ps()
                    for q in range(4):
                        g = hf * 4 + q
                        P.mm(pt, pt.f[:, q * 128:(q + 1) * 128], [(wT.f[:, g, :], suv.f[:, 1024 + g * 128:1024 + (g + 1) * 128])],
                             [wT, suv])
                    for q in range(4):
                        g = hf * 4 + q
                        P.stt(uc.f[:, g * 128:(g + 1) * 128], pt.f[:, q * 128:(q + 1) * 128], sgb.f[:, g:g + 1],
                              suv.f[:, g * 128:(g + 1) * 128], ALU.add, ALU.mult, [pt, sgb, suv], [uc])
                P.store(uc, U[t0:t0 + 128, 3072:4096], uc.f)
            P.reset(mk)
            if cfg.get('stop') == 'c3':
                break

            mk = P.mark()
            S = P.alloc(2048)
            xbc = P.alloc(3072)
            zt = P.alloc(2048)
            ypt = P.alloc(2048)
            xw = P.alloc(2048)
            yt = P.alloc(2048)
            nwbc = P.alloc(2048)
            Rs = [P.alloc(4, 128) for _ in range(2)]
            Qs = [P.alloc(4, 128) for _ in range(2)]
            BT = P.alloc(4, 128)
            CT = P.alloc(4, 128)
            ST = P.alloc(4, 128)
            Lts = [P.alloc(4, 128) for _ in range(2)]
            WTs = [P.alloc(4, 128) for _ in range(2)]
            abc = P.alloc(2, 32)
            Dbc = P.alloc(32)
            dts = P.alloc(32)
            da = P.alloc(32)
            PT = P.alloc(64)
            lnw = P.alloc(32)
            biasL = P.alloc(32)
            eP = P.alloc(32)
            etot = P.alloc(32)
            wdec = P.alloc(32)
            st4 = P.alloc(16)
            P.load(abc, abc.f, ssd_a_log[l].partition_broadcast(128))
            P.act(abc.f, abc.f, AF.Exp, [abc], [abc])
            P.ts('dve', abc.f, abc.f, -1.0, None, ALU.mult, None, [abc], [abc])
            P.load(Dbc, Dbc.f, ssd_d[l].partition_broadcast(128))
            P.load(nwbc, nwbc.f, ssd_norm_w[l].partition_broadcast(128))
            li = 0
            for dr in range(2):
                cum = Umat if dr == 0 else UTmat
                msk = maskF if dr == 0 else maskB
                for si in seqinfo:
                    if si['kind'] == 0:
                        P.load(xw, xw.f.rearrange("p (k n) -> p k n", k=16), h0[l, dr].rearrange("(k p) n -> p k n", p=128))
                        for q4 in range(4):
                            pt = P.ps()
                            for q in range(4):
                                k = q4 * 4 + q
                                P.tr(pt, pt.f[:, q * 128:(q + 1) * 128], xw.f[:, k * 128:(k + 1) * 128], ident, [xw, cst])
                            P.copy('dve', S.f[:, q4 * 512:(q4 + 1) * 512], pt.f, [pt], [S])
                    else:
                        P.op('pool', lambda e: e.memset(S.f, 0.0), [], [S])
                    order = list(range(si['n'])) if dr == 0 else list(range(si['n'] - 1, -1, -1))
                    for ci in order:
                        ch = chunks[si['first'] + ci]
                        pr, t0 = ch['pr'], ch['t0']
                        P.load(xbc, xbc.f, XBC[t0:t0 + 128, :])
                        P.load(dts, dts.f, DT[t0:t0 + 128, dr * 32:(dr + 1) * 32])
                        if dr == 1:
                            P.load(zt, zt.f, PROJ[pr:pr + 128, C_Z:C_Z + 2048])
                            P.load(ypt, ypt.f, YP[t0:t0 + 128, :])
                        P.tt('dve', da.f, dts.f, abc.f[:, dr, :], ALU.mult, [dts, abc], [da])
                        pt0 = P.ps()
                        P.mm(pt0, pt0.f[:, 0:32], [(cum, da.f)], [cst, da])
                        P.mm(pt0, pt0.f[:, 32:64], [(ones, da.f)], [cst, da])
                        P.copy('dve', PT.f, pt0.f[:, 0:64], [pt0], [PT])
                        P.act(lnw.f, dts.f, AF.Ln, [dts], [lnw])
                        P.tt('dve', biasL.f, lnw.f, PT.f[:, 0:32], ALU.subtract, [lnw, PT], [biasL])
                        P.act(eP.f, PT.f[:, 0:32], AF.Exp, [PT], [eP])
                        P.act(etot.f, PT.f[:, 32:64], AF.Exp, [PT], [etot])
                        P.tt('dve', wdec.f, biasL.f, PT.f[:, 32:64], ALU.add, [biasL, PT], [wdec])
                        P.act(wdec.f, wdec.f, AF.Exp, [wdec], [wdec])
                        ptb = P.ps()
                        ptc = P.ps()
                        for g in range(4):
                            P.tr(ptb, ptb.f[:, g * 128:(g + 1) * 128], xbc.f[:, 2048 + g * 128:2048 + (g + 1) * 128], ident, [xbc, cst])
                            P.tr(ptc, ptc.f[:, g * 128:(g + 1) * 128], xbc.f[:, 2560 + g * 128:2560 + (g + 1) * 128], ident, [xbc, cst])
                        P.copy('act', BT.f, ptb.f.rearrange("p (a b) -> p a b", a=4), [ptb], [BT])
                        P.copy('dve', CT.f, ptc.f.rearrange("p (a b) -> p a b", a=4), [ptc], [CT])
                        pts = P.ps()
                        for g in range(4):
                            P.mm(pts, pts.f[:, g * 128:(g + 1) * 128], [(BT.f[:, g, :], CT.f[:, g, :])], [BT, CT])
                        P.copy('act', ST.f, pts.f.rearrange("p (a b) -> p a b", a=4), [pts], [ST])
                        for g in range(4):
                            pty = P.ps()
                            pto = P.ps()
                            P.mm(pto, pto.f, [(CT.f[:, g, :], S.f[:, g * 512:(g + 1) * 512])], [CT, S])
                            for hh in range(2):
                                hb = g * 8 + hh * 4
                                R_, Q_, Lt, WT = Rs[li % 2], Qs[li % 2], Lts[li % 2], WTs[li % 2]
                                li += 1
                                P.tt('pool', R_.f, bc(cum.unsqueeze(1), [128, 4, 128]), bc(da.f[:, hb:hb + 4].unsqueeze(2), [128, 4, 128]),
                                     ALU.mult, [cst, da], [R_])
                                P.tt('pool', Q_.f, bc(msk.unsqueeze(1), [128, 4, 128]), bc(biasL.f[:, hb:hb + 4].unsqueeze(2), [128, 4, 128]),
                                     ALU.add, [cst, biasL], [Q_])
                                ptl = P.ps()
                                P.mm(ptl, ptl.f, [(ones, R_.f.rearrange("p a b -> p (a b)")), (ident, Q_.f.rearrange("p a b -> p (a b)"))],
                                     [cst, R_, Q_])
                                P.act(Lt.f, ptl.f.rearrange("p (a b) -> p a b", a=4), AF.Exp, [ptl], [Lt])
                                P.tt('dve', WT.f, Lt.f, bc(ST.f[:, g:g + 1, :], [128, 4, 128]), ALU.mult, [Lt, ST], [WT])
                                for h4 in range(4):
                                    hd = hb + h4
                                    c0 = (hh * 4 + h4) * 64
                                    P.mm(pty, pty.f[:, c0:c0 + 64], [(WT.f[:, h4, :], xbc.f[:, hd * 64:(hd + 1) * 64])], [WT, xbc])
                            yg = yt.f[:, g * 512:(g + 1) * 512]
                            P.tt('dve', yg.rearrange("p (a b) -> p a b", a=8), pto.f.rearrange("p (a b) -> p a b", a=8),
                                 bc(eP.f[:, g * 8:(g + 1) * 8].unsqueeze(2), [128, 8, 64]), ALU.mult, [pto, eP], [yt])
                            P.tt('dve', yg, yg, pty.f, ALU.add, [yt, pty], [yt])
                        P.tt('pool', xw.f.rearrange("p (a b) -> p a b", a=32), xbc.f[:, 0:2048].rearrange("p (a b) -> p a b", a=32),
                             bc(wdec.f.unsqueeze(2), [128, 32, 64]), ALU.mult, [xbc, wdec], [xw])
                        for g in range(4):
                            ptg = P.ps()
                            P.mm(ptg, ptg.f, [(xbc.f[:, 2048 + g * 128:2048 + (g + 1) * 128], xw.f[:, g * 512:(g + 1) * 512])], [xbc, xw])
                            Sg = S.f[:, g * 512:(g + 1) * 512]
                            P.tt('dve', Sg.rearrange("p (a b) -> p a b", a=8), Sg.rearrange("p (a b) -> p a b", a=8),
                                 bc(etot.f[:, g * 8:(g + 1) * 8].unsqueeze(2), [128, 8, 64]), ALU.mult, [S, etot], [S])
                            P.tt('dve', Sg, Sg, ptg.f, ALU.add, [S, ptg], [S])
                        if dr == 0:
                            P.store(yt, YP[t0:t0 + 128, :], yt.f)
                        else:
                            P.tt('dve', yt.f, yt.f, ypt.f, ALU.add, [yt, ypt], [yt])
                            P.tt('pool', ypt.f.rearrange("p (a b) -> p a b", a=32), xbc.f[:, 0:2048].rearrange("p (a b) -> p a b", a=32),
                                 bc(Dbc.f.unsqueeze(2), [128, 32, 64]), ALU.mult, [xbc, Dbc], [ypt])
                            P.tt('dve', yt.f, yt.f, ypt.f, ALU.add, [yt, ypt], [yt])
                            P.act(zt.f, zt.f, AF.Silu, [zt], [zt])
                            P.tt('dve', yt.f, yt.f, zt.f, ALU.mult, [yt, zt], [yt])
                            for g in range(4):
                                P.op('act', lambda e, g=g: e.activation(out=zt.f[:, g * 512:(g + 1) * 512], in_=yt.f[:, g * 512:(g + 1) * 512],
                                                                       func=AF.Square, accum_out=st4.f[:, g:g + 1]), [yt], [zt, st4])
                            P.ts('dve', st4.f[:, 4:8], st4.f[:, 0:4], 1.0 / 512, EPS, ALU.mult, ALU.add, [st4], [st4])
                            P.act(st4.f[:, 8:12], st4.f[:, 4:8], AF.Sqrt, [st4], [st4])
                            P.op('dve', lambda e: e.reciprocal(out=st4.f[:, 12:16], in_=st4.f[:, 8:12]), [st4], [st4])
                            for g in range(4):
                                P.stt(yt.f[:, g * 512:(g + 1) * 512], yt.f[:, g * 512:(g + 1) * 512], st4.f[:, 12 + g:13 + g],
                                      nwbc.f[:, g * 512:(g + 1) * 512], ALU.mult, ALU.mult, [yt, st4, nwbc], [yt])
                            P.store(yt, U[t0:t0 + 128, 1024:3072], yt.f)
                    if si['kind'] == 1:
                        for half in range(2):
                            for q in range(4):
                                pt = P.ps()
                                for h4 in range(4):
                                    hd = half * 16 + q * 4 + h4
                                    P.tr(pt, pt.f[0:64, h4 * 128:(h4 + 1) * 128], S.f[:, hd * 64:(hd + 1) * 64], ident, [S, cst])
                                P.copy('dve', xw.f[0:64, q * 512:(q + 1) * 512], pt.f[0:64, :], [pt], [xw])
                            P.store(xw, st_out[si['pri'], l, dr, half * 16:(half + 1) * 16].rearrange("h p n -> p h n"),
                                    xw.f[0:64, :].rearrange("p (h n) -> p h n", h=16))
            P.reset(mk)
            if cfg.get('stop') == 'ssd':
                break

            mk = P.mark()
            TDN = 2
            UT = P.allocr(32, TDN * 128)
            mT = P.allocr(KD, TDN * 128)
            wds = [P.allocr(KD, 256) for _ in range(3)]
            utile = P.alloc(4096)
            merged = [P.alloc(2048) for _ in range(TDN)]
            xch = [P.alloc(2048) for _ in range(TDN)]
            gts = [P.alloc(256) for _ in range(2)]
            g1s = [P.alloc(256) for _ in range(2)]
            wi = 0
            gi_ = 0
            ci_ = 0
            for g0 in range(0, NCH, TDN):
                grp = chunks[g0:g0 + TDN]
                for j, ch in enumerate(grp):
                    P.load(utile, utile.f, U[ch['t0']:ch['t0'] + 128, :])
                    P.load(xch[j], xch[j].f, xsrc[ch['t0']:ch['t0'] + 128, :])
                    for q4 in range(8):
                        pt = P.ps()
                        for q in range(4):
                            k = q4 * 4 + q
                            P.tr(pt, pt.f[:, q * 128:(q + 1) * 128], utile.f[:, k * 128:(k + 1) * 128], ident, [utile, cst])
                        ci_ += 1
                        P.copy('act' if ci_ % 2 else 'dve', UT.r[:, q4 * 4:(q4 + 1) * 4, j * 128:(j + 1) * 128],
                               pt.f.rearrange("p (a b) -> p a b", a=4), [pt], [UT])
                for bi, (wsrc, k0, KC, gcol) in enumerate(((w_out_a, 0, 8, 0), (w_out_b, 8, 16, 2048), (w_out_c, 24, 8, 4096))):
                    for nt in range(8):
                        w = wds[wi % 3]
                        wi += 1
                        wtile_load(w, w.r[:, 0:KC, :], wsrc[l, :, nt * 256:(nt + 1) * 256].rearrange("(k p) n -> p k n", p=128))
                        for j, ch in enumerate(grp):
                            pt = P.ps()
                            P.mm(pt, pt.f[:, 0:256], [(UT.r[:, k0 + k, j * 128:(j + 1) * 128], w.r[:, k, :]) for k in range(KC)], [UT, w])
                            gt = gts[gi_ % 2]
                            gi_ += 1
                            c0 = C_GATE + gcol + nt * 256
                            P.load(gt, gt.f, PROJ[ch['pr']:ch['pr'] + 128, c0:c0 + 256])
                            P.act(gt.f, gt.f, AF.Sigmoid, [gt], [gt])
                            mg = merged[j].f[:, nt * 256:(nt + 1) * 256]
                            if bi == 0:
                                P.tt('dve', mg, gt.f, pt.f[:, 0:256], ALU.mult, [gt, pt], [merged[j]])
                            else:
                                P.tt('dve', gt.f, gt.f, pt.f[:, 0:256], ALU.mult, [gt, pt], [gt])
                                P.tt('dve', mg, mg, gt.f, ALU.add, [merged[j], gt], [merged[j]])
                for j, ch in enumerate(grp):
                    for q4 in range(4):
                        pt = P.ps()
                        for q in range(4):
                            k = q4 * 4 + q
                            P.tr(pt, pt.f[:, q * 128:(q + 1) * 128], merged[j].f[:, k * 128:(k + 1) * 128], ident, [merged[j], cst])
                        ci_ += 1
                        P.copy('act' if ci_ % 2 else 'dve', mT.r[:, q4 * 4:(q4 + 1) * 4, j * 128:(j + 1) * 128],
                               pt.f.rearrange("p (a b) -> p a b", a=4), [pt], [mT])
                for nt in range(8):
                    w = wds[wi % 3]
                    wi += 1
                    wtile_load(w, w.r, w_o[l, :, nt * 256:(nt + 1) * 256].rearrange("(k p) n -> p k n", p=128))
                    for j, ch in enumerate(grp):
                        r = 1 if ch['kind'] == 0 else 0
                        pt = P.ps()
                        P.mm(pt, pt.f[:, 0:256], [(mT.r[:, k, j * 128:(j + 1) * 128], w.r[:, k, :]) for k in range(KD)], [mT, w])
                        g1 = g1s[gi_ % 2]
                        gi_ += 1
                        P.load(g1, g1.f, MOD[r, 2 * D + nt * 256:2 * D + (nt + 1) * 256].partition_broadcast(128))
                        P.tt('dve', g1.f, g1.f, pt.f[:, 0:256], ALU.mult, [g1, pt], [g1])
                        xs_ = xch[j].f[:, nt * 256:(nt + 1) * 256]
                        P.tt('dve', xs_, xs_, g1.f, ALU.add, [xch[j], g1], [xch[j]])
                for j, ch in enumerate(grp):
                    P.store(xch[j], XM[ch['t0']:ch['t0'] + 128, :], xch[j].f)
            P.reset(mk)
            if cfg.get('stop') == 'mix':
                break

            mk = P.mark()
            TE = 3
            last = (l == DEPTH - 1)
            h2T = P.allocr(KD, TE * 128)
            actT = P.allocr(4, TE * 128)
            wgu = [[P.allocr(KD, 128) for _ in range(2)] for _ in range(2)]
            wdn = [P.allocr(4, 1024) for _ in range(2)]
            xe = [P.alloc(2048) for _ in range(TE)]
            acc = [P.alloc(2048) for _ in range(TE)]
            tmp = dict(ss=P.alloc(8), junk=P.alloc(D))
            h32 = P.alloc(KD, 128)
            sgs = [P.alloc(TE * 128) for _ in range(2)]
            rw = P.alloc(KD, 20)
            rbb = P.alloc(20)
            lg = P.alloc(20)
            rs = P.alloc(64)
            combs = [P.alloc(16) for _ in range(TE)]
            g2s = [P.alloc(512) for _ in range(2)]
            P.load(rw, rw.f, router_w[l].rearrange("(k p) n -> p k n", p=128))
            P.load(rbb, rbb.f, router_b[l].partition_broadcast(128))
            if last:
                fnw = P.alloc(2048)
                P.load(fnw, fnw.f, final_norm_w[0].partition_broadcast(128))
            ui = 0
            di = 0
            si_ = 0
            g2i = 0
            for g0 in range(0, NCH, TE):
                grp = chunks[g0:g0 + TE]
                nj = len(grp)
                NTK = nj * 128
                for j, ch in enumerate(grp):
                    P.load(xe[j], xe[j].f, XM[ch['t0']:ch['t0'] + 128, :])
                    make_hT(xe[j], 1, ch, h2T, j * 128, tmp, posB=None, hT32=h32)
                    pt = P.ps()
                    P.mm(pt, pt.f[:, 0:20], [(h32.f[:, k, :], rw.f[:, k, :]) for k in range(KD)], [h32, rw])
                    P.tt('dve', lg.f, pt.f[:, 0:20], rbb.f, ALU.add, [pt, rbb], [lg])
                    R_ = rs.f
                    cb_ = combs[j]
                    P.op('dve', lambda e, R_=R_: e.tensor_reduce(out=R_[:, 0:1], in_=lg.f[:, 0:4], axis=mybir.AxisListType.X, op=ALU.max), [lg], [rs])
                    P.ts('dve', R_[:, 1:2], R_[:, 0:1], -1.0, None, ALU.mult, None, [rs], [rs])
                    P.op('act', lambda e, R_=R_: e.activation(out=R_[:, 4:8], in_=lg.f[:, 0:4], func=AF.Exp, bias=R_[:, 1:2], scale=1.0,
                                                            accum_out=R_[:, 2:3]), [lg, rs], [rs])
                    P.op('dve', lambda e, R_=R_: e.reciprocal(out=R_[:, 3:4], in_=R_[:, 2:3]), [rs], [rs])
                    P.ts('dve', R_[:, 8:12], lg.f[:, 0:4], R_[:, 0:1], None, ALU.is_equal, None, [lg, rs], [rs])
                    P.tt('dve', R_[:, 16:32].rearrange("p (g k) -> p g k", g=4), lg.f[:, 4:20].rearrange("p (g k) -> p g k", g=4),
                         bc(R_[:, 8:12].unsqueeze(2), [128, 4, 4]), ALU.mult, [lg, rs], [rs])
                    P.op('dve', lambda e, R_=R_: e.tensor_reduce(out=R_[:, 12:16], in_=R_[:, 16:32].rearrange("p (g k) -> p k g", g=4),
                                                               axis=mybir.AxisListType.X, op=ALU.add), [rs], [rs])
                    P.op('dve', lambda e, R_=R_: e.tensor_reduce(out=R_[:, 32:33], in_=R_[:, 12:16], axis=mybir.AxisListType.X, op=ALU.max), [rs], [rs])
                    P.ts('dve', R_[:, 33:34], R_[:, 32:33], -1.0, None, ALU.mult, None, [rs], [rs])
                    P.op('act', lambda e, R_=R_: e.activation(out=R_[:, 36:40], in_=R_[:, 12:16], func=AF.Exp, bias=R_[:, 33:34], scale=1.0), [rs], [rs])
                    P.ts('dve', R_[:, 40:44], R_[:, 12:16], R_[:, 32:33], None, ALU.is_equal, None, [rs], [rs])
                    P.stt(R_[:, 44:48], R_[:, 40:44], -2.0, R_[:, 36:40], ALU.mult, ALU.add, [rs], [rs])
                    P.op('dve', lambda e, R_=R_: e.tensor_reduce(out=R_[:, 34:35], in_=R_[:, 44:48], axis=mybir.AxisListType.X, op=ALU.max), [rs], [rs])
                    P.ts('dve', R_[:, 48:52], R_[:, 44:48], R_[:, 34:35], None, ALU.is_equal, None, [rs], [rs])
                    P.ts('dve', R_[:, 35:36], R_[:, 34:35], 1.0, None, ALU.add, None, [rs], [rs])
                    P.op('dve', lambda e, R_=R_: e.reciprocal(out=R_[:, 52:53], in_=R_[:, 35:36]), [rs], [rs])
                    P.tt('dve', R_[:, 53:54], R_[:, 52:53], R_[:, 3:4], ALU.mult, [rs], [rs])
                    P.tt('dve', R_[:, 54:55], R_[:, 53:54], R_[:, 34:35], ALU.mult, [rs], [rs])
                    P.ts('dve', R_[:, 56:60], R_[:, 40:44], R_[:, 53:54], None, ALU.mult, None, [rs], [rs])
                    P.stt(R_[:, 56:60], R_[:, 48:52], R_[:, 54:55], R_[:, 56:60], ALU.mult, ALU.add, [rs], [rs])
                    P.tt('dve', cb_.f.rearrange("p (g k) -> p g k", g=4), bc(R_[:, 8:12].unsqueeze(2), [128, 4, 4]),
                         bc(R_[:, 56:60].unsqueeze(1), [128, 4, 4]), ALU.mult, [rs], [cb_])
                    if DBG:
                        P.store(cb_, COMB[ch['t0']:ch['t0'] + 128, :], cb_.f)
                for ex in range(NE):
                    for fc in range(4):
                        wg, wu = wgu[ui % 2]
                        ui += 1
                        wtile_load(wg, wg.r, exp_w_gate[l, ex, :, fc * 128:(fc + 1) * 128].rearrange("(k p) f -> p k f", p=128))
                        wtile_load(wu, wu.r, exp_w_up[l, ex, :, fc * 128:(fc + 1) * 128].rearrange("(k p) f -> p k f", p=128))
                        ptg = P.ps()
                        P.mm(ptg, ptg.f[:, 0:NTK], [(wg.r[:, k, :], h2T.r[:, k, 0:NTK]) for k in range(KD)], [wg, h2T])
                        ptu = P.ps()
                        P.mm(ptu, ptu.f[:, 0:NTK], [(wu.r[:, k, :], h2T.r[:, k, 0:NTK]) for k in range(KD)], [wu, h2T])
                        sg = sgs[si_ % 2]
                        si_ += 1
                        P.act(sg.f[:, 0:NTK], ptg.f[:, 0:NTK], AF.Silu, [ptg], [sg])
                        P.tt('dve', actT.r[:, fc, 0:NTK], sg.f[:, 0:NTK], ptu.f[:, 0:NTK], ALU.mult, [sg, ptu], [actT])
                    for dh in range(2):
                        wd = wdn[di % 2]
                        di += 1
                        wtile_load(wd, wd.r, exp_w_down[l, ex, :, dh * 1024:(dh + 1) * 1024].rearrange("(c p) n -> p c n", p=128))
                        for j in range(nj):
                            for d2 in range(2):
                                pt = P.ps()
                                P.mm(pt, pt.f, [(actT.r[:, fc, j * 128:(j + 1) * 128], wd.r[:, fc, d2 * 512:(d2 + 1) * 512]) for fc in range(4)],
                                     [actT, wd])
                                dst = acc[j].f[:, dh * 1024 + d2 * 512:dh * 1024 + (d2 + 1) * 512]
                                if ex == 0:
                                    P.ts('dve', dst, pt.f, combs[j].f[:, ex:ex + 1], None, ALU.mult, None, [pt, combs[j]], [acc[j]])
                                else:
                                    P.stt(dst, pt.f, combs[j].f[:, ex:ex + 1], dst, ALU.mult, ALU.add, [pt, combs[j], acc[j]], [acc[j]])
                for j, ch in enumerate(grp):
                    r = 1 if ch['kind'] == 0 else 0
                    for d4 in range(4):
                        g2 = g2s[g2i % 2]
                        g2i += 1
                        P.load(g2, g2.f, MOD[r, 5 * D + d4 * 512:5 * D + (d4 + 1) * 512].partition_broadcast(128))
                        sl = slice(d4 * 512, (d4 + 1) * 512)
                        P.tt('dve', g2.f, g2.f, acc[j].f[:, sl], ALU.mult, [g2, acc[j]], [g2])
                        P.tt('dve', xe[j].f[:, sl], xe[j].f[:, sl], g2.f, ALU.add, [xe[j], g2], [xe[j]])
                    if not last:
                        P.store(xe[j], XR[ch['t0']:ch['t0'] + 128, :], xe[j].f)
                    else:
                        if DBG:
                            P.store(xe[j], XR[ch['t0']:ch['t0'] + 128, :], xe[j].f)
                        ss = tmp['ss']
                        junk = tmp['junk']
                        P.op('act', lambda e, j=j: e.activation(out=junk.f, in_=xe[j].f, func=AF.Square, accum_out=ss.f[:, 0:1]),
                             [xe[j]], [junk, ss])
                        P.ts('dve', ss.f[:, 1:2], ss.f[:, 0:1], 1.0 / D, EPS, ALU.mult, ALU.add, [ss], [ss])
                        P.act(ss.f[:, 2:3], ss.f[:, 1:2], AF.Sqrt, [ss], [ss])
                        P.op('dve', lambda e: e.reciprocal(out=ss.f[:, 3:4], in_=ss.f[:, 2:3]), [ss], [ss])
                        P.stt(junk.f, xe[j].f, ss.f[:, 3:4], fnw.f, ALU.mult, ALU.mult, [xe[j], ss, fnw], [junk])
                        P.store(junk, y_out[ch['t0']:ch['t0'] + 128, :], junk.f)
            P.reset(mk)

        P.barrier()
        P.emit(block)
    return nc


def make_consts():
    k = np.arange(128)[:, None]
    i = np.arange(128)[None, :]
    c = np.zeros((128, 6, 128), np.float32)
    c[:, 0] = (k == i)
    c[:, 1] = (k <= i)
    c[:, 2] = (k >= i)
    c[:, 3] = 1.0
    c[:, 4] = np.where(k > i, -BIG, 0.0)
    c[:, 5] = np.where(k < i, -BIG, 0.0)
    return c


def make_posT(n_tok):
    rows = n_tok // 64
    rr, cc = np.meshgrid(np.arange(rows, dtype=np.float32), np.arange(64, dtype=np.float32), indexing='ij')
    quarter = D // 4
    omega = (1.0 / (np.float32(10000.0) ** (np.arange(quarter, dtype=np.float32) / np.float32(quarter)))).astype(np.float32)
    ar = rr.reshape(-1)[:, None] * omega
    ac = cc.reshape(-1)[:, None] * omega
    pos = np.concatenate([np.sin(ar), np.cos(ar), np.sin(ac), np.cos(ac)], axis=-1).astype(np.float32)
    return np.ascontiguousarray(pos.T)


def make_in_map(inp, sample_idx, prompt_idxs, sample_len=None):
    f = lambda a: np.ascontiguousarray(np.asarray(a, dtype=np.float32))
    xs = f(inp['x_sample'])[sample_idx]
    if sample_len is not None:
        xs = xs[:sample_len]
    xp = [f(inp['x_prompt'])[i] for i in prompt_idxs]
    m = {}
    m['x_in'] = np.ascontiguousarray(np.concatenate([xs] + xp, axis=0))
    m['c2'] = np.ascontiguousarray(np.stack([f(inp['c_ctx']), f(inp['c'])[sample_idx]], axis=0))
    m['h0'] = np.ascontiguousarray(f(inp['state_ssd'])[sample_idx].reshape(2, 2, H * HP, NS))
    m['posT'] = make_posT(xs.shape[0])
    m['consts'] = make_consts()
    for k in ['w_ada', 'b_ada', 'w_in', 'norm_mix_w', 'norm_ffn_w', 'conv_a_w', 'w_out_a', 'ssd_conv_w', 'ssd_conv_b',
              'ssd_a_log', 'ssd_d', 'ssd_norm_w', 'w_out_b', 'sgu_ln_g', 'sgu_ln_b', 'sgu_w', 'sgu_b', 'w_out_c', 'w_o',
              'exp_w_gate', 'exp_w_up', 'exp_w_down']:
        m[k] = f(inp[k])
    m['ssd_dt_bias'] = f(inp['ssd_dt_bias']).reshape(2, 2 * H)
    m['router_w'] = np.ascontiguousarray(np.concatenate([f(inp['router_g_w']), f(inp['router_e_w'])], axis=-1))
    m['router_b'] = np.ascontiguousarray(np.concatenate([f(inp['router_g_b']), f(inp['router_e_b'])], axis=-1))
    m['final_norm_w'] = f(inp['final_norm_w']).reshape(1, D)
    return m


_NC_CACHE = {}


def kernel(**inputs):
    seqs = [(4096, 0), (256, 1), (256, 1)]
    key = 'full'
    if key not in _NC_CACHE:
        _NC_CACHE[key] = build(dict(seqs=seqs, depth=2, debug=False))
    nc = _NC_CACHE[key]
    base = make_in_map(inputs, 0, [0, 1])
    in_maps = []
    xs_all = np.asarray(inputs['x_sample'], dtype=np.float32)
    xp_all = np.asarray(inputs['x_prompt'], dtype=np.float32)
    c_all = np.asarray(inputs['c'], dtype=np.float32)
    cctx = np.asarray(inputs['c_ctx'], dtype=np.float32)
    st_all = np.asarray(inputs['state_ssd'], dtype=np.float32)
    for core in range(8):
        si = core % 2
        m = dict(base)
        m['x_in'] = np.ascontiguousarray(np.concatenate([xs_all[si], xp_all[2 * core], xp_all[2 * core + 1]], axis=0))
        m['c2'] = np.ascontiguousarray(np.stack([cctx, c_all[si]], axis=0))
        m['h0'] = np.ascontiguousarray(st_all[si].reshape(2, 2, H * HP, NS))
        in_maps.append(m)
    res = run_bass_kernel_spmd(nc, in_maps, core_ids=list(range(8)))
    y_prompt = np.zeros((16, 256, D), np.float32)
    y_sample = np.zeros((2, 4096, D), np.float32)
    new_state = np.zeros((16, 2, 2, H, HP, NS), np.float32)
    for core in range(8):
        r = res.results[core]
        yo = np.asarray(r['y_out'])
        if core < 2:
            y_sample[core] = yo[0:4096]
        y_prompt[2 * core] = yo[4096:4352]
        y_prompt[2 * core + 1] = yo[4352:4608]
        so = np.asarray(r['st_out'])
        new_state[2 * core] = so[0]
        new_state[2 * core + 1] = so[1]
    return (y_prompt, y_sample, new_state)
```

```python
import numpy as np
from contextlib import ExitStack
import concourse.bass as bass
import concourse.mybir as mybir
from concourse.bass_utils import run_bass_kernel_spmd

F32 = mybir.dt.float32
F32R = mybir.dt.float32r
AF = mybir.ActivationFunctionType
ALU = mybir.AluOpType

D = 2048
KD = 16
IN_DIM = 16448
ADA = 6 * D
H = 32
HP = 64
NS = 128
G = 4
NE = 16
DE = 512
EPS = 1e-6
BIG = 30000.0
C_AB, C_AC, C_AH, C_Z, C_XBC, C_DT, C_SU, C_SV, C_GATE = 0, 1024, 2048, 3072, 5120, 8192, 8256, 9280, 10304
ARENA = 24 * 1024
NDS = 96


class Tile:
    def __init__(self, r, f):
        self.r = r
        self.f = f
        self.w = None
        self.rd = {}
        self.dsem = None


class Prog:
    ENG = ['pe', 'act', 'dve', 'pool', 'sp']

    def __init__(self, nc, es, arena, arena_r, psum):
        self.nc = nc
        self.q = {e: [] for e in self.ENG}
        self.csem = {e: es.enter_context(nc.semaphore('c_' + e)) for e in self.ENG}
        self.cnt = {e: 0 for e in self.ENG}
        self.dsems = [es.enter_context(nc.semaphore('d%d' % i)) for i in range(NDS)]
        self.dcnt = [0] * NDS
        self.dfree = list(range(NDS))
        self.seen = {e: {} for e in self.ENG}
        self.arena = arena
        self.arena_r = arena_r
        self.off = 0
        self.off_r = 0
        self.phase_tiles = []
        self.pbanks = [Tile(None, p[:]) for p in psum]
        self.pidx = 0

    def alloc(self, *shape, r=False):
        n = int(np.prod(shape))
        if r:
            assert self.off_r + n <= ARENA, ("arena_r overflow", self.off_r, n)
            r = self.arena_r[:, self.off_r:self.off_r + n]
            f = r.bitcast(F32)
            self.off_r += n
        else:
            assert self.off + n <= ARENA, ("arena overflow", self.off, n)
            f = self.arena[:, self.off:self.off + n]
            r = f
            self.off += n
        if len(shape) == 2:
            r = r.rearrange("p (a b) -> p a b", a=shape[0])
            f = f.rearrange("p (a b) -> p a b", a=shape[0])
        elif len(shape) == 3:
            r = r.rearrange("p (a b c) -> p a b c", a=shape[0], b=shape[1])
            f = f.rearrange("p (a b c) -> p a b c", a=shape[0], b=shape[1])
        t = Tile(r, f)
        self.phase_tiles.append(t)
        return t

    def allocr(self, *shape):
        return self.alloc(*shape, r=True)

    def mark(self):
        return (self.off, len(self.phase_tiles), self.off_r)

    def reset(self, mark):
        self.barrier()
        off, nt, off_r = mark
        for t in self.phase_tiles[nt:]:
            if t.dsem is not None:
                self.dfree.append(t.dsem)
        del self.phase_tiles[nt:]
        self.off = off
        self.off_r = off_r

    def ps(self):
        t = self.pbanks[self.pidx]
        self.pidx = (self.pidx + 1) % len(self.pbanks)
        return t

    def _semobj(self, k):
        return self.csem[k[1]] if k[0] == 'c' else self.dsems[k[1]]

    def _deps(self, eng, reads, writes):
        need = {}

        def add(ev):
            if ev is None:
                return
            k, v = ev
            if k == ('c', 'pe') and eng == 'pe':
                return
            if need.get(k, 0) < v:
                need[k] = v
        for t in reads:
            add(t.w)
        for t in writes:
            add(t.w)
            for k, v in t.rd.items():
                add((k, v))
        waits = []
        for k, v in need.items():
            if self.seen[eng].get(k, 0) >= v:
                continue
            self.seen[eng][k] = v
            waits.append((self._semobj(k), v))
        return waits

    def _commit(self, ev, reads, writes):
        k, v = ev
        for t in reads:
            if t.rd.get(k, 0) < v:
                t.rd[k] = v
        for t in writes:
            t.w = ev
            t.rd = {}

    def op(self, eng, fn, reads=(), writes=()):
        waits = self._deps(eng, reads, writes)
        self.cnt[eng] += 1
        ev = (('c', eng), self.cnt[eng])
        self._commit(ev, reads, writes)
        sem = self.csem[eng]

        def run(e):
            for s, v in waits:
                e.wait_ge(s, v)
            ins = fn(e)
            ins.then_inc(sem, 1)
        self.q[eng].append(run)

    def dma(self, eng, tile, pairs, write, slow=False):
        reads, writes = ((), (tile,)) if write else ((tile,), ())
        waits = self._deps(eng, reads, writes)
        if tile.dsem is None:
            tile.dsem = self.dfree.pop()
        i = tile.dsem
        self.dcnt[i] += 16 * len(pairs)
        ev = (('d', i), self.dcnt[i])
        self._commit(ev, reads, writes)
        sem = self.dsems[i]
        kw = dict(allow_slow_non_contiguous=True) if slow else {}

        def run(e):
            for s, v in waits:
                e.wait_ge(s, v)
            for (o, a) in pairs:
                e.dma_start(out=o, in_=a, **kw).then_inc(sem, 16)
        self.q[eng].append(run)

    def load(self, tile, out_ap, in_ap, eng='sp', slow=False):
        self.dma(eng, tile, [(out_ap, in_ap)], True, slow)

    def store(self, tile, out_ap, in_ap, eng='sp', slow=False):
        self.dma(eng, tile, [(out_ap, in_ap)], False, slow)

    def barrier(self):
        for e in self.ENG:
            waits = []
            for e2 in self.ENG:
                v = self.cnt[e2]
                k = ('c', e2)
                if v and self.seen[e].get(k, 0) < v:
                    self.seen[e][k] = v
                    waits.append((self.csem[e2], v))
            for i in range(NDS):
                v = self.dcnt[i]
                k = ('d', i)
                if v and self.seen[e].get(k, 0) < v:
                    self.seen[e][k] = v
                    waits.append((self.dsems[i], v))
            if waits:
                self.q[e].append(lambda eng, waits=waits: [eng.wait_ge(s, v) for s, v in waits])

    def emit(self, block):
        m = {'pe': block.tensor, 'act': block.scalar, 'dve': block.vector, 'pool': block.gpsimd, 'sp': block.sync}
        for en in self.ENG:
            def f(eng, en=en):
                for run in self.q[en]:
                    run(eng)
            m[en](f)

    def mm(self, pt, out, pairs, reads):
        n = len(pairs)

        def fn(e):
            ins = None
            for i, (l, r) in enumerate(pairs):
                ins = e.matmul(out, l, r, start=(i == 0), stop=(i == n - 1))
            return ins
        self.op('pe', fn, reads, [pt])

    def tr(self, pt, out, in_, ident, reads):
        self.op('pe', lambda e: e.transpose(out, in_, ident), reads, [pt])

    def act(self, out, in_, func, reads, writes, eng='act', **kw):
        self.op('act', lambda e: e.activation(out=out, in_=in_, func=func, **kw), reads, writes)

    def tt(self, eng, out, in0, in1, op, reads, writes):
        self.op(eng, lambda e: e.tensor_tensor(out=out, in0=in0, in1=in1, op=op), reads, writes)

    def ts(self, eng, out, in0, s1, s2, op0, op1, reads, writes):
        if s2 is None:
            self.op(eng, lambda e: e.tensor_scalar(out=out, in0=in0, scalar1=s1, scalar2=None, op0=op0), reads, writes)
        else:
            self.op(eng, lambda e: e.tensor_scalar(out=out, in0=in0, scalar1=s1, scalar2=s2, op0=op0, op1=op1), reads, writes)

    def stt(self, out, in0, scalar, in1, op0, op1, reads, writes):
        self.op('dve', lambda e: e.scalar_tensor_tensor(out=out, in0=in0, scalar=scalar, in1=in1, op0=op0, op1=op1),
                reads, writes)

    def copy(self, eng, out, in_, reads, writes):
        if eng == 'act':
            self.op('act', lambda e: e.activation(out=out, in_=in_, func=AF.Copy), reads, writes)
        else:
            self.op(eng, lambda e: e.tensor_copy(out=out, in_=in_), reads, writes)


def bc(ap, shape):
    return ap.to_broadcast(list(shape))


def build(cfg):
    SEQS = cfg['seqs']
    DEPTH = cfg.get('depth', 2)
    DBG = cfg.get('debug', False)
    NT = sum(l for l, _ in SEQS)
    LS = max([l for l, k in SEQS if k == 0] + [128])
    NPR = sum(1 for _, k in SEQS if k == 1)
    NPROW = NT + 2 * len(SEQS)
    chunks = []
    t0 = 0
    pb = 0
    seqinfo = []
    pri = 0
    for s, (l, k) in enumerate(SEQS):
        nci = l // 128
        first = len(chunks)
        for ci in range(nci):
            chunks.append(dict(s=s, ci=ci, t0=t0 + ci * 128, pr=pb + 1 + ci * 128, kind=k, nci=nci, p0=ci * 128))
        seqinfo.append(dict(first=first, n=nci, kind=k, pb=pb, len=l, t0=t0, pri=(pri if k == 1 else -1)))
        if k == 1:
            pri += 1
        t0 += l
        pb += l + 2
    NCH = len(chunks)

    nc = bass.Bass("TRN2", target_bir_lowering=False)

    def din(name, shape, dt=F32):
        return nc.dram_tensor(name, list(shape), dt, kind="ExternalInput").ap()

    def dscr(name, shape):
        return nc.dram_tensor(name, list(shape), F32, kind=("ExternalOutput" if DBG else "Internal")).ap()

    x_in = din("x_in", [NT, D])
    c2 = din("c2", [2, D])
    h0 = din("h0", [2, 2, H * HP, NS])
    posT = din("posT", [D, LS])
    consts = din("consts", [128, 6, 128])
    w_ada = din("w_ada", [2, D, ADA], F32R)
    b_ada = din("b_ada", [2, ADA])
    w_in = din("w_in", [2, D, IN_DIM], F32R)
    norm_mix_w = din("norm_mix_w", [2, D])
    norm_ffn_w = din("norm_ffn_w", [2, D])
    conv_a_w = din("conv_a_w", [2, 3, 1024])
    w_out_a = din("w_out_a", [2, 1024, D], F32R)
    ssd_conv_w = din("ssd_conv_w", [2, 3, 3072])
    ssd_conv_b = din("ssd_conv_b", [2, 3072])
    ssd_a_log = din("ssd_a_log", [2, 2, H])
    ssd_dt_bias = din("ssd_dt_bias", [2, 2 * H])
    ssd_d = din("ssd_d", [2, H])
    ssd_norm_w = din("ssd_norm_w", [2, D])
    w_out_b = din("w_out_b", [2, D, D], F32R)
    sgu_ln_g = din("sgu_ln_g", [2, 1024])
    sgu_ln_b = din("sgu_ln_b", [2, 1024])
    sgu_w = din("sgu_w", [2, 8, 128, 128])
    sgu_b = din("sgu_b", [2, 128, 8])
    w_out_c = din("w_out_c", [2, 1024, D], F32R)
    w_o = din("w_o", [2, D, D], F32R)
    router_w = din("router_w", [2, D, 20])
    router_b = din("router_b", [2, 20])
    exp_w_gate = din("exp_w_gate", [2, NE, D, DE], F32R)
    exp_w_up = din("exp_w_up", [2, NE, D, DE], F32R)
    exp_w_down = din("exp_w_down", [2, NE, DE, D], F32R)
    final_norm_w = din("final_norm_w", [1, D])

    y_out = nc.dram_tensor("y_out", [NT, D], F32, kind="ExternalOutput").ap()
    st_out = nc.dram_tensor("st_out", [max(NPR, 1), 2, 2, H, HP, NS], F32, kind="ExternalOutput").ap()

    PROJ_A = dscr("PROJ", [NPROW, 8192])
    PROJ_B = dscr("PROJB", [NPROW, IN_DIM - 8192])

    class _Proj:
        def __getitem__(self, key):
            rs, cs = key
            if cs.start >= 8192:
                return PROJ_B[rs, cs.start - 8192:cs.stop - 8192]
            assert cs.stop <= 8192
            return PROJ_A[rs, cs]
    PROJ = _Proj()
    XBC = dscr("XBC", [NT, 3072])
    DT = dscr("DT", [NT, 64])
    U = dscr("U", [NT, 4096])
    YP = dscr("YP", [NT, D])
    XR = dscr("XR", [NT, D])
    MOD = dscr("MOD", [2, ADA])
    XM = dscr("XM", [NT, D])
    COMB = dscr("COMB", [NT, 16])

    es = ExitStack()
    with es:
        arena_t = es.enter_context(nc.sbuf_tensor("arena", [128, ARENA], F32))
        arena_r = es.enter_context(nc.sbuf_tensor("arena_r", [128, ARENA], F32R))
        psum = [es.enter_context(nc.psum_tensor("pb%d" % i, [128, 512], F32)) for i in range(8)]
        block = es.enter_context(nc.Block())
        P = Prog(nc, es, arena_t, arena_r, psum)

        cst = P.alloc(6, 128)
        P.load(cst, cst.f, consts)
        ident, Umat, UTmat, ones, maskF, maskB = [cst.f[:, i, :] for i in range(6)]
        modT = P.alloc(96, 2)
        AB = P.alloc(4, KD, 2)
        nwT = P.alloc(2, KD)
        mk = P.mark()
        zrow = P.alloc(2048)
        P.op('pool', lambda e: e.memset(zrow.f, 0.0), [], [zrow])
        for si in seqinfo:
            for rr in (si['pb'], si['pb'] + si['len'] + 1):
                P.store(zrow, PROJ_A[rr:rr + 1, :].rearrange("o (a b) -> (o a) b", a=4), zrow.f[0:4, :])
        P.reset(mk)

        def wtile_load(t, dst, src):
            P.load(t, dst, src, eng='pool')

        def make_hT(xt, which, ch, hT, col0, tmp, posB=None, hT32=None):
            r = 1 if ch['kind'] == 0 else 0
            Aap = AB.f[:, 2 * which, :, r]
            Bap = AB.f[:, 2 * which + 1, :, r]
            ss = tmp['ss']
            junk = tmp['junk']
            P.op('act', lambda e: e.activation(out=junk.f, in_=xt.f, func=AF.Square, accum_out=ss.f[:, 0:1]),
                 [xt], [junk, ss])
            P.ts('dve', ss.f[:, 1:2], ss.f[:, 0:1], 1.0 / D, EPS, ALU.mult, ALU.add, [ss], [ss])
            P.act(ss.f[:, 2:3], ss.f[:, 1:2], AF.Sqrt, [ss], [ss])
            P.op('dve', lambda e: e.reciprocal(out=ss.f[:, 3:4], in_=ss.f[:, 2:3]), [ss], [ss])
            P.op('act', lambda e: e.activation(out=junk.f, in_=xt.f, func=AF.Copy, scale=ss.f[:, 3:4]), [xt, ss], [junk])
            for q4 in range(4):
                pt = P.ps()
                for q in range(4):
                    k = q4 * 4 + q
                    P.tr(pt, pt.f[:, q * 128:(q + 1) * 128], junk.f[:, k * 128:(k + 1) * 128], ident, [junk, cst])
                for q in range(4):
                    k = q4 * 4 + q
                    in1 = posB.f[:, k, :] if posB is not None else bc(Bap[:, k:k + 1], [128, 128])
                    rds = [pt, AB] + ([posB] if posB is not None else [])
                    P.stt(hT.r[:, k, col0:col0 + 128], pt.f[:, q * 128:(q + 1) * 128], Aap[:, k:k + 1], in1,
                          ALU.mult, ALU.add, rds, [hT])
                    if hT32 is not None:
                        P.stt(hT32.f[:, k, :], pt.f[:, q * 128:(q + 1) * 128], Aap[:, k:k + 1], in1,
                              ALU.mult, ALU.add, rds, [hT32])

        for l in range(DEPTH):
            xsrc = x_in if l == 0 else XR
            mk = P.mark()
            sc = P.alloc(D)
            scT = P.allocr(KD, 2)
            modrow = P.alloc(ADA)
            badats = [P.alloc(512) for _ in range(2)]
            wr = [P.allocr(KD, 512) for _ in range(2)]
            P.load(sc, sc.f[0:2, :], c2)
            P.act(sc.f[0:2, :], sc.f[0:2, :], AF.Silu, [sc], [sc])
            pt = P.ps()
            for k in range(KD):
                P.tr(pt, pt.f[:, 2 * k:2 * k + 2], sc.f[0:2, k * 128:(k + 1) * 128], ident[0:2, 0:2], [sc, cst])
            P.copy('dve', scT.r, pt.f[:, 0:32].rearrange("p (k r) -> p k r", r=2), [pt], [scT])
            for nt in range(ADA // 512):
                w = wr[nt % 2]
                wtile_load(w, w.r, w_ada[l, :, nt * 512:(nt + 1) * 512].rearrange("(k p) n -> p k n", p=128))
                badat = badats[nt % 2]
                P.dma('sp', badat, [(badat.f[0:1, :], b_ada[l:l + 1, nt * 512:(nt + 1) * 512]),
                                    (badat.f[1:2, :], b_ada[l:l + 1, nt * 512:(nt + 1) * 512])], True)
                pt = P.ps()
                P.mm(pt, pt.f[0:2, :], [(scT.r[:, k, :], w.r[:, k, :]) for k in range(KD)], [scT, w])
                P.tt('dve', modrow.f[0:2, nt * 512:(nt + 1) * 512], pt.f[0:2, :], badat.f[0:2, :],
                     ALU.add, [pt, badat], [modrow])
            P.store(modrow, MOD, modrow.f[0:2, :])
            pt = P.ps()
            for c in range(96):
                P.tr(pt, pt.f[:, 2 * c:2 * c + 2], modrow.f[0:2, c * 128:(c + 1) * 128], ident[0:2, 0:2], [modrow, cst])
            P.copy('dve', modT.f, pt.f[:, 0:192].rearrange("p (c r) -> p c r", r=2), [pt], [modT])
            P.load(nwT, nwT.f[:, 0, :], norm_mix_w[l].rearrange("(k p) -> p k", p=128), slow=True)
            P.load(nwT, nwT.f[:, 1, :], norm_ffn_w[l].rearrange("(k p) -> p k", p=128), slow=True)
            for which, (isc, ish) in enumerate(((1, 0), (4, 3))):
                for r in range(2):
                    P.stt(AB.f[:, 2 * which, :, r], modT.f[:, isc * 16:(isc + 1) * 16, r], 1.0, nwT.f[:, which, :],
                          ALU.add, ALU.mult, [modT, nwT], [AB])
                    P.copy('dve', AB.f[:, 2 * which + 1, :, r], modT.f[:, ish * 16:(ish + 1) * 16, r], [modT], [AB])
            P.reset(mk)

            mk = P.mark()
            TS = 4
            hT = P.allocr(KD, TS * 128)
            xts = [P.alloc(D) for _ in range(2)]
            tmp = dict(ss=P.alloc(8), junk=P.alloc(D))
            posb = P.alloc(KD, 128)
            wts = [P.allocr(KD, 512) for _ in range(2)]
            outs = [P.alloc(512) for _ in range(4)]
            oi = 0
            wi = 0
            for g0 in range(0, NCH, TS):
                grp = chunks[g0:g0 + TS]
                for j, ch in enumerate(grp):
                    xt = xts[j % 2]
                    P.load(xt, xt.f, xsrc[ch['t0']:ch['t0'] + 128, :])
                    pB = None
                    if ch['kind'] == 0:
                        P.load(posb, posb.f, posT[:, ch['p0']:ch['p0'] + 128].rearrange("(k p) t -> p k t", p=128))
                        P.tt('pool', posb.f, posb.f, bc(AB.f[:, 1, :, 1:2], [128, KD, 128]), ALU.add, [posb, AB], [posb])
                        pB = posb
                    make_hT(xt, 0, ch, hT, j * 128, tmp, posB=pB)
                ntile = (IN_DIM + 511) // 512
                for nt in range(ntile):
                    ncol = min(512, IN_DIM - nt * 512)
                    w = wts[wi % 2]
                    wi += 1
                    wtile_load(w, w.r[:, :, 0:ncol], w_in[l, :, nt * 512:nt * 512 + ncol].rearrange("(k p) n -> p k n", p=128))
                    for j, ch in enumerate(grp):
                        pt = P.ps()
                        P.mm(pt, pt.f[:, 0:ncol], [(hT.r[:, k, j * 128:(j + 1) * 128], w.r[:, k, 0:ncol]) for k in range(KD)],
                             [hT, w])
                        o = outs[oi % 4]
                        oi += 1
                        P.copy('act' if oi % 2 else 'dve', o.f[:, 0:ncol], pt.f[:, 0:ncol], [pt], [o])
                        P.store(o, PROJ[ch['pr']:ch['pr'] + 128, nt * 512:nt * 512 + ncol], o.f[:, 0:ncol])
            P.reset(mk)
            if cfg.get('stop') == 'proj':
                break

            mk = P.mark()
            cw = P.alloc(3, 3072)
            cb = P.alloc(3072)
            dtb = P.alloc(64)
            dtts = [P.alloc(64) for _ in range(2)]
            tins = [[P.alloc(1536) for _ in range(3)] for _ in range(2)]
            P.load(cw, cw.f, ssd_conv_w[l].partition_broadcast(128))
            P.load(cb, cb.f, ssd_conv_b[l].partition_broadcast(128))
            P.load(dtb, dtb.f, ssd_dt_bias[l].partition_broadcast(128))
            ri = 0
            for cidx, ch in enumerate(chunks):
                pr, t0 = ch['pr'], ch['t0']
                for hf in range(2):
                    tin = tins[ri % 2]
                    ri += 1
                    cs = slice(hf * 1536, (hf + 1) * 1536)
                    for s3 in range(3):
                        P.load(tin[s3], tin[s3].f, PROJ[pr - 1 + s3:pr - 1 + s3 + 128, C_XBC + hf * 1536:C_XBC + (hf + 1) * 1536])
                    P.tt('pool', tin[0].f, tin[0].f, cw.f[:, 0, cs], ALU.mult, [tin[0], cw], [tin[0]])
                    P.tt('dve', tin[1].f, tin[1].f, cw.f[:, 1, cs], ALU.mult, [tin[1], cw], [tin[1]])
                    P.tt('pool', tin[2].f, tin[2].f, cw.f[:, 2, cs], ALU.mult, [tin[2], cw], [tin[2]])
                    P.tt('dve', tin[1].f, tin[1].f, cb.f[:, cs], ALU.add, [tin[1], cb], [tin[1]])
                    P.tt('dve', tin[0].f, tin[0].f, tin[1].f, ALU.add, [tin[0], tin[1]], [tin[0]])
                    P.tt('dve', tin[0].f, tin[0].f, tin[2].f, ALU.add, [tin[0], tin[2]], [tin[0]])
                    P.act(tin[1].f, tin[0].f, AF.Silu, [tin[0]], [tin[1]])
                    P.store(tin[1], XBC[t0:t0 + 128, cs], tin[1].f)
                dtt = dtts[cidx % 2]
                P.load(dtt, dtt.f, PROJ[pr:pr + 128, C_DT:C_DT + 64])
                P.tt('dve', dtt.f, dtt.f, dtb.f, ALU.add, [dtt, dtb], [dtt])
                P.act(dtt.f, dtt.f, AF.Exp, [dtt], [dtt])
                P.ts('dve', dtt.f, dtt.f, 1.0, None, ALU.add, None, [dtt], [dtt])
                P.act(dtt.f, dtt.f, AF.Ln, [dtt], [dtt])
                P.store(dtt, DT[t0:t0 + 128, :], dtt.f)
            P.reset(mk)

            mk = P.mark()
            caw = P.alloc(3, 1024)
            tabs = [P.alloc(1024) for _ in range(2)]
            tchs = [[P.alloc(2048) for _ in range(3)] for _ in range(2)]
            P.load(caw, caw.f, conv_a_w[l].partition_broadcast(128))
            for cidx, ch in enumerate(chunks):
                pr, t0 = ch['pr'], ch['t0']
                tab = tabs[cidx % 2]
                tch = tchs[cidx % 2]
                P.load(tab, tab.f, PROJ[pr:pr + 128, C_AB:C_AB + 1024])
                for s3 in range(3):
                    t = tch[s3]
                    P.load(t, t.f, PROJ[pr - 1 + s3:pr - 1 + s3 + 128, C_AC:C_AC + 2048])
                    e1 = 'pool' if s3 != 1 else 'dve'
                    P.tt(e1, t.f[:, 0:1024], t.f[:, 0:1024], t.f[:, 1024:2048], ALU.mult, [t], [t])
                    P.tt(e1, t.f[:, 0:1024], t.f[:, 0:1024], caw.f[:, s3, :], ALU.mult, [t, caw], [t])
                a0 = tch[0]
                P.tt('dve', a0.f[:, 0:1024], a0.f[:, 0:1024], tch[1].f[:, 0:1024], ALU.add, [a0, tch[1]], [a0])
                P.tt('dve', a0.f[:, 0:1024], a0.f[:, 0:1024], tch[2].f[:, 0:1024], ALU.add, [a0, tch[2]], [a0])
                P.tt('dve', a0.f[:, 0:1024], a0.f[:, 0:1024], tab.f, ALU.mult, [a0, tab], [a0])
                P.store(a0, U[t0:t0 + 128, 0:1024], a0.f[:, 0:1024])
            P.reset(mk)

            mk = P.mark()
            lng = P.alloc(1024)
            lnb = P.alloc(1024)
            wT = P.alloc(8, 128)
            wtmp = P.alloc(8, 128)
            sgb = P.alloc(8)
            c3sets = [(P.alloc(2048), P.alloc(2048), P.alloc(1024), P.alloc(8), P.alloc(1024)) for _ in range(2)]
            P.load(lng, lng.f, sgu_ln_g[l].partition_broadcast(128))
            P.load(lnb, lnb.f, sgu_ln_b[l].partition_broadcast(128))
            P.load(sgb, sgb.f, sgu_b[l])
            P.load(wtmp, wtmp.f, sgu_w[l].rearrange("g i j -> i g j"))
            for hf in range(2):
                pt = P.ps()
                for q in range(4):
                    P.tr(pt, pt.f[:, q * 128:(q + 1) * 128], wtmp.f[:, hf * 4 + q, :], ident, [wtmp, cst])
                P.copy('dve', wT.f[:, hf * 4:(hf + 1) * 4, :], pt.f.rearrange("p (a b) -> p a b", a=4), [pt], [wT])
            for cidx, ch in enumerate(chunks):
                pr, t0 = ch['pr'], ch['t0']
                suv, gt_, junkc3, stc3, uc = c3sets[cidx % 2]
                P.load(suv, suv.f, PROJ[pr:pr + 128, C_SU:C_SU + 2048])
                P.tt('pool', gt_.f, suv.f, suv.f, ALU.mult, [suv], [gt_])
                P.ts('dve', gt_.f, gt_.f, 0.044715, 1.0, ALU.mult, ALU.add, [gt_], [gt_])
                P.tt('dve', gt_.f, gt_.f, suv.f, ALU.mult, [gt_, suv], [gt_])
                P.act(gt_.f, gt_.f, AF.Sigmoid, [gt_], [gt_], scale=1.5957691216057308)
                P.tt('dve', suv.f, suv.f, gt_.f, ALU.mult, [suv, gt_], [suv])
                u_ = suv.f[:, 0:1024]
                v_ = suv.f[:, 1024:2048]
                P.op('dve', lambda e, v_=v_, stc3=stc3: e.tensor_reduce(out=stc3.f[:, 0:1], in_=v_, axis=mybir.AxisListType.X, op=ALU.add),
                     [suv], [stc3])
                P.ts('dve', stc3.f[:, 1:2], stc3.f[:, 0:1], -1.0 / 1024, None, ALU.mult, None, [stc3], [stc3])
                P.ts('dve', v_, v_, stc3.f[:, 1:2], None, ALU.add, None, [suv, stc3], [suv])
                P.op('act', lambda e, v_=v_, stc3=stc3, junkc3=junkc3: e.activation(out=junkc3.f, in_=v_, func=AF.Square, accum_out=stc3.f[:, 2:3]),
                     [suv], [junkc3, stc3])
                P.ts('dve', stc3.f[:, 3:4], stc3.f[:, 2:3], 1.0 / 1024, EPS, ALU.mult, ALU.add, [stc3], [stc3])
                P.act(stc3.f[:, 4:5], stc3.f[:, 3:4], AF.Sqrt, [stc3], [stc3])
                P.op('dve', lambda e, stc3=stc3: e.reciprocal(out=stc3.f[:, 5:6], in_=stc3.f[:, 4:5]), [stc3], [stc3])
                P.stt(v_, v_, stc3.f[:, 5:6], lng.f, ALU.mult, ALU.mult, [suv, stc3, lng], [suv])
                P.tt('dve', v_, v_, lnb.f, ALU.add, [suv, lnb], [suv])
                for hf in range(2):
                    pt = P.ps()
                    for q in range(4):
                        g = hf * 4 + q
                        P.mm(pt, pt.f[:, q * 128:(q + 1) * 128], [(wT.f[:, g, :], suv.f[:, 1024 + g * 128:1024 + (g + 1) * 128])],
                             [wT, suv])
                    for q in range(4):
                        g = hf * 4 + q
                        P.stt(uc.f[:, g * 128:(g + 1) * 128], pt.f[:, q * 128:(q + 1) * 128], sgb.f[:, g:g + 1],
                              suv.f[:, g * 128:(g + 1) * 128], ALU.add, ALU.mult, [pt, sgb, suv], [uc])
                P.store(uc, U[t0:t0 + 128, 3072:4096], uc.f)
            P.reset(mk)
            if cfg.get('stop') == 'c3':
                break

            mk = P.mark()
            S = P.alloc(2048)
            xbc = P.alloc(3072)
            zt = P.alloc(2048)
            ypt = P.alloc(2048)
            xw = P.alloc(2048)
            yt = P.alloc(2048)
            nwbc = P.alloc(2048)
            Rs = [P.alloc(4, 128) for _ in range(2)]
            Qs = [P.alloc(4, 128) for _ in range(2)]
            BT = P.alloc(4, 128)
            CT = P.alloc(4, 128)
            ST = P.alloc(4, 128)
            Lts = [P.alloc(4, 128) for _ in range(2)]
            WTs = [P.alloc(4, 128) for _ in range(2)]
            abc = P.alloc(2, 32)
            Dbc = P.alloc(32)
            dts = P.alloc(32)
            da = P.alloc(32)
            PT = P.alloc(64)
            lnw = P.alloc(32)
            biasL = P.alloc(32)
            eP = P.alloc(32)
            etot = P.alloc(32)
            wdec = P.alloc(32)
            st4 = P.alloc(16)
            P.load(abc, abc.f, ssd_a_log[l].partition_broadcast(128))
            P.act(abc.f, abc.f, AF.Exp, [abc], [abc])
            P.ts('dve', abc.f, abc.f, -1.0, None, ALU.mult, None, [abc], [abc])
            P.load(Dbc, Dbc.f, ssd_d[l].partition_broadcast(128))
            P.load(nwbc, nwbc.f, ssd_norm_w[l].partition_broadcast(128))
            li = 0
            for dr in range(2):
                cum = Umat if dr == 0 else UTmat
                msk = maskF if dr == 0 else maskB
                for si in seqinfo:
                    if si['kind'] == 0:
                        P.load(xw, xw.f.rearrange("p (k n) -> p k n", k=16), h0[l, dr].rearrange("(k p) n -> p k n", p=128))
                        for q4 in range(4):
                            pt = P.ps()
                            for q in range(4):
                                k = q4 * 4 + q
                                P.tr(pt, pt.f[:, q * 128:(q + 1) * 128], xw.f[:, k * 128:(k + 1) * 128], ident, [xw, cst])
                            P.copy('dve', S.f[:, q4 * 512:(q4 + 1) * 512], pt.f, [pt], [S])
                    else:
                        P.op('pool', lambda e: e.memset(S.f, 0.0), [], [S])
                    order = list(range(si['n'])) if dr == 0 else list(range(si['n'] - 1, -1, -1))
                    for ci in order:
                        ch = chunks[si['first'] + ci]
                        pr, t0 = ch['pr'], ch['t0']
                        P.load(xbc, xbc.f, XBC[t0:t0 + 128, :])
                        P.load(dts, dts.f, DT[t0:t0 + 128, dr * 32:(dr + 1) * 32])
                        if dr == 1:
                            P.load(zt, zt.f, PROJ[pr:pr + 128, C_Z:C_Z + 2048])
                            P.load(ypt, ypt.f, YP[t0:t0 + 128, :])
                        P.tt('dve', da.f, dts.f, abc.f[:, dr, :], ALU.mult, [dts, abc], [da])
                        pt0 = P.ps()
                        P.mm(pt0, pt0.f[:, 0:32], [(cum, da.f)], [cst, da])
                        P.mm(pt0, pt0.f[:, 32:64], [(ones, da.f)], [cst, da])
                        P.copy('dve', PT.f, pt0.f[:, 0:64], [pt0], [PT])
                        P.act(lnw.f, dts.f, AF.Ln, [dts], [lnw])
                        P.tt('dve', biasL.f, lnw.f, PT.f[:, 0:32], ALU.subtract, [lnw, PT], [biasL])
                        P.act(eP.f, PT.f[:, 0:32], AF.Exp, [PT], [eP])
                        P.act(etot.f, PT.f[:, 32:64], AF.Exp, [PT], [etot])
                        P.tt('dve', wdec.f, biasL.f, PT.f[:, 32:64], ALU.add, [biasL, PT], [wdec])
                        P.act(wdec.f, wdec.f, AF.Exp, [wdec], [wdec])
                        ptb = P.ps()
                        ptc = P.ps()
                        for g in range(4):
                            P.tr(ptb, ptb.f[:, g * 128:(g + 1) * 128], xbc.f[:, 2048 + g * 128:2048 + (g + 1) * 128], ident, [xbc, cst])
                            P.tr(ptc, ptc.f[:, g * 128:(g + 1) * 128], xbc.f[:, 2560 + g * 128:2560 + (g + 1) * 128], ident, [xbc, cst])
                        P.copy('act', BT.f, ptb.f.rearrange("p (a b) -> p a b", a=4), [ptb], [BT])
                        P.copy('dve', CT.f, ptc.f.rearrange("p (a b) -> p a b", a=4), [ptc], [CT])
                        pts = P.ps()
                        for g in range(4):
                            P.mm(pts, pts.f[:, g * 128:(g + 1) * 128], [(BT.f[:, g, :], CT.f[:, g, :])], [BT, CT])
                        P.copy('act', ST.f, pts.f.rearrange("p (a b) -> p a b", a=4), [pts], [ST])
                        for g in range(4):
                            pty = P.ps()
                            pto = P.ps()
                            P.mm(pto, pto.f, [(CT.f[:, g, :], S.f[:, g * 512:(g + 1) * 512])], [CT, S])
                            for hh in range(2):
                                hb = g * 8 + hh * 4
                                R_, Q_, Lt, WT = Rs[li % 2], Qs[li % 2], Lts[li % 2], WTs[li % 2]
                                li += 1
                                P.tt('pool', R_.f, bc(cum.unsqueeze(1), [128, 4, 128]), bc(da.f[:, hb:hb + 4].unsqueeze(2), [128, 4, 128]),
                                     ALU.mult, [cst, da], [R_])
                                P.tt('pool', Q_.f, bc(msk.unsqueeze(1), [128, 4, 128]), bc(biasL.f[:, hb:hb + 4].unsqueeze(2), [128, 4, 128]),
                                     ALU.add, [cst, biasL], [Q_])
                                ptl = P.ps()
                                P.mm(ptl, ptl.f, [(ones, R_.f.rearrange("p a b -> p (a b)")), (ident, Q_.f.rearrange("p a b -> p (a b)"))],
                                     [cst, R_, Q_])
                                P.act(Lt.f, ptl.f.rearrange("p (a b) -> p a b", a=4), AF.Exp, [ptl], [Lt])
                                P.tt('dve', WT.f, Lt.f, bc(ST.f[:, g:g + 1, :], [128, 4, 128]), ALU.mult, [Lt, ST], [WT])
                                for h4 in range(4):
                                    hd = hb + h4
                                    c0 = (hh * 4 + h4) * 64
                                    P.mm(pty, pty.f[:, c0:c0 + 64], [(WT.f[:, h4, :], xbc.f[:, hd * 64:(hd + 1) * 64])], [WT, xbc])
                            yg = yt.f[:, g * 512:(g + 1) * 512]
                            P.tt('dve', yg.rearrange("p (a b) -> p a b", a=8), pto.f.rearrange("p (a b) -> p a b", a=8),
                                 bc(eP.f[:, g * 8:(g + 1) * 8].unsqueeze(2), [128, 8, 64]), ALU.mult, [pto, eP], [yt])
                            P.tt('dve', yg, yg, pty.f, ALU.add, [yt, pty], [yt])
                        P.tt('pool', xw.f.rearrange("p (a b) -> p a b", a=32), xbc.f[:, 0:2048].rearrange("p (a b) -> p a b", a=32),
                             bc(wdec.f.unsqueeze(2), [128, 32, 64]), ALU.mult, [xbc, wdec], [xw])
                        for g in range(4):
                            ptg = P.ps()
                            P.mm(ptg, ptg.f, [(xbc.f[:, 2048 + g * 128:2048 + (g + 1) * 128], xw.f[:, g * 512:(g + 1) * 512])], [xbc, xw])
                            Sg = S.f[:, g * 512:(g + 1) * 512]
                            P.tt('dve', Sg.rearrange("p (a b) -> p a b", a=8), Sg.rearrange("p (a b) -> p a b", a=8),
                                 bc(etot.f[:, g * 8:(g + 1) * 8].unsqueeze(2), [128, 8, 64]), ALU.mult, [S, etot], [S])
                            P.tt('dve', Sg, Sg, ptg.f, ALU.add, [S, ptg], [S])
                        if dr == 0:
                            P.store(yt, YP[t0:t0 + 128, :], yt.f)
                        else:
                            P.tt('dve', yt.f, yt.f, ypt.f, ALU.add, [yt, ypt], [yt])
                            P.tt('pool', ypt.f.rearrange("p (a b) -> p a b", a=32), xbc.f[:, 0:2048].rearrange("p (a b) -> p a b", a=32),
                                 bc(Dbc.f.unsqueeze(2), [128, 32, 64]), ALU.mult, [xbc, Dbc], [ypt])
                            P.tt('dve', yt.f, yt.f, ypt.f, ALU.add, [yt, ypt], [yt])
                            P.act(zt.f, zt.f, AF.Silu, [zt], [zt])
                            P.tt('dve', yt.f, yt.f, zt.f, ALU.mult, [yt, zt], [yt])
                            for g in range(4):
                                P.op('act', lambda e, g=g: e.activation(out=zt.f[:, g * 512:(g + 1) * 512], in_=yt.f[:, g * 512:(g + 1) * 512],
                                                                       func=AF.Square, accum_out=st4.f[:, g:g + 1]), [yt], [zt, st4])
                            P.ts('dve', st4.f[:, 4:8], st4.f[:, 0:4], 1.0 / 512, EPS, ALU.mult, ALU.add, [st4], [st4])
                            P.act(st4.f[:, 8:12], st4.f[:, 4:8], AF.Sqrt, [st4], [st4])
                            P.op('dve', lambda e: e.reciprocal(out=st4.f[:, 12:16], in_=st4.f[:, 8:12]), [st4], [st4])
                            for g in range(4):
                                P.stt(yt.f[:, g * 512:(g + 1) * 512], yt.f[:, g * 512:(g + 1) * 512], st4.f[:, 12 + g:13 + g],
                                      nwbc.f[:, g * 512:(g + 1) * 512], ALU.mult, ALU.mult, [yt, st4, nwbc], [yt])
                            P.store(yt, U[t0:t0 + 128, 1024:3072], yt.f)
                    if si['kind'] == 1:
                        for half in range(2):
                            for q in range(4):
                                pt = P.ps()
                                for h4 in range(4):
                                    hd = half * 16 + q * 4 + h4
                                    P.tr(pt, pt.f[0:64, h4 * 128:(h4 + 1) * 128], S.f[:, hd * 64:(hd + 1) * 64], ident, [S, cst])
                                P.copy('dve', xw.f[0:64, q * 512:(q + 1) * 512], pt.f[0:64, :], [pt], [xw])
                            P.store(xw, st_out[si['pri'], l, dr, half * 16:(half + 1) * 16].rearrange("h p n -> p h n"),
                                    xw.f[0:64, :].rearrange("p (h n) -> p h n", h=16))
            P.reset(mk)
            if cfg.get('stop') == 'ssd':
                break

            mk = P.mark()
            TDN = 4
            UT = P.allocr(32, TDN * 128)
            mT = UT
            wds = [P.allocr(KD, 256) for _ in range(2)]
            utile = P.alloc(4096)
            merged = [P.alloc(2048) for _ in range(TDN)]
            xch = [P.alloc(2048) for _ in range(TDN)]
            gts = [P.alloc(256) for _ in range(2)]
            g1s = [P.alloc(256) for _ in range(2)]
            wi = 0
            gi_ = 0
            ci_ = 0
            for g0 in range(0, NCH, TDN):
                grp = chunks[g0:g0 + TDN]
                for j, ch in enumerate(grp):
                    P.load(utile, utile.f, U[ch['t0']:ch['t0'] + 128, :])
                    P.load(xch[j], xch[j].f, xsrc[ch['t0']:ch['t0'] + 128, :])
                    for q4 in range(8):
                        pt = P.ps()
                        for q in range(4):
                            k = q4 * 4 + q
                            P.tr(pt, pt.f[:, q * 128:(q + 1) * 128], utile.f[:, k * 128:(k + 1) * 128], ident, [utile, cst])
                        ci_ += 1
                        P.copy('act' if ci_ % 2 else 'dve', UT.r[:, q4 * 4:(q4 + 1) * 4, j * 128:(j + 1) * 128],
                               pt.f.rearrange("p (a b) -> p a b", a=4), [pt], [UT])
                for bi, (wsrc, k0, KC, gcol) in enumerate(((w_out_a, 0, 8, 0), (w_out_b, 8, 16, 2048), (w_out_c, 24, 8, 4096))):
                    for nt in range(8):
                        w = wds[wi % 2]
                        wi += 1
                        wtile_load(w, w.r[:, 0:KC, :], wsrc[l, :, nt * 256:(nt + 1) * 256].rearrange("(k p) n -> p k n", p=128))
                        for j, ch in enumerate(grp):
                            pt = P.ps()
                            P.mm(pt, pt.f[:, 0:256], [(UT.r[:, k0 + k, j * 128:(j + 1) * 128], w.r[:, k, :]) for k in range(KC)], [UT, w])
                            gt = gts[gi_ % 2]
                            gi_ += 1
                            c0 = C_GATE + gcol + nt * 256
                            P.load(gt, gt.f, PROJ[ch['pr']:ch['pr'] + 128, c0:c0 + 256])
                            P.act(gt.f, gt.f, AF.Sigmoid, [gt], [gt])
                            mg = merged[j].f[:, nt * 256:(nt + 1) * 256]
                            if bi == 0:
                                P.tt('dve', mg, gt.f, pt.f[:, 0:256], ALU.mult, [gt, pt], [merged[j]])
                            else:
                                P.tt('dve', gt.f, gt.f, pt.f[:, 0:256], ALU.mult, [gt, pt], [gt])
                                P.tt('dve', mg, mg, gt.f, ALU.add, [merged[j], gt], [merged[j]])
                for j, ch in enumerate(grp):
                    for q4 in range(4):
                        pt = P.ps()
                        for q in range(4):
                            k = q4 * 4 + q
                            P.tr(pt, pt.f[:, q * 128:(q + 1) * 128], merged[j].f[:, k * 128:(k + 1) * 128], ident, [merged[j], cst])
                        ci_ += 1
                        P.copy('act' if ci_ % 2 else 'dve', mT.r[:, q4 * 4:(q4 + 1) * 4, j * 128:(j + 1) * 128],
                               pt.f.rearrange("p (a b) -> p a b", a=4), [pt], [mT])
                for nt in range(8):
                    w = wds[wi % 2]
                    wi += 1
                    wtile_load(w, w.r, w_o[l, :, nt * 256:(nt + 1) * 256].rearrange("(k p) n -> p k n", p=128))
                    for j, ch in enumerate(grp):
                        r = 1 if ch['kind'] == 0 else 0
                        pt = P.ps()
                        P.mm(pt, pt.f[:, 0:256], [(mT.r[:, k, j * 128:(j + 1) * 128], w.r[:, k, :]) for k in range(KD)], [mT, w])
                        g1 = g1s[gi_ % 2]
                        gi_ += 1
                        P.load(g1, g1.f, MOD[r, 2 * D + nt * 256:2 * D + (nt + 1) * 256].partition_broadcast(128))
                        P.tt('dve', g1.f, g1.f, pt.f[:, 0:256], ALU.mult, [g1, pt], [g1])
                        xs_ = xch[j].f[:, nt * 256:(nt + 1) * 256]
                        P.tt('dve', xs_, xs_, g1.f, ALU.add, [xch[j], g1], [xch[j]])
                for j, ch in enumerate(grp):
                    P.store(xch[j], XM[ch['t0']:ch['t0'] + 128, :], xch[j].f)
            P.reset(mk)
            if cfg.get('stop') == 'mix':
                break

            mk = P.mark()
            TE = 4
            last = (l == DEPTH - 1)
            h2T = P.allocr(KD, TE * 128)
            actT = P.allocr(4, TE * 128)
            wgu = [[P.allocr(KD, 128) for _ in range(2)] for _ in range(2)]
            wdn = [P.allocr(4, 512) for _ in range(2)]
            xe = [P.alloc(2048) for _ in range(TE)]
            acc = [P.alloc(2048) for _ in range(TE)]
            tmp = dict(ss=P.alloc(8), junk=P.alloc(D))
            h32 = P.alloc(KD, 128)
            sgs = [P.alloc(TE * 128) for _ in range(2)]
            rw = P.alloc(KD, 20)
            rbb = P.alloc(20)
            lg = P.alloc(20)
            rs = P.alloc(64)
            combs = [P.alloc(16) for _ in range(TE)]
            g2s = [P.alloc(512) for _ in range(2)]
            P.load(rw, rw.f, router_w[l].rearrange("(k p) n -> p k n", p=128))
            P.load(rbb, rbb.f, router_b[l].partition_broadcast(128))
            ui = 0
            di = 0
            si_ = 0
            g2i = 0
            for g0 in range(0, NCH, TE):
                grp = chunks[g0:g0 + TE]
                nj = len(grp)
                NTK = nj * 128
                for j, ch in enumerate(grp):
                    P.load(xe[j], xe[j].f, XM[ch['t0']:ch['t0'] + 128, :])
                    make_hT(xe[j], 1, ch, h2T, j * 128, tmp, posB=None, hT32=h32)
                    pt = P.ps()
                    P.mm(pt, pt.f[:, 0:20], [(h32.f[:, k, :], rw.f[:, k, :]) for k in range(KD)], [h32, rw])
                    P.tt('dve', lg.f, pt.f[:, 0:20], rbb.f, ALU.add, [pt, rbb], [lg])
                    R_ = rs.f
                    cb_ = combs[j]
                    P.op('dve', lambda e, R_=R_: e.tensor_reduce(out=R_[:, 0:1], in_=lg.f[:, 0:4], axis=mybir.AxisListType.X, op=ALU.max), [lg], [rs])
                    P.ts('dve', R_[:, 1:2], R_[:, 0:1], -1.0, None, ALU.mult, None, [rs], [rs])
                    P.op('act', lambda e, R_=R_: e.activation(out=R_[:, 4:8], in_=lg.f[:, 0:4], func=AF.Exp, bias=R_[:, 1:2], scale=1.0,
                                                            accum_out=R_[:, 2:3]), [lg, rs], [rs])
                    P.op('dve', lambda e, R_=R_: e.reciprocal(out=R_[:, 3:4], in_=R_[:, 2:3]), [rs], [rs])
                    P.ts('dve', R_[:, 8:12], lg.f[:, 0:4], R_[:, 0:1], None, ALU.is_equal, None, [lg, rs], [rs])
                    P.tt('dve', R_[:, 16:32].rearrange("p (g k) -> p g k", g=4), lg.f[:, 4:20].rearrange("p (g k) -> p g k", g=4),
                         bc(R_[:, 8:12].unsqueeze(2), [128, 4, 4]), ALU.mult, [lg, rs], [rs])
                    P.op('dve', lambda e, R_=R_: e.tensor_reduce(out=R_[:, 12:16], in_=R_[:, 16:32].rearrange("p (g k) -> p k g", g=4),
                                                               axis=mybir.AxisListType.X, op=ALU.add), [rs], [rs])
                    P.op('dve', lambda e, R_=R_: e.tensor_reduce(out=R_[:, 32:33], in_=R_[:, 12:16], axis=mybir.AxisListType.X, op=ALU.max), [rs], [rs])
                    P.ts('dve', R_[:, 33:34], R_[:, 32:33], -1.0, None, ALU.mult, None, [rs], [rs])
                    P.op('act', lambda e, R_=R_: e.activation(out=R_[:, 36:40], in_=R_[:, 12:16], func=AF.Exp, bias=R_[:, 33:34], scale=1.0), [rs], [rs])
                    P.ts('dve', R_[:, 40:44], R_[:, 12:16], R_[:, 32:33], None, ALU.is_equal, None, [rs], [rs])
                    P.stt(R_[:, 44:48], R_[:, 40:44], -2.0, R_[:, 36:40], ALU.mult, ALU.add, [rs], [rs])
                    P.op('dve', lambda e, R_=R_: e.tensor_reduce(out=R_[:, 34:35], in_=R_[:, 44:48], axis=mybir.AxisListType.X, op=ALU.max), [rs], [rs])
                    P.ts('dve', R_[:, 48:52], R_[:, 44:48], R_[:, 34:35], None, ALU.is_equal, None, [rs], [rs])
                    P.ts('dve', R_[:, 35:36], R_[:, 34:35], 1.0, None, ALU.add, None, [rs], [rs])
                    P.op('dve', lambda e, R_=R_: e.reciprocal(out=R_[:, 52:53], in_=R_[:, 35:36]), [rs], [rs])
                    P.tt('dve', R_[:, 53:54], R_[:, 52:53], R_[:, 3:4], ALU.mult, [rs], [rs])
                    P.tt('dve', R_[:, 54:55], R_[:, 53:54], R_[:, 34:35], ALU.mult, [rs], [rs])
                    P.ts('dve', R_[:, 56:60], R_[:, 40:44], R_[:, 53:54], None, ALU.mult, None, [rs], [rs])
                    P.stt(R_[:, 56:60], R_[:, 48:52], R_[:, 54:55], R_[:, 56:60], ALU.mult, ALU.add, [rs], [rs])
                    P.tt('dve', cb_.f.rearrange("p (g k) -> p g k", g=4), bc(R_[:, 8:12].unsqueeze(2), [128, 4, 4]),
                         bc(R_[:, 56:60].unsqueeze(1), [128, 4, 4]), ALU.mult, [rs], [cb_])
                    if DBG:
                        P.store(cb_, COMB[ch['t0']:ch['t0'] + 128, :], cb_.f)
                for ex in range(NE):
                    for fc in range(4):
                        wg, wu = wgu[ui % 2]
                        ui += 1
                        wtile_load(wg, wg.r, exp_w_gate[l, ex, :, fc * 128:(fc + 1) * 128].rearrange("(k p) f -> p k f", p=128))
                        wtile_load(wu, wu.r, exp_w_up[l, ex, :, fc * 128:(fc + 1) * 128].rearrange("(k p) f -> p k f", p=128))
                        ptg = P.ps()
                        P.mm(ptg, ptg.f[:, 0:NTK], [(wg.r[:, k, :], h2T.r[:, k, 0:NTK]) for k in range(KD)], [wg, h2T])
                        ptu = P.ps()
                        P.mm(ptu, ptu.f[:, 0:NTK], [(wu.r[:, k, :], h2T.r[:, k, 0:NTK]) for k in range(KD)], [wu, h2T])
                        sg = sgs[si_ % 2]
                        si_ += 1
                        P.act(sg.f[:, 0:NTK], ptg.f[:, 0:NTK], AF.Silu, [ptg], [sg])
                        P.tt('dve', actT.r[:, fc, 0:NTK], sg.f[:, 0:NTK], ptu.f[:, 0:NTK], ALU.mult, [sg, ptu], [actT])
                    for dq in range(4):
                        wd = wdn[di % 2]
                        di += 1
                        wtile_load(wd, wd.r, exp_w_down[l, ex, :, dq * 512:(dq + 1) * 512].rearrange("(c p) n -> p c n", p=128))
                        for j in range(nj):
                            pt = P.ps()
                            P.mm(pt, pt.f, [(actT.r[:, fc, j * 128:(j + 1) * 128], wd.r[:, fc, :]) for fc in range(4)], [actT, wd])
                            dst = acc[j].f[:, dq * 512:(dq + 1) * 512]
                            if ex == 0:
                                P.ts('dve', dst, pt.f, combs[j].f[:, ex:ex + 1], None, ALU.mult, None, [pt, combs[j]], [acc[j]])
                            else:
                                P.stt(dst, pt.f, combs[j].f[:, ex:ex + 1], dst, ALU.mult, ALU.add, [pt, combs[j], acc[j]], [acc[j]])
                for j, ch in enumerate(grp):
                    r = 1 if ch['kind'] == 0 else 0
                    for d4 in range(4):
                        g2 = g2s[g2i % 2]
                        g2i += 1
                        P.load(g2, g2.f, MOD[r, 5 * D + d4 * 512:5 * D + (d4 + 1) * 512].partition_broadcast(128))
                        sl = slice(d4 * 512, (d4 + 1) * 512)
                        P.tt('dve', g2.f, g2.f, acc[j].f[:, sl], ALU.mult, [g2, acc[j]], [g2])
                        P.tt('dve', xe[j].f[:, sl], xe[j].f[:, sl], g2.f, ALU.add, [xe[j], g2], [xe[j]])
                    if not last:
                        P.store(xe[j], XR[ch['t0']:ch['t0'] + 128, :], xe[j].f)
                    else:
                        if DBG:
                            P.store(xe[j], XR[ch['t0']:ch['t0'] + 128, :], xe[j].f)
                        ss = tmp['ss']
                        junk = tmp['junk']
                        P.op('act', lambda e, j=j: e.activation(out=junk.f, in_=xe[j].f, func=AF.Square, accum_out=ss.f[:, 0:1]),
                             [xe[j]], [junk, ss])
                        P.ts('dve', ss.f[:, 1:2], ss.f[:, 0:1], 1.0 / D, EPS, ALU.mult, ALU.add, [ss], [ss])
                        P.act(ss.f[:, 2:3], ss.f[:, 1:2], AF.Sqrt, [ss], [ss])
                        P.op('dve', lambda e: e.reciprocal(out=ss.f[:, 3:4], in_=ss.f[:, 2:3]), [ss], [ss])
                        for d4 in range(4):
                            g2 = g2s[g2i % 2]
                            g2i += 1
                            sl = slice(d4 * 512, (d4 + 1) * 512)
                            P.load(g2, g2.f, final_norm_w[0, sl].partition_broadcast(128))
                            P.stt(junk.f[:, sl], xe[j].f[:, sl], ss.f[:, 3:4], g2.f, ALU.mult, ALU.mult, [xe[j], ss, g2], [junk])
                        P.store(junk, y_out[ch['t0']:ch['t0'] + 128, :], junk.f)
            P.reset(mk)

        P.barrier()
        P.emit(block)
    return nc


def make_consts():
    k = np.arange(128)[:, None]
    i = np.arange(128)[None, :]
    c = np.zeros((128, 6, 128), np.float32)
    c[:, 0] = (k == i)
    c[:, 1] = (k <= i)
    c[:, 2] = (k >= i)
    c[:, 3] = 1.0
    c[:, 4] = np.where(k > i, -BIG, 0.0)
    c[:, 5] = np.where(k < i, -BIG, 0.0)
    return c


def make_posT(n_tok):
    rows = n_tok // 64
    rr, cc = np.meshgrid(np.arange(rows, dtype=np.float32), np.arange(64, dtype=np.float32), indexing='ij')
    quarter = D // 4
    omega = (1.0 / (np.float32(10000.0) ** (np.arange(quarter, dtype=np.float32) / np.float32(quarter)))).astype(np.float32)
    ar = rr.reshape(-1)[:, None] * omega
    ac = cc.reshape(-1)[:, None] * omega
    pos = np.concatenate([np.sin(ar), np.cos(ar), np.sin(ac), np.cos(ac)], axis=-1).astype(np.float32)
    return np.ascontiguousarray(pos.T)


def make_in_map(inp, sample_idx, prompt_idxs, sample_len=None):
    f = lambda a: np.ascontiguousarray(np.asarray(a, dtype=np.float32))
    xs = f(inp['x_sample'])[sample_idx]
    if sample_len is not None:
        xs = xs[:sample_len]
    xp = [f(inp['x_prompt'])[i] for i in prompt_idxs]
    m = {}
    m['x_in'] = np.ascontiguousarray(np.concatenate([xs] + xp, axis=0))
    m['c2'] = np.ascontiguousarray(np.stack([f(inp['c_ctx']), f(inp['c'])[sample_idx]], axis=0))
    m['h0'] = np.ascontiguousarray(f(inp['state_ssd'])[sample_idx].reshape(2, 2, H * HP, NS))
    m['posT'] = make_posT(xs.shape[0])
    m['consts'] = make_consts()
    for k in ['w_ada', 'b_ada', 'w_in', 'norm_mix_w', 'norm_ffn_w', 'conv_a_w', 'w_out_a', 'ssd_conv_w', 'ssd_conv_b',
              'ssd_a_log', 'ssd_d', 'ssd_norm_w', 'w_out_b', 'sgu_ln_g', 'sgu_ln_b', 'sgu_w', 'sgu_b', 'w_out_c', 'w_o',
              'exp_w_gate', 'exp_w_up', 'exp_w_down']:
        m[k] = f(inp[k])
    m['ssd_dt_bias'] = f(inp['ssd_dt_bias']).reshape(2, 2 * H)
    m['router_w'] = np.ascontiguousarray(np.concatenate([f(inp['router_g_w']), f(inp['router_e_w'])], axis=-1))
    m['router_b'] = np.ascontiguousarray(np.concatenate([f(inp['router_g_b']), f(inp['router_e_b'])], axis=-1))
    m['final_norm_w'] = f(inp['final_norm_w']).reshape(1, D)
    return m


_NC_CACHE = {}


def kernel(**inputs):
    seqs = [(4096, 0), (256, 1), (256, 1)]
    key = 'full'
    if key not in _NC_CACHE:
        _NC_CACHE[key] = build(dict(seqs=seqs, depth=2, debug=False))
    nc = _NC_CACHE[key]
    base = make_in_map(inputs, 0, [0, 1])
    in_maps = []
    xs_all = np.asarray(inputs['x_sample'], dtype=np.float32)
    xp_all = np.asarray(inputs['x_prompt'], dtype=np.float32)
    c_all = np.asarray(inputs['c'], dtype=np.float32)
    cctx = np.asarray(inputs['c_ctx'], dtype=np.float32)
    st_all = np.asarray(inputs['state_ssd'], dtype=np.float32)
    for core in range(8):
        si = core % 2
        m = dict(base)
        m['x_in'] = np.ascontiguousarray(np.concatenate([xs_all[si], xp_all[2 * core], xp_all[2 * core + 1]], axis=0))
        m['c2'] = np.ascontiguousarray(np.stack([cctx, c_all[si]], axis=0))
        m['h0'] = np.ascontiguousarray(st_all[si].reshape(2, 2, H * HP, NS))
        in_maps.append(m)
    res = run_bass_kernel_spmd(nc, in_maps, core_ids=list(range(8)))
    y_prompt = np.zeros((16, 256, D), np.float32)
    y_sample = np.zeros((2, 4096, D), np.float32)
    new_state = np.zeros((16, 2, 2, H, HP, NS), np.float32)
    for core in range(8):
        r = res.results[core]
        yo = np.asarray(r['y_out'])
        if core < 2:
            y_sample[core] = yo[0:4096]
        y_prompt[2 * core] = yo[4096:4352]
        y_prompt[2 * core + 1] = yo[4352:4608]
        so = np.asarray(r['st_out'])
        new_state[2 * core] = so[0]
        new_state[2 * core + 1] = so[1]
    return (y_prompt, y_sample, new_state)
```

```python
import numpy as np
from contextlib import ExitStack
import concourse.bass as bass
import concourse.mybir as mybir
from concourse.bass_utils import run_bass_kernel_spmd

F32 = mybir.dt.float32
F32R = mybir.dt.float32r
AF = mybir.ActivationFunctionType
ALU = mybir.AluOpType

D = 2048
KD = 16
IN_DIM = 16448
ADA = 6 * D
H = 32
HP = 64
NS = 128
G = 4
NE = 16
DE = 512
EPS = 1e-6
BIG = 30000.0
C_AB, C_AC, C_AH, C_Z, C_XBC, C_DT, C_SU, C_SV, C_GATE = 0, 1024, 2048, 3072, 5120, 8192, 8256, 9280, 10304
ARENA = 24 * 1024
NDS = 96


class Tile:
    def __init__(self, r, f):
        self.r = r
        self.f = f
        self.w = None
        self.rd = {}
        self.dsem = None


class Prog:
    ENG = ['pe', 'act', 'dve', 'pool', 'sp']

    def __init__(self, nc, es, arena, arena_r, psum):
        self.nc = nc
        self.q = {e: [] for e in self.ENG}
        self.csem = {e: es.enter_context(nc.semaphore('c_' + e)) for e in self.ENG}
        self.cnt = {e: 0 for e in self.ENG}
        self.dsems = [es.enter_context(nc.semaphore('d%d' % i)) for i in range(NDS)]
        self.dcnt = [0] * NDS
        self.dfree = list(range(NDS))
        self.seen = {e: {} for e in self.ENG}
        self.arena = arena
        self.arena_r = arena_r
        self.off = 0
        self.off_r = 0
        self.phase_tiles = []
        self.pbanks = [Tile(None, p[:]) for p in psum]
        self.pidx = 0

    def alloc(self, *shape, r=False):
        n = int(np.prod(shape))
        if r:
            assert self.off_r + n <= ARENA, ("arena_r overflow", self.off_r, n)
            r = self.arena_r[:, self.off_r:self.off_r + n]
            f = r.bitcast(F32)
            self.off_r += n
        else:
            assert self.off + n <= ARENA, ("arena overflow", self.off, n)
            f = self.arena[:, self.off:self.off + n]
            r = f
            self.off += n
        if len(shape) == 2:
            r = r.rearrange("p (a b) -> p a b", a=shape[0])
            f = f.rearrange("p (a b) -> p a b", a=shape[0])
        elif len(shape) == 3:
            r = r.rearrange("p (a b c) -> p a b c", a=shape[0], b=shape[1])
            f = f.rearrange("p (a b c) -> p a b c", a=shape[0], b=shape[1])
        t = Tile(r, f)
        self.phase_tiles.append(t)
        return t

    def allocr(self, *shape):
        return self.alloc(*shape, r=True)

    def mark(self):
        return (self.off, len(self.phase_tiles), self.off_r)

    def reset(self, mark):
        self.barrier()
        off, nt, off_r = mark
        for t in self.phase_tiles[nt:]:
            if t.dsem is not None:
                self.dfree.append(t.dsem)
        del self.phase_tiles[nt:]
        self.off = off
        self.off_r = off_r

    def ps(self):
        t = self.pbanks[self.pidx]
        self.pidx = (self.pidx + 1) % len(self.pbanks)
        return t

    def _semobj(self, k):
        return self.csem[k[1]] if k[0] == 'c' else self.dsems[k[1]]

    def _deps(self, eng, reads, writes):
        need = {}

        def add(ev):
            if ev is None:
                return
            k, v = ev
            if k == ('c', 'pe') and eng == 'pe':
                return
            if need.get(k, 0) < v:
                need[k] = v
        for t in reads:
            add(t.w)
        for t in writes:
            add(t.w)
            for k, v in t.rd.items():
                add((k, v))
        waits = []
        for k, v in need.items():
            if self.seen[eng].get(k, 0) >= v:
                continue
            self.seen[eng][k] = v
            waits.append((self._semobj(k), v))
        return waits

    def _commit(self, ev, reads, writes):
        k, v = ev
        for t in reads:
            if t.rd.get(k, 0) < v:
                t.rd[k] = v
        for t in writes:
            t.w = ev
            t.rd = {}

    def op(self, eng, fn, reads=(), writes=()):
        waits = self._deps(eng, reads, writes)
        self.cnt[eng] += 1
        ev = (('c', eng), self.cnt[eng])
        self._commit(ev, reads, writes)
        sem = self.csem[eng]

        def run(e):
            for s, v in waits:
                e.wait_ge(s, v)
            ins = fn(e)
            ins.then_inc(sem, 1)
        self.q[eng].append(run)

    def dma(self, eng, tile, pairs, write, slow=False):
        reads, writes = ((), (tile,)) if write else ((tile,), ())
        waits = self._deps(eng, reads, writes)
        if tile.dsem is None:
            tile.dsem = self.dfree.pop()
        i = tile.dsem
        self.dcnt[i] += 16 * len(pairs)
        ev = (('d', i), self.dcnt[i])
        self._commit(ev, reads, writes)
        sem = self.dsems[i]
        kw = dict(allow_slow_non_contiguous=True) if slow else {}

        def run(e):
            for s, v in waits:
                e.wait_ge(s, v)
            for (o, a) in pairs:
                e.dma_start(out=o, in_=a, **kw).then_inc(sem, 16)
        self.q[eng].append(run)

    def load(self, tile, out_ap, in_ap, eng='sp', slow=False):
        self.dma(eng, tile, [(out_ap, in_ap)], True, slow)

    def store(self, tile, out_ap, in_ap, eng='sp', slow=False):
        self.dma(eng, tile, [(out_ap, in_ap)], False, slow)

    def barrier(self):
        for e in self.ENG:
            waits = []
            for e2 in self.ENG:
                v = self.cnt[e2]
                k = ('c', e2)
                if v and self.seen[e].get(k, 0) < v:
                    self.seen[e][k] = v
                    waits.append((self.csem[e2], v))
            for i in range(NDS):
                v = self.dcnt[i]
                k = ('d', i)
                if v and self.seen[e].get(k, 0) < v:
                    self.seen[e][k] = v
                    waits.append((self.dsems[i], v))
            if waits:
                self.q[e].append(lambda eng, waits=waits: [eng.wait_ge(s, v) for s, v in waits])

    def emit(self, block):
        m = {'pe': block.tensor, 'act': block.scalar, 'dve': block.vector, 'pool': block.gpsimd, 'sp': block.sync}
        for en in self.ENG:
            def f(eng, en=en):
                for run in self.q[en]:
                    run(eng)
            m[en](f)

    def mm(self, pt, out, pairs, reads):
        n = len(pairs)

        def fn(e):
            ins = None
            for i, (l, r) in enumerate(pairs):
                ins = e.matmul(out, l, r, start=(i == 0), stop=(i == n - 1))
            return ins
        self.op('pe', fn, reads, [pt])

    def tr(self, pt, out, in_, ident, reads):
        self.op('pe', lambda e: e.transpose(out, in_, ident), reads, [pt])

    def act(self, out, in_, func, reads, writes, eng='act', **kw):
        self.op('act', lambda e: e.activation(out=out, in_=in_, func=func, **kw), reads, writes)

    def tt(self, eng, out, in0, in1, op, reads, writes):
        self.op(eng, lambda e: e.tensor_tensor(out=out, in0=in0, in1=in1, op=op), reads, writes)

    def ts(self, eng, out, in0, s1, s2, op0, op1, reads, writes):
        if s2 is None:
            self.op(eng, lambda e: e.tensor_scalar(out=out, in0=in0, scalar1=s1, scalar2=None, op0=op0), reads, writes)
        else:
            self.op(eng, lambda e: e.tensor_scalar(out=out, in0=in0, scalar1=s1, scalar2=s2, op0=op0, op1=op1), reads, writes)

    def stt(self, out, in0, scalar, in1, op0, op1, reads, writes):
        self.op('dve', lambda e: e.scalar_tensor_tensor(out=out, in0=in0, scalar=scalar, in1=in1, op0=op0, op1=op1),
                reads, writes)

    def copy(self, eng, out, in_, reads, writes):
        if eng == 'act':
            self.op('act', lambda e: e.activation(out=out, in_=in_, func=AF.Copy), reads, writes)
        else:
            self.op(eng, lambda e: e.tensor_copy(out=out, in_=in_), reads, writes)


def bc(ap, shape):
    return ap.to_broadcast(list(shape))


def build(cfg):
    SEQS = cfg['seqs']
    DEPTH = cfg.get('depth', 2)
    DBG = cfg.get('debug', False)
    NT = sum(l for l, _ in SEQS)
    LS = max([l for l, k in SEQS if k == 0] + [128])
    NPR = sum(1 for _, k in SEQS if k == 1)
    NPROW = NT + 2 * len(SEQS)
    chunks = []
    t0 = 0
    pb = 0
    seqinfo = []
    pri = 0
    for s, (l, k) in enumerate(SEQS):
        nci = l // 128
        first = len(chunks)
        for ci in range(nci):
            chunks.append(dict(s=s, ci=ci, t0=t0 + ci * 128, pr=pb + 1 + ci * 128, kind=k, nci=nci, p0=ci * 128))
        seqinfo.append(dict(first=first, n=nci, kind=k, pb=pb, len=l, t0=t0, pri=(pri if k == 1 else -1)))
        if k == 1:
            pri += 1
        t0 += l
        pb += l + 2
    NCH = len(chunks)

    nc = bass.Bass("TRN2", target_bir_lowering=False)

    def din(name, shape, dt=F32):
        return nc.dram_tensor(name, list(shape), dt, kind="ExternalInput").ap()

    def dscr(name, shape):
        return nc.dram_tensor(name, list(shape), F32, kind=("ExternalOutput" if DBG else "Internal")).ap()

    x_in = din("x_in", [NT, D])
    c2 = din("c2", [2, D])
    h0 = din("h0", [2, 2, H * HP, NS])
    posT = din("posT", [D, LS])
    consts = din("consts", [128, 6, 128])
    w_ada = din("w_ada", [2, D, ADA], F32R)
    b_ada = din("b_ada", [2, ADA])
    w_in = din("w_in", [2, D, IN_DIM], F32R)
    norm_mix_w = din("norm_mix_w", [2, D])
    norm_ffn_w = din("norm_ffn_w", [2, D])
    conv_a_w = din("conv_a_w", [2, 3, 1024])
    w_out_a = din("w_out_a", [2, 1024, D], F32R)
    ssd_conv_w = din("ssd_conv_w", [2, 3, 3072])
    ssd_conv_b = din("ssd_conv_b", [2, 3072])
    ssd_a_log = din("ssd_a_log", [2, 2, H])
    ssd_dt_bias = din("ssd_dt_bias", [2, 2 * H])
    ssd_d = din("ssd_d", [2, H])
    ssd_norm_w = din("ssd_norm_w", [2, D])
    w_out_b = din("w_out_b", [2, D, D], F32R)
    sgu_ln_g = din("sgu_ln_g", [2, 1024])
    sgu_ln_b = din("sgu_ln_b", [2, 1024])
    sgu_w = din("sgu_w", [2, 8, 128, 128])
    sgu_b = din("sgu_b", [2, 128, 8])
    w_out_c = din("w_out_c", [2, 1024, D], F32R)
    w_o = din("w_o", [2, D, D], F32R)
    router_w = din("router_w", [2, D, 20])
    router_b = din("router_b", [2, 20])
    exp_w_gate = din("exp_w_gate", [2, NE, D, DE], F32R)
    exp_w_up = din("exp_w_up", [2, NE, D, DE], F32R)
    exp_w_down = din("exp_w_down", [2, NE, DE, D], F32R)
    final_norm_w = din("final_norm_w", [1, D])

    y_out = nc.dram_tensor("y_out", [NT, D], F32, kind="ExternalOutput").ap()
    st_out = nc.dram_tensor("st_out", [max(NPR, 1), 2, 2, H, HP, NS], F32, kind="ExternalOutput").ap()

    PROJ_A = dscr("PROJ", [NPROW, 8192])
    PROJ_B = dscr("PROJB", [NPROW, IN_DIM - 8192])

    class _Proj:
        def __getitem__(self, key):
            rs, cs = key
            if cs.start >= 8192:
                return PROJ_B[rs, cs.start - 8192:cs.stop - 8192]
            assert cs.stop <= 8192
            return PROJ_A[rs, cs]
    PROJ = _Proj()
    XBC = dscr("XBC", [NT, 3072])
    DT = dscr("DT", [NT, 64])
    U = dscr("U", [NT, 4096])
    YP = dscr("YP", [NT, D])
    XR = dscr("XR", [NT, D])
    MOD = dscr("MOD", [2, ADA])
    XM = dscr("XM", [NT, D])
    COMB = dscr("COMB", [NT, 16])

    es = ExitStack()
    with es:
        arena_t = es.enter_context(nc.sbuf_tensor("arena", [128, ARENA], F32))
        arena_r = es.enter_context(nc.sbuf_tensor("arena_r", [128, ARENA], F32R))
        psum = [es.enter_context(nc.psum_tensor("pb%d" % i, [128, 512], F32)) for i in range(8)]
        block = es.enter_context(nc.Block())
        P = Prog(nc, es, arena_t, arena_r, psum)

        cst = P.alloc(6, 128)
        P.load(cst, cst.f, consts)
        ident, Umat, UTmat, ones, maskF, maskB = [cst.f[:, i, :] for i in range(6)]
        modT = P.alloc(96, 2)
        AB = P.alloc(4, KD, 2)
        nwT = P.alloc(2, KD)
        mk = P.mark()
        zrow = P.alloc(2048)
        P.op('pool', lambda e: e.memset(zrow.f, 0.0), [], [zrow])
        for si in seqinfo:
            for rr in (si['pb'], si['pb'] + si['len'] + 1):
                P.store(zrow, PROJ_A[rr:rr + 1, :].rearrange("o (a b) -> (o a) b", a=4), zrow.f[0:4, :])
        P.reset(mk)

        def wtile_load(t, dst, src):
            P.load(t, dst, src, eng='pool')

        def make_hT(xt, which, ch, hT, col0, tmp, posB=None, hT32=None):
            r = 1 if ch['kind'] == 0 else 0
            Aap = AB.f[:, 2 * which, :, r]
            Bap = AB.f[:, 2 * which + 1, :, r]
            ss = tmp['ss']
            junk = tmp['junk']
            P.op('act', lambda e: e.activation(out=junk.f, in_=xt.f, func=AF.Square, accum_out=ss.f[:, 0:1]),
                 [xt], [junk, ss])
            P.ts('dve', ss.f[:, 1:2], ss.f[:, 0:1], 1.0 / D, EPS, ALU.mult, ALU.add, [ss], [ss])
            P.act(ss.f[:, 2:3], ss.f[:, 1:2], AF.Sqrt, [ss], [ss])
            P.op('dve', lambda e: e.reciprocal(out=ss.f[:, 3:4], in_=ss.f[:, 2:3]), [ss], [ss])
            P.op('act', lambda e: e.activation(out=junk.f, in_=xt.f, func=AF.Copy, scale=ss.f[:, 3:4]), [xt, ss], [junk])
            for q4 in range(4):
                pt = P.ps()
                for q in range(4):
                    k = q4 * 4 + q
                    P.tr(pt, pt.f[:, q * 128:(q + 1) * 128], junk.f[:, k * 128:(k + 1) * 128], ident, [junk, cst])
                for q in range(4):
                    k = q4 * 4 + q
                    in1 = posB.f[:, k, :] if posB is not None else bc(Bap[:, k:k + 1], [128, 128])
                    rds = [pt, AB] + ([posB] if posB is not None else [])
                    P.stt(hT.r[:, k, col0:col0 + 128], pt.f[:, q * 128:(q + 1) * 128], Aap[:, k:k + 1], in1,
                          ALU.mult, ALU.add, rds, [hT])
                    if hT32 is not None:
                        P.stt(hT32.f[:, k, :], pt.f[:, q * 128:(q + 1) * 128], Aap[:, k:k + 1], in1,
                              ALU.mult, ALU.add, rds, [hT32])

        for l in range(DEPTH):
            xsrc = x_in if l == 0 else XR
            mk = P.mark()
            sc = P.alloc(D)
            scT = P.allocr(KD, 2)
            modrow = P.alloc(ADA)
            badats = [P.alloc(512) for _ in range(2)]
            wr = [P.allocr(KD, 512) for _ in range(2)]
            P.load(sc, sc.f[0:2, :], c2)
            P.act(sc.f[0:2, :], sc.f[0:2, :], AF.Silu, [sc], [sc])
            pt = P.ps()
            for k in range(KD):
                P.tr(pt, pt.f[:, 2 * k:2 * k + 2], sc.f[0:2, k * 128:(k + 1) * 128], ident[0:2, 0:2], [sc, cst])
            P.copy('dve', scT.r, pt.f[:, 0:32].rearrange("p (k r) -> p k r", r=2), [pt], [scT])
            for nt in range(ADA // 512):
                w = wr[nt % 2]
                wtile_load(w, w.r, w_ada[l, :, nt * 512:(nt + 1) * 512].rearrange("(k p) n -> p k n", p=128))
                badat = badats[nt % 2]
                P.dma('sp', badat, [(badat.f[0:1, :], b_ada[l:l + 1, nt * 512:(nt + 1) * 512]),
                                    (badat.f[1:2, :], b_ada[l:l + 1, nt * 512:(nt + 1) * 512])], True)
                pt = P.ps()
                P.mm(pt, pt.f[0:2, :], [(scT.r[:, k, :], w.r[:, k, :]) for k in range(KD)], [scT, w])
                P.tt('dve', modrow.f[0:2, nt * 512:(nt + 1) * 512], pt.f[0:2, :], badat.f[0:2, :],
                     ALU.add, [pt, badat], [modrow])
            P.store(modrow, MOD, modrow.f[0:2, :])
            pt = P.ps()
            for c in range(96):
                P.tr(pt, pt.f[:, 2 * c:2 * c + 2], modrow.f[0:2, c * 128:(c + 1) * 128], ident[0:2, 0:2], [modrow, cst])
            P.copy('dve', modT.f, pt.f[:, 0:192].rearrange("p (c r) -> p c r", r=2), [pt], [modT])
            P.load(nwT, nwT.f[:, 0, :], norm_mix_w[l].rearrange("(k p) -> p k", p=128), slow=True)
            P.load(nwT, nwT.f[:, 1, :], norm_ffn_w[l].rearrange("(k p) -> p k", p=128), slow=True)
            for which, (isc, ish) in enumerate(((1, 0), (4, 3))):
                for r in range(2):
                    P.stt(AB.f[:, 2 * which, :, r], modT.f[:, isc * 16:(isc + 1) * 16, r], 1.0, nwT.f[:, which, :],
                          ALU.add, ALU.mult, [modT, nwT], [AB])
                    P.copy('dve', AB.f[:, 2 * which + 1, :, r], modT.f[:, ish * 16:(ish + 1) * 16, r], [modT], [AB])
            P.reset(mk)

            mk = P.mark()
            TS = 4
            hT = P.allocr(KD, TS * 128)
            xts = [P.alloc(D) for _ in range(2)]
            tmp = dict(ss=P.alloc(8), junk=P.alloc(D))
            posb = P.alloc(KD, 128)
            wts = [P.allocr(KD, 512) for _ in range(2)]
            outs = [P.alloc(512) for _ in range(4)]
            oi = 0
            wi = 0
            for g0 in range(0, NCH, TS):
                grp = chunks[g0:g0 + TS]
                for j, ch in enumerate(grp):
                    xt = xts[j % 2]
                    P.load(xt, xt.f, xsrc[ch['t0']:ch['t0'] + 128, :])
                    pB = None
                    if ch['kind'] == 0:
                        P.load(posb, posb.f, posT[:, ch['p0']:ch['p0'] + 128].rearrange("(k p) t -> p k t", p=128))
                        P.tt('pool', posb.f, posb.f, bc(AB.f[:, 1, :, 1:2], [128, KD, 128]), ALU.add, [posb, AB], [posb])
                        pB = posb
                    make_hT(xt, 0, ch, hT, j * 128, tmp, posB=pB)
                ntile = (IN_DIM + 511) // 512
                for nt in range(ntile):
                    ncol = min(512, IN_DIM - nt * 512)
                    w = wts[wi % 2]
                    wi += 1
                    wtile_load(w, w.r[:, :, 0:ncol], w_in[l, :, nt * 512:nt * 512 + ncol].rearrange("(k p) n -> p k n", p=128))
                    for j, ch in enumerate(grp):
                        pt = P.ps()
                        P.mm(pt, pt.f[:, 0:ncol], [(hT.r[:, k, j * 128:(j + 1) * 128], w.r[:, k, 0:ncol]) for k in range(KD)],
                             [hT, w])
                        o = outs[oi % 4]
                        oi += 1
                        P.copy('act' if oi % 2 else 'dve', o.f[:, 0:ncol], pt.f[:, 0:ncol], [pt], [o])
                        P.store(o, PROJ[ch['pr']:ch['pr'] + 128, nt * 512:nt * 512 + ncol], o.f[:, 0:ncol])
            P.reset(mk)
            if cfg.get('stop') == 'proj':
                break

            mk = P.mark()
            cw = P.alloc(3, 3072)
            cb = P.alloc(3072)
            dtb = P.alloc(64)
            dtts = [P.alloc(64) for _ in range(2)]
            tins = [[P.alloc(1536) for _ in range(3)] for _ in range(2)]
            P.load(cw, cw.f, ssd_conv_w[l].partition_broadcast(128))
            P.load(cb, cb.f, ssd_conv_b[l].partition_broadcast(128))
            P.load(dtb, dtb.f, ssd_dt_bias[l].partition_broadcast(128))
            ri = 0
            for cidx, ch in enumerate(chunks):
                pr, t0 = ch['pr'], ch['t0']
                for hf in range(2):
                    tin = tins[ri % 2]
                    ri += 1
                    cs = slice(hf * 1536, (hf + 1) * 1536)
                    for s3 in range(3):
                        P.load(tin[s3], tin[s3].f, PROJ[pr - 1 + s3:pr - 1 + s3 + 128, C_XBC + hf * 1536:C_XBC + (hf + 1) * 1536])
                    P.tt('pool', tin[0].f, tin[0].f, cw.f[:, 0, cs], ALU.mult, [tin[0], cw], [tin[0]])
                    P.tt('dve', tin[1].f, tin[1].f, cw.f[:, 1, cs], ALU.mult, [tin[1], cw], [tin[1]])
                    P.tt('pool', tin[2].f, tin[2].f, cw.f[:, 2, cs], ALU.mult, [tin[2], cw], [tin[2]])
                    P.tt('dve', tin[1].f, tin[1].f, cb.f[:, cs], ALU.add, [tin[1], cb], [tin[1]])
                    P.tt('dve', tin[0].f, tin[0].f, tin[1].f, ALU.add, [tin[0], tin[1]], [tin[0]])
                    P.tt('dve', tin[0].f, tin[0].f, tin[2].f, ALU.add, [tin[0], tin[2]], [tin[0]])
                    P.act(tin[1].f, tin[0].f, AF.Silu, [tin[0]], [tin[1]])
                    P.store(tin[1], XBC[t0:t0 + 128, cs], tin[1].f)
                dtt = dtts[cidx % 2]
                P.load(dtt, dtt.f, PROJ[pr:pr + 128, C_DT:C_DT + 64])
                P.tt('dve', dtt.f, dtt.f, dtb.f, ALU.add, [dtt, dtb], [dtt])
                P.act(dtt.f, dtt.f, AF.Exp, [dtt], [dtt])
                P.ts('dve', dtt.f, dtt.f, 1.0, None, ALU.add, None, [dtt], [dtt])
                P.act(dtt.f, dtt.f, AF.Ln, [dtt], [dtt])
                P.store(dtt, DT[t0:t0 + 128, :], dtt.f)
            P.reset(mk)

            mk = P.mark()
            caw = P.alloc(3, 1024)
            tabs = [P.alloc(1024) for _ in range(2)]
            tchs = [[P.alloc(2048) for _ in range(3)] for _ in range(2)]
            P.load(caw, caw.f, conv_a_w[l].partition_broadcast(128))
            for cidx, ch in enumerate(chunks):
                pr, t0 = ch['pr'], ch['t0']
                tab = tabs[cidx % 2]
                tch = tchs[cidx % 2]
                P.load(tab, tab.f, PROJ[pr:pr + 128, C_AB:C_AB + 1024])
                for s3 in range(3):
                    t = tch[s3]
                    P.load(t, t.f, PROJ[pr - 1 + s3:pr - 1 + s3 + 128, C_AC:C_AC + 2048])
                    e1 = 'pool' if s3 != 1 else 'dve'
                    P.tt(e1, t.f[:, 0:1024], t.f[:, 0:1024], t.f[:, 1024:2048], ALU.mult, [t], [t])
                    P.tt(e1, t.f[:, 0:1024], t.f[:, 0:1024], caw.f[:, s3, :], ALU.mult, [t, caw], [t])
                a0 = tch[0]
                P.tt('dve', a0.f[:, 0:1024], a0.f[:, 0:1024], tch[1].f[:, 0:1024], ALU.add, [a0, tch[1]], [a0])
                P.tt('dve', a0.f[:, 0:1024], a0.f[:, 0:1024], tch[2].f[:, 0:1024], ALU.add, [a0, tch[2]], [a0])
                P.tt('dve', a0.f[:, 0:1024], a0.f[:, 0:1024], tab.f, ALU.mult, [a0, tab], [a0])
                P.store(a0, U[t0:t0 + 128, 0:1024], a0.f[:, 0:1024])
            P.reset(mk)

            mk = P.mark()
            lng = P.alloc(1024)
            lnb = P.alloc(1024)
            wT = P.alloc(8, 128)
            wtmp = P.alloc(8, 128)
            sgb = P.alloc(8)
            c3sets = [(P.alloc(2048), P.alloc(2048), P.alloc(1024), P.alloc(8), P.alloc(1024)) for _ in range(2)]
            P.load(lng, lng.f, sgu_ln_g[l].partition_broadcast(128))
            P.load(lnb, lnb.f, sgu_ln_b[l].partition_broadcast(128))
            P.load(sgb, sgb.f, sgu_b[l])
            P.load(wtmp, wtmp.f, sgu_w[l].rearrange("g i j -> i g j"))
            for hf in range(2):
                pt = P.ps()
                for q in range(4):
                    P.tr(pt, pt.f[:, q * 128:(q + 1) * 128], wtmp.f[:, hf * 4 + q, :], ident, [wtmp, cst])
                P.copy('dve', wT.f[:, hf * 4:(hf + 1) * 4, :], pt.f.rearrange("p (a b) -> p a b", a=4), [pt], [wT])
            for cidx, ch in enumerate(chunks):
                pr, t0 = ch['pr'], ch['t0']
                suv, gt_, junkc3, stc3, uc = c3sets[cidx % 2]
                P.load(suv, suv.f, PROJ[pr:pr + 128, C_SU:C_SU + 2048])
                P.tt('pool', gt_.f, suv.f, suv.f, ALU.mult, [suv], [gt_])
                P.ts('dve', gt_.f, gt_.f, 0.044715, 1.0, ALU.mult, ALU.add, [gt_], [gt_])
                P.tt('dve', gt_.f, gt_.f, suv.f, ALU.mult, [gt_, suv], [gt_])
                P.act(gt_.f, gt_.f, AF.Sigmoid, [gt_], [gt_], scale=1.5957691216057308)
                P.tt('dve', suv.f, suv.f, gt_.f, ALU.mult, [suv, gt_], [suv])
                u_ = suv.f[:, 0:1024]
                v_ = suv.f[:, 1024:2048]
                P.op('dve', lambda e, v_=v_, stc3=stc3: e.tensor_reduce(out=stc3.f[:, 0:1], in_=v_, axis=mybir.AxisListType.X, op=ALU.add),
                     [suv], [stc3])
                P.ts('dve', stc3.f[:, 1:2], stc3.f[:, 0:1], -1.0 / 1024, None, ALU.mult, None, [stc3], [stc3])
                P.ts('dve', v_, v_, stc3.f[:, 1:2], None, ALU.add, None, [suv, stc3], [suv])
                P.op('act', lambda e, v_=v_, stc3=stc3, junkc3=junkc3: e.activation(out=junkc3.f, in_=v_, func=AF.Square, accum_out=stc3.f[:, 2:3]),
                     [suv], [junkc3, stc3])
                P.ts('dve', stc3.f[:, 3:4], stc3.f[:, 2:3], 1.0 / 1024, EPS, ALU.mult, ALU.add, [stc3], [stc3])
                P.act(stc3.f[:, 4:5], stc3.f[:, 3:4], AF.Sqrt, [stc3], [stc3])
                P.op('dve', lambda e, stc3=stc3: e.reciprocal(out=stc3.f[:, 5:6], in_=stc3.f[:, 4:5]), [stc3], [stc3])
                P.stt(v_, v_, stc3.f[:, 5:6], lng.f, ALU.mult, ALU.mult, [suv, stc3, lng], [suv])
                P.tt('dve', v_, v_, lnb.f, ALU.add, [suv, lnb], [suv])
                for hf in range(2):
                    pt = P.ps()
                    for q in range(4):
                        g = hf * 4 + q
                        P.mm(pt, pt.f[:, q * 128:(q + 1) * 128], [(wT.f[:, g, :], suv.f[:, 1024 + g * 128:1024 + (g + 1) * 128])],
                             [wT, suv])
                    for q in range(4):
                        g = hf * 4 + q
                        P.stt(uc.f[:, g * 128:(g + 1) * 128], pt.f[:, q * 128:(q + 1) * 128], sgb.f[:, g:g + 1],
                              suv.f[:, g * 128:(g + 1) * 128], ALU.add, ALU.mult, [pt, sgb, suv], [uc])
                P.store(uc, U[t0:t0 + 128, 3072:4096], uc.f)
            P.reset(mk)
            if cfg.get('stop') == 'c3':
                break

            li = 0
            for dr in range(2):
                mk = P.mark()
                NB = 2 if dr == 0 else 1
                S = P.alloc(2048)
                xw = P.alloc(2048)
                yt = P.alloc(2048)
                if dr == 1:
                    zt = P.alloc(2048)
                    ypt = P.alloc(2048)
                    nwbc = P.alloc(2048)
                    P.load(nwbc, nwbc.f, ssd_norm_w[l].partition_broadcast(128))
                Rs = [P.alloc(4, 128) for _ in range(2)]
                Qs = [P.alloc(4, 128) for _ in range(2)]
                Lts = [P.alloc(4, 128) for _ in range(2)]
                WTs = [P.alloc(4, 128) for _ in range(2)]
                csets = [dict(xbc=P.alloc(3072), BT=P.alloc(4, 128), CT=P.alloc(4, 128), ST=P.alloc(4, 128), dts=P.alloc(32),
                              da=P.alloc(32), PT=P.alloc(64), lnw=P.alloc(32), biasL=P.alloc(32), eP=P.alloc(32),
                              etot=P.alloc(32), wdec=P.alloc(32)) for _ in range(NB)]
                abc = P.alloc(2, 32)
                Dbc = P.alloc(32)
                st4 = P.alloc(16)
                P.load(abc, abc.f, ssd_a_log[l].partition_broadcast(128))
                P.act(abc.f, abc.f, AF.Exp, [abc], [abc])
                P.ts('dve', abc.f, abc.f, -1.0, None, ALU.mult, None, [abc], [abc])
                P.load(Dbc, Dbc.f, ssd_d[l].partition_broadcast(128))
                cnum = 0
                cum = Umat if dr == 0 else UTmat
                msk = maskF if dr == 0 else maskB
                for si in seqinfo:
                    if si['kind'] == 0:
                        P.load(xw, xw.f.rearrange("p (k n) -> p k n", k=16), h0[l, dr].rearrange("(k p) n -> p k n", p=128))
                        for q4 in range(4):
                            pt = P.ps()
                            for q in range(4):
                                k = q4 * 4 + q
                                P.tr(pt, pt.f[:, q * 128:(q + 1) * 128], xw.f[:, k * 128:(k + 1) * 128], ident, [xw, cst])
                            P.copy('dve', S.f[:, q4 * 512:(q4 + 1) * 512], pt.f, [pt], [S])
                    else:
                        P.op('pool', lambda e, S=S: e.memset(S.f, 0.0), [], [S])
                    order = list(range(si['n'])) if dr == 0 else list(range(si['n'] - 1, -1, -1))
                    for ci in order:
                        ch = chunks[si['first'] + ci]
                        pr, t0 = ch['pr'], ch['t0']
                        cs_ = csets[cnum % NB]
                        cnum += 1
                        xbc, BT, CT, ST, dts, da, PT = cs_['xbc'], cs_['BT'], cs_['CT'], cs_['ST'], cs_['dts'], cs_['da'], cs_['PT']
                        lnw, biasL, eP, etot, wdec = cs_['lnw'], cs_['biasL'], cs_['eP'], cs_['etot'], cs_['wdec']
                        P.load(xbc, xbc.f, XBC[t0:t0 + 128, :])
                        P.load(dts, dts.f, DT[t0:t0 + 128, dr * 32:(dr + 1) * 32])
                        if dr == 1:
                            P.load(zt, zt.f, PROJ[pr:pr + 128, C_Z:C_Z + 2048])
                            P.load(ypt, ypt.f, YP[t0:t0 + 128, :])
                        P.tt('dve', da.f, dts.f, abc.f[:, dr, :], ALU.mult, [dts, abc], [da])
                        pt0 = P.ps()
                        P.mm(pt0, pt0.f[:, 0:32], [(cum, da.f)], [cst, da])
                        P.mm(pt0, pt0.f[:, 32:64], [(ones, da.f)], [cst, da])
                        P.copy('dve', PT.f, pt0.f[:, 0:64], [pt0], [PT])
                        P.act(lnw.f, dts.f, AF.Ln, [dts], [lnw])
                        P.tt('dve', biasL.f, lnw.f, PT.f[:, 0:32], ALU.subtract, [lnw, PT], [biasL])
                        P.act(eP.f, PT.f[:, 0:32], AF.Exp, [PT], [eP])
                        P.act(etot.f, PT.f[:, 32:64], AF.Exp, [PT], [etot])
                        P.tt('dve', wdec.f, biasL.f, PT.f[:, 32:64], ALU.add, [biasL, PT], [wdec])
                        P.act(wdec.f, wdec.f, AF.Exp, [wdec], [wdec])
                        ptb = P.ps()
                        ptc = P.ps()
                        for g in range(4):
                            P.tr(ptb, ptb.f[:, g * 128:(g + 1) * 128], xbc.f[:, 2048 + g * 128:2048 + (g + 1) * 128], ident, [xbc, cst])
                            P.tr(ptc, ptc.f[:, g * 128:(g + 1) * 128], xbc.f[:, 2560 + g * 128:2560 + (g + 1) * 128], ident, [xbc, cst])
                        P.copy('act', BT.f, ptb.f.rearrange("p (a b) -> p a b", a=4), [ptb], [BT])
                        P.copy('dve', CT.f, ptc.f.rearrange("p (a b) -> p a b", a=4), [ptc], [CT])
                        pts = P.ps()
                        for g in range(4):
                            P.mm(pts, pts.f[:, g * 128:(g + 1) * 128], [(BT.f[:, g, :], CT.f[:, g, :])], [BT, CT])
                        P.copy('act', ST.f, pts.f.rearrange("p (a b) -> p a b", a=4), [pts], [ST])
                        for g in range(4):
                            pty = P.ps()
                            pto = P.ps()
                            P.mm(pto, pto.f, [(CT.f[:, g, :], S.f[:, g * 512:(g + 1) * 512])], [CT, S])
                            for hh in range(2):
                                hb = g * 8 + hh * 4
                                R_, Q_, Lt, WT = Rs[li % 2], Qs[li % 2], Lts[li % 2], WTs[li % 2]
                                li += 1
                                P.tt('pool', R_.f, bc(cum.unsqueeze(1), [128, 4, 128]), bc(da.f[:, hb:hb + 4].unsqueeze(2), [128, 4, 128]),
                                     ALU.mult, [cst, da], [R_])
                                P.tt('pool', Q_.f, bc(msk.unsqueeze(1), [128, 4, 128]), bc(biasL.f[:, hb:hb + 4].unsqueeze(2), [128, 4, 128]),
                                     ALU.add, [cst, biasL], [Q_])
                                ptl = P.ps()
                                P.mm(ptl, ptl.f, [(ones, R_.f.rearrange("p a b -> p (a b)")), (ident, Q_.f.rearrange("p a b -> p (a b)"))],
                                     [cst, R_, Q_])
                                P.act(Lt.f, ptl.f.rearrange("p (a b) -> p a b", a=4), AF.Exp, [ptl], [Lt])
                                P.tt('dve', WT.f, Lt.f, bc(ST.f[:, g:g + 1, :], [128, 4, 128]), ALU.mult, [Lt, ST], [WT])
                                for h4 in range(4):
                                    hd = hb + h4
                                    c0 = (hh * 4 + h4) * 64
                                    P.mm(pty, pty.f[:, c0:c0 + 64], [(WT.f[:, h4, :], xbc.f[:, hd * 64:(hd + 1) * 64])], [WT, xbc])
                            yg = yt.f[:, g * 512:(g + 1) * 512]
                            P.tt('dve', yg.rearrange("p (a b) -> p a b", a=8), pto.f.rearrange("p (a b) -> p a b", a=8),
                                 bc(eP.f[:, g * 8:(g + 1) * 8].unsqueeze(2), [128, 8, 64]), ALU.mult, [pto, eP], [yt])
                            P.tt('dve', yg, yg, pty.f, ALU.add, [yt, pty], [yt])
                        P.tt('pool', xw.f.rearrange("p (a b) -> p a b", a=32), xbc.f[:, 0:2048].rearrange("p (a b) -> p a b", a=32),
                             bc(wdec.f.unsqueeze(2), [128, 32, 64]), ALU.mult, [xbc, wdec], [xw])
                        for g in range(4):
                            ptg = P.ps()
                            P.mm(ptg, ptg.f, [(xbc.f[:, 2048 + g * 128:2048 + (g + 1) * 128], xw.f[:, g * 512:(g + 1) * 512])], [xbc, xw])
                            Sg = S.f[:, g * 512:(g + 1) * 512]
                            P.tt('dve', Sg.rearrange("p (a b) -> p a b", a=8), Sg.rearrange("p (a b) -> p a b", a=8),
                                 bc(etot.f[:, g * 8:(g + 1) * 8].unsqueeze(2), [128, 8, 64]), ALU.mult, [S, etot], [S])
                            P.tt('dve', Sg, Sg, ptg.f, ALU.add, [S, ptg], [S])
                        if dr == 0:
                            P.store(yt, YP[t0:t0 + 128, :], yt.f)
                        else:
                            P.tt('dve', yt.f, yt.f, ypt.f, ALU.add, [yt, ypt], [yt])
                            P.tt('pool', ypt.f.rearrange("p (a b) -> p a b", a=32), xbc.f[:, 0:2048].rearrange("p (a b) -> p a b", a=32),
                                 bc(Dbc.f.unsqueeze(2), [128, 32, 64]), ALU.mult, [xbc, Dbc], [ypt])
                            P.tt('dve', yt.f, yt.f, ypt.f, ALU.add, [yt, ypt], [yt])
                            P.act(zt.f, zt.f, AF.Silu, [zt], [zt])
                            P.tt('dve', yt.f, yt.f, zt.f, ALU.mult, [yt, zt], [yt])
                            for g in range(4):
                                P.op('act', lambda e, g=g, zt=zt, yt=yt, st4=st4: e.activation(out=zt.f[:, g * 512:(g + 1) * 512], in_=yt.f[:, g * 512:(g + 1) * 512],
                                                                       func=AF.Square, accum_out=st4.f[:, g:g + 1]), [yt], [zt, st4])
                            P.ts('dve', st4.f[:, 4:8], st4.f[:, 0:4], 1.0 / 512, EPS, ALU.mult, ALU.add, [st4], [st4])
                            P.act(st4.f[:, 8:12], st4.f[:, 4:8], AF.Sqrt, [st4], [st4])
                            P.op('dve', lambda e, st4=st4: e.reciprocal(out=st4.f[:, 12:16], in_=st4.f[:, 8:12]), [st4], [st4])
                            for g in range(4):
                                P.stt(yt.f[:, g * 512:(g + 1) * 512], yt.f[:, g * 512:(g + 1) * 512], st4.f[:, 12 + g:13 + g],
                                      nwbc.f[:, g * 512:(g + 1) * 512], ALU.mult, ALU.mult, [yt, st4, nwbc], [yt])
                            P.store(yt, U[t0:t0 + 128, 1024:3072], yt.f)
                    if si['kind'] == 1:
                        for half in range(2):
                            for q in range(4):
                                pt = P.ps()
                                for h4 in range(4):
                                    hd = half * 16 + q * 4 + h4
                                    P.tr(pt, pt.f[0:64, h4 * 128:(h4 + 1) * 128], S.f[:, hd * 64:(hd + 1) * 64], ident, [S, cst])
                                P.copy('dve', xw.f[0:64, q * 512:(q + 1) * 512], pt.f[0:64, :], [pt], [xw])
                            P.store(xw, st_out[si['pri'], l, dr, half * 16:(half + 1) * 16].rearrange("h p n -> p h n"),
                                    xw.f[0:64, :].rearrange("p (h n) -> p h n", h=16))
                P.reset(mk)
            if cfg.get('stop') == 'ssd':
                break

            mk = P.mark()
            TDN = 4
            UT = P.allocr(32, TDN * 128)
            mT = UT
            wds = [P.allocr(KD, 256) for _ in range(2)]
            utile = P.alloc(4096)
            merged = [P.alloc(2048) for _ in range(TDN)]
            xch = [P.alloc(2048) for _ in range(TDN)]
            gts = [P.alloc(256) for _ in range(2)]
            g1s = [P.alloc(256) for _ in range(2)]
            wi = 0
            gi_ = 0
            ci_ = 0
            for g0 in range(0, NCH, TDN):
                grp = chunks[g0:g0 + TDN]
                for j, ch in enumerate(grp):
                    P.load(utile, utile.f, U[ch['t0']:ch['t0'] + 128, :])
                    P.load(xch[j], xch[j].f, xsrc[ch['t0']:ch['t0'] + 128, :])
                    for q4 in range(8):
                        pt = P.ps()
                        for q in range(4):
                            k = q4 * 4 + q
                            P.tr(pt, pt.f[:, q * 128:(q + 1) * 128], utile.f[:, k * 128:(k + 1) * 128], ident, [utile, cst])
                        ci_ += 1
                        P.copy('act' if ci_ % 2 else 'dve', UT.r[:, q4 * 4:(q4 + 1) * 4, j * 128:(j + 1) * 128],
                               pt.f.rearrange("p (a b) -> p a b", a=4), [pt], [UT])
                for bi, (wsrc, k0, KC, gcol) in enumerate(((w_out_a, 0, 8, 0), (w_out_b, 8, 16, 2048), (w_out_c, 24, 8, 4096))):
                    for nt in range(8):
                        w = wds[wi % 2]
                        wi += 1
                        wtile_load(w, w.r[:, 0:KC, :], wsrc[l, :, nt * 256:(nt + 1) * 256].rearrange("(k p) n -> p k n", p=128))
                        for j, ch in enumerate(grp):
                            pt = P.ps()
                            P.mm(pt, pt.f[:, 0:256], [(UT.r[:, k0 + k, j * 128:(j + 1) * 128], w.r[:, k, :]) for k in range(KC)], [UT, w])
                            gt = gts[gi_ % 2]
                            gi_ += 1
                            c0 = C_GATE + gcol + nt * 256
                            P.load(gt, gt.f, PROJ[ch['pr']:ch['pr'] + 128, c0:c0 + 256])
                            P.act(gt.f, gt.f, AF.Sigmoid, [gt], [gt])
                            mg = merged[j].f[:, nt * 256:(nt + 1) * 256]
                            if bi == 0:
                                P.tt('dve', mg, gt.f, pt.f[:, 0:256], ALU.mult, [gt, pt], [merged[j]])
                            else:
                                P.tt('dve', gt.f, gt.f, pt.f[:, 0:256], ALU.mult, [gt, pt], [gt])
                                P.tt('dve', mg, mg, gt.f, ALU.add, [merged[j], gt], [merged[j]])
                for j, ch in enumerate(grp):
                    for q4 in range(4):
                        pt = P.ps()
                        for q in range(4):
                            k = q4 * 4 + q
                            P.tr(pt, pt.f[:, q * 128:(q + 1) * 128], merged[j].f[:, k * 128:(k + 1) * 128], ident, [merged[j], cst])
                        ci_ += 1
                        P.copy('act' if ci_ % 2 else 'dve', mT.r[:, q4 * 4:(q4 + 1) * 4, j * 128:(j + 1) * 128],
                               pt.f.rearrange("p (a b) -> p a b", a=4), [pt], [mT])
                for nt in range(8):
                    w = wds[wi % 2]
                    wi += 1
                    wtile_load(w, w.r, w_o[l, :, nt * 256:(nt + 1) * 256].rearrange("(k p) n -> p k n", p=128))
                    for j, ch in enumerate(grp):
                        r = 1 if ch['kind'] == 0 else 0
                        pt = P.ps()
                        P.mm(pt, pt.f[:, 0:256], [(mT.r[:, k, j * 128:(j + 1) * 128], w.r[:, k, :]) for k in range(KD)], [mT, w])
                        g1 = g1s[gi_ % 2]
                        gi_ += 1
                        P.load(g1, g1.f, MOD[r, 2 * D + nt * 256:2 * D + (nt + 1) * 256].partition_broadcast(128))
                        P.tt('dve', g1.f, g1.f, pt.f[:, 0:256], ALU.mult, [g1, pt], [g1])
                        xs_ = xch[j].f[:, nt * 256:(nt + 1) * 256]
                        P.tt('dve', xs_, xs_, g1.f, ALU.add, [xch[j], g1], [xch[j]])
                for j, ch in enumerate(grp):
                    P.store(xch[j], XM[ch['t0']:ch['t0'] + 128, :], xch[j].f)
            P.reset(mk)
            if cfg.get('stop') == 'mix':
                break

            mk = P.mark()
            TE = 4
            last = (l == DEPTH - 1)
            h2T = P.allocr(KD, TE * 128)
            actT = P.allocr(4, TE * 128)
            wgu = [[P.allocr(KD, 128) for _ in range(2)] for _ in range(2)]
            wdn = [P.allocr(4, 512) for _ in range(2)]
            xe = [P.alloc(2048) for _ in range(TE)]
            acc = [P.alloc(2048) for _ in range(TE)]
            tmp = dict(ss=P.alloc(8), junk=P.alloc(D))
            h32 = P.alloc(KD, 128)
            sgs = [P.alloc(TE * 128) for _ in range(2)]
            rw = P.alloc(KD, 20)
            rbb = P.alloc(20)
            lg = P.alloc(20)
            rs = P.alloc(64)
            combs = [P.alloc(16) for _ in range(TE)]
            g2s = [P.alloc(512) for _ in range(2)]
            P.load(rw, rw.f, router_w[l].rearrange("(k p) n -> p k n", p=128))
            P.load(rbb, rbb.f, router_b[l].partition_broadcast(128))
            ui = 0
            di = 0
            si_ = 0
            g2i = 0
            for g0 in range(0, NCH, TE):
                grp = chunks[g0:g0 + TE]
                nj = len(grp)
                NTK = nj * 128
                for j, ch in enumerate(grp):
                    P.load(xe[j], xe[j].f, XM[ch['t0']:ch['t0'] + 128, :])
                    make_hT(xe[j], 1, ch, h2T, j * 128, tmp, posB=None, hT32=h32)
                    pt = P.ps()
                    P.mm(pt, pt.f[:, 0:20], [(h32.f[:, k, :], rw.f[:, k, :]) for k in range(KD)], [h32, rw])
                    P.tt('dve', lg.f, pt.f[:, 0:20], rbb.f, ALU.add, [pt, rbb], [lg])
                    R_ = rs.f
                    cb_ = combs[j]
                    P.op('dve', lambda e, R_=R_: e.tensor_reduce(out=R_[:, 0:1], in_=lg.f[:, 0:4], axis=mybir.AxisListType.X, op=ALU.max), [lg], [rs])
                    P.ts('dve', R_[:, 1:2], R_[:, 0:1], -1.0, None, ALU.mult, None, [rs], [rs])
                    P.op('act', lambda e, R_=R_: e.activation(out=R_[:, 4:8], in_=lg.f[:, 0:4], func=AF.Exp, bias=R_[:, 1:2], scale=1.0,
                                                            accum_out=R_[:, 2:3]), [lg, rs], [rs])
                    P.op('dve', lambda e, R_=R_: e.reciprocal(out=R_[:, 3:4], in_=R_[:, 2:3]), [rs], [rs])
                    P.ts('dve', R_[:, 8:12], lg.f[:, 0:4], R_[:, 0:1], None, ALU.is_equal, None, [lg, rs], [rs])
                    P.tt('dve', R_[:, 16:32].rearrange("p (g k) -> p g k", g=4), lg.f[:, 4:20].rearrange("p (g k) -> p g k", g=4),
                         bc(R_[:, 8:12].unsqueeze(2), [128, 4, 4]), ALU.mult, [lg, rs], [rs])
                    P.op('dve', lambda e, R_=R_: e.tensor_reduce(out=R_[:, 12:16], in_=R_[:, 16:32].rearrange("p (g k) -> p k g", g=4),
                                                               axis=mybir.AxisListType.X, op=ALU.add), [rs], [rs])
                    P.op('dve', lambda e, R_=R_: e.tensor_reduce(out=R_[:, 32:33], in_=R_[:, 12:16], axis=mybir.AxisListType.X, op=ALU.max), [rs], [rs])
                    P.ts('dve', R_[:, 33:34], R_[:, 32:33], -1.0, None, ALU.mult, None, [rs], [rs])
                    P.op('act', lambda e, R_=R_: e.activation(out=R_[:, 36:40], in_=R_[:, 12:16], func=AF.Exp, bias=R_[:, 33:34], scale=1.0), [rs], [rs])
                    P.ts('dve', R_[:, 40:44], R_[:, 12:16], R_[:, 32:33], None, ALU.is_equal, None, [rs], [rs])
                    P.stt(R_[:, 44:48], R_[:, 40:44], -2.0, R_[:, 36:40], ALU.mult, ALU.add, [rs], [rs])
                    P.op('dve', lambda e, R_=R_: e.tensor_reduce(out=R_[:, 34:35], in_=R_[:, 44:48], axis=mybir.AxisListType.X, op=ALU.max), [rs], [rs])
                    P.ts('dve', R_[:, 48:52], R_[:, 44:48], R_[:, 34:35], None, ALU.is_equal, None, [rs], [rs])
                    P.ts('dve', R_[:, 35:36], R_[:, 34:35], 1.0, None, ALU.add, None, [rs], [rs])
                    P.op('dve', lambda e, R_=R_: e.reciprocal(out=R_[:, 52:53], in_=R_[:, 35:36]), [rs], [rs])
                    P.tt('dve', R_[:, 53:54], R_[:, 52:53], R_[:, 3:4], ALU.mult, [rs], [rs])
                    P.tt('dve', R_[:, 54:55], R_[:, 53:54], R_[:, 34:35], ALU.mult, [rs], [rs])
                    P.ts('dve', R_[:, 56:60], R_[:, 40:44], R_[:, 53:54], None, ALU.mult, None, [rs], [rs])
                    P.stt(R_[:, 56:60], R_[:, 48:52], R_[:, 54:55], R_[:, 56:60], ALU.mult, ALU.add, [rs], [rs])
                    P.tt('dve', cb_.f.rearrange("p (g k) -> p g k", g=4), bc(R_[:, 8:12].unsqueeze(2), [128, 4, 4]),
                         bc(R_[:, 56:60].unsqueeze(1), [128, 4, 4]), ALU.mult, [rs], [cb_])
                    if DBG:
                        P.store(cb_, COMB[ch['t0']:ch['t0'] + 128, :], cb_.f)
                for ex in range(NE):
                    for fc in range(4):
                        wg, wu = wgu[ui % 2]
                        ui += 1
                        wtile_load(wg, wg.r, exp_w_gate[l, ex, :, fc * 128:(fc + 1) * 128].rearrange("(k p) f -> p k f", p=128))
                        wtile_load(wu, wu.r, exp_w_up[l, ex, :, fc * 128:(fc + 1) * 128].rearrange("(k p) f -> p k f", p=128))
                        ptg = P.ps()
                        P.mm(ptg, ptg.f[:, 0:NTK], [(wg.r[:, k, :], h2T.r[:, k, 0:NTK]) for k in range(KD)], [wg, h2T])
                        ptu = P.ps()
                        P.mm(ptu, ptu.f[:, 0:NTK], [(wu.r[:, k, :], h2T.r[:, k, 0:NTK]) for k in range(KD)], [wu, h2T])
                        sg = sgs[si_ % 2]
                        si_ += 1
                        P.act(sg.f[:, 0:NTK], ptg.f[:, 0:NTK], AF.Silu, [ptg], [sg])
                        P.tt('dve', actT.r[:, fc, 0:NTK], sg.f[:, 0:NTK], ptu.f[:, 0:NTK], ALU.mult, [sg, ptu], [actT])
                    for dq in range(4):
                        wd = wdn[di % 2]
                        di += 1
                        wtile_load(wd, wd.r, exp_w_down[l, ex, :, dq * 512:(dq + 1) * 512].rearrange("(c p) n -> p c n", p=128))
                        for j in range(nj):
                            pt = P.ps()
                            P.mm(pt, pt.f, [(actT.r[:, fc, j * 128:(j + 1) * 128], wd.r[:, fc, :]) for fc in range(4)], [actT, wd])
                            dst = acc[j].f[:, dq * 512:(dq + 1) * 512]
                            if ex == 0:
                                P.ts('dve', dst, pt.f, combs[j].f[:, ex:ex + 1], None, ALU.mult, None, [pt, combs[j]], [acc[j]])
                            else:
                                P.stt(dst, pt.f, combs[j].f[:, ex:ex + 1], dst, ALU.mult, ALU.add, [pt, combs[j], acc[j]], [acc[j]])
                for j, ch in enumerate(grp):
                    r = 1 if ch['kind'] == 0 else 0
                    for d4 in range(4):
                        g2 = g2s[g2i % 2]
                        g2i += 1
                        P.load(g2, g2.f, MOD[r, 5 * D + d4 * 512:5 * D + (d4 + 1) * 512].partition_broadcast(128))
                        sl = slice(d4 * 512, (d4 + 1) * 512)
                        P.tt('dve', g2.f, g2.f, acc[j].f[:, sl], ALU.mult, [g2, acc[j]], [g2])
                        P.tt('dve', xe[j].f[:, sl], xe[j].f[:, sl], g2.f, ALU.add, [xe[j], g2], [xe[j]])
                    if not last:
                        P.store(xe[j], XR[ch['t0']:ch['t0'] + 128, :], xe[j].f)
                    else:
                        if DBG:
                            P.store(xe[j], XR[ch['t0']:ch['t0'] + 128, :], xe[j].f)
                        ss = tmp['ss']
                        junk = tmp['junk']
                        P.op('act', lambda e, j=j: e.activation(out=junk.f, in_=xe[j].f, func=AF.Square, accum_out=ss.f[:, 0:1]),
                             [xe[j]], [junk, ss])
                        P.ts('dve', ss.f[:, 1:2], ss.f[:, 0:1], 1.0 / D, EPS, ALU.mult, ALU.add, [ss], [ss])
                        P.act(ss.f[:, 2:3], ss.f[:, 1:2], AF.Sqrt, [ss], [ss])
                        P.op('dve', lambda e: e.reciprocal(out=ss.f[:, 3:4], in_=ss.f[:, 2:3]), [ss], [ss])
                        for d4 in range(4):
                            g2 = g2s[g2i % 2]
                            g2i += 1
                            sl = slice(d4 * 512, (d4 + 1) * 512)
                            P.load(g2, g2.f, final_norm_w[0, sl].partition_broadcast(128))
                            P.stt(junk.f[:, sl], xe[j].f[:, sl], ss.f[:, 3:4], g2.f, ALU.mult, ALU.mult, [xe[j], ss, g2], [junk])
                        P.store(junk, y_out[ch['t0']:ch['t0'] + 128, :], junk.f)
            P.reset(mk)

        P.barrier()
        P.emit(block)
    return nc


def make_consts():
    k = np.arange(128)[:, None]
    i = np.arange(128)[None, :]
    c = np.zeros((128, 6, 128), np.float32)
    c[:, 0] = (k == i)
    c[:, 1] = (k <= i)
    c[:, 2] = (k >= i)
    c[:, 3] = 1.0
    c[:, 4] = np.where(k > i, -BIG, 0.0)
    c[:, 5] = np.where(k < i, -BIG, 0.0)
    return c


def make_posT(n_tok):
    rows = n_tok // 64
    rr, cc = np.meshgrid(np.arange(rows, dtype=np.float32), np.arange(64, dtype=np.float32), indexing='ij')
    quarter = D // 4
    omega = (1.0 / (np.float32(10000.0) ** (np.arange(quarter, dtype=np.float32) / np.float32(quarter)))).astype(np.float32)
    ar = rr.reshape(-1)[:, None] * omega
    ac = cc.reshape(-1)[:, None] * omega
    pos = np.concatenate([np.sin(ar), np.cos(ar), np.sin(ac), np.cos(ac)], axis=-1).astype(np.float32)
    return np.ascontiguousarray(pos.T)


def make_in_map(inp, sample_idx, prompt_idxs, sample_len=None):
    f = lambda a: np.ascontiguousarray(np.asarray(a, dtype=np.float32))
    xs = f(inp['x_sample'])[sample_idx]
    if sample_len is not None:
        xs = xs[:sample_len]
    xp = [f(inp['x_prompt'])[i] for i in prompt_idxs]
    m = {}
    m['x_in'] = np.ascontiguousarray(np.concatenate([xs] + xp, axis=0))
    m['c2'] = np.ascontiguousarray(np.stack([f(inp['c_ctx']), f(inp['c'])[sample_idx]], axis=0))
    m['h0'] = np.ascontiguousarray(f(inp['state_ssd'])[sample_idx].reshape(2, 2, H * HP, NS))
    m['posT'] = make_posT(xs.shape[0])
    m['consts'] = make_consts()
    for k in ['w_ada', 'b_ada', 'w_in', 'norm_mix_w', 'norm_ffn_w', 'conv_a_w', 'w_out_a', 'ssd_conv_w', 'ssd_conv_b',
              'ssd_a_log', 'ssd_d', 'ssd_norm_w', 'w_out_b', 'sgu_ln_g', 'sgu_ln_b', 'sgu_w', 'sgu_b', 'w_out_c', 'w_o',
              'exp_w_gate', 'exp_w_up', 'exp_w_down']:
        m[k] = f(inp[k])
    m['ssd_dt_bias'] = f(inp['ssd_dt_bias']).reshape(2, 2 * H)
    m['router_w'] = np.ascontiguousarray(np.concatenate([f(inp['router_g_w']), f(inp['router_e_w'])], axis=-1))
    m['router_b'] = np.ascontiguousarray(np.concatenate([f(inp['router_g_b']), f(inp['router_e_b'])], axis=-1))
    m['final_norm_w'] = f(inp['final_norm_w']).reshape(1, D)
    return m


_NC_CACHE = {}


def kernel(**inputs):
    seqs = [(4096, 0), (256, 1), (256, 1)]
    key = 'full'
    if key not in _NC_CACHE:
        _NC_CACHE[key] = build(dict(seqs=seqs, depth=2, debug=False))
    nc = _NC_CACHE[key]
    base = make_in_map(inputs, 0, [0, 1])
    in_maps = []
    xs_all = np.asarray(inputs['x_sample'], dtype=np.float32)
    xp_all = np.asarray(inputs['x_prompt'], dtype=np.float32)
    c_all = np.asarray(inputs['c'], dtype=np.float32)
    cctx = np.asarray(inputs['c_ctx'], dtype=np.float32)
    st_all = np.asarray(inputs['state_ssd'], dtype=np.float32)
    for core in range(8):
        si = core % 2
        m = dict(base)
        m['x_in'] = np.ascontiguousarray(np.concatenate([xs_all[si], xp_all[2 * core], xp_all[2 * core + 1]], axis=0))
        m['c2'] = np.ascontiguousarray(np.stack([cctx, c_all[si]], axis=0))
        m['h0'] = np.ascontiguousarray(st_all[si].reshape(2, 2, H * HP, NS))
        in_maps.append(m)
    res = run_bass_kernel_spmd(nc, in_maps, core_ids=list(range(8)))
    y_prompt = np.zeros((16, 256, D), np.float32)
    y_sample = np.zeros((2, 4096, D), np.float32)
    new_state = np.zeros((16, 2, 2, H, HP, NS), np.float32)
    for core in range(8):
        r = res.results[core]
        yo = np.asarray(r['y_out'])
        if core < 2:
            y_sample[core] = yo[0:4096]
        y_prompt[2 * core] = yo[4096:4352]
        y_prompt[2 * core + 1] = yo[4352:4608]
        so = np.asarray(r['st_out'])
        new_state[2 * core] = so[0]
        new_state[2 * core + 1] = so[1]
    return (y_prompt, y_sample, new_state)
```

```python
import numpy as np
from contextlib import ExitStack
import concourse.bass as bass
import concourse.mybir as mybir
from concourse.bass_utils import run_bass_kernel_spmd

F32 = mybir.dt.float32
F32R = mybir.dt.float32r
AF = mybir.ActivationFunctionType
ALU = mybir.AluOpType

D = 2048
KD = 16
IN_DIM = 16448
ADA = 6 * D
H = 32
HP = 64
NS = 128
G = 4
NE = 16
DE = 512
EPS = 1e-6
BIG = 30000.0
C_AB, C_AC, C_AH, C_Z, C_XBC, C_DT, C_SU, C_SV, C_GATE = 0, 1024, 2048, 3072, 5120, 8192, 8256, 9280, 10304
ARENA = 24 * 1024
NDS = 96


class Tile:
    def __init__(self, r, f):
        self.r = r
        self.f = f
        self.w = None
        self.rd = {}
        self.dsem = None


class Prog:
    ENG = ['pe', 'act', 'dve', 'pool', 'sp']

    def __init__(self, nc, es, arena, arena_r, psum):
        self.nc = nc
        self.q = {e: [] for e in self.ENG}
        self.csem = {e: es.enter_context(nc.semaphore('c_' + e)) for e in self.ENG}
        self.cnt = {e: 0 for e in self.ENG}
        self.dsems = [es.enter_context(nc.semaphore('d%d' % i)) for i in range(NDS)]
        self.dcnt = [0] * NDS
        self.dfree = list(range(NDS))
        self.seen = {e: {} for e in self.ENG}
        self.arena = arena
        self.arena_r = arena_r
        self.off = 0
        self.off_r = 0
        self.phase_tiles = []
        self.pbanks = [Tile(None, p[:]) for p in psum]
        self.pidx = 0

    def alloc(self, *shape, r=False):
        n = int(np.prod(shape))
        if r:
            assert self.off_r + n <= ARENA, ("arena_r overflow", self.off_r, n)
            r = self.arena_r[:, self.off_r:self.off_r + n]
            f = r.bitcast(F32)
            self.off_r += n
        else:
            assert self.off + n <= ARENA, ("arena overflow", self.off, n)
            f = self.arena[:, self.off:self.off + n]
            r = f
            self.off += n
        if len(shape) == 2:
            r = r.rearrange("p (a b) -> p a b", a=shape[0])
            f = f.rearrange("p (a b) -> p a b", a=shape[0])
        elif len(shape) == 3:
            r = r.rearrange("p (a b c) -> p a b c", a=shape[0], b=shape[1])
            f = f.rearrange("p (a b c) -> p a b c", a=shape[0], b=shape[1])
        t = Tile(r, f)
        self.phase_tiles.append(t)
        return t

    def allocr(self, *shape):
        return self.alloc(*shape, r=True)

    def mark(self):
        return (self.off, len(self.phase_tiles), self.off_r)

    def reset(self, mark):
        self.barrier()
        off, nt, off_r = mark
        for t in self.phase_tiles[nt:]:
            if t.dsem is not None:
                self.dfree.append(t.dsem)
        del self.phase_tiles[nt:]
        self.off = off
        self.off_r = off_r

    def ps(self):
        t = self.pbanks[self.pidx]
        self.pidx = (self.pidx + 1) % len(self.pbanks)
        return t

    def _semobj(self, k):
        return self.csem[k[1]] if k[0] == 'c' else self.dsems[k[1]]

    def _deps(self, eng, reads, writes):
        need = {}

        def add(ev):
            if ev is None:
                return
            k, v = ev
            if k == ('c', 'pe') and eng == 'pe':
                return
            if need.get(k, 0) < v:
                need[k] = v
        for t in reads:
            add(t.w)
        for t in writes:
            add(t.w)
            for k, v in t.rd.items():
                add((k, v))
        waits = []
        for k, v in need.items():
            if self.seen[eng].get(k, 0) >= v:
                continue
            self.seen[eng][k] = v
            waits.append((self._semobj(k), v))
        return waits

    def _commit(self, ev, reads, writes):
        k, v = ev
        for t in reads:
            if t.rd.get(k, 0) < v:
                t.rd[k] = v
        for t in writes:
            t.w = ev
            t.rd = {}

    def op(self, eng, fn, reads=(), writes=()):
        waits = self._deps(eng, reads, writes)
        self.cnt[eng] += 1
        ev = (('c', eng), self.cnt[eng])
        self._commit(ev, reads, writes)
        sem = self.csem[eng]

        def run(e):
            for s, v in waits:
                e.wait_ge(s, v)
            ins = fn(e)
            ins.then_inc(sem, 1)
        self.q[eng].append(run)

    def dma(self, eng, tile, pairs, write, slow=False):
        reads, writes = ((), (tile,)) if write else ((tile,), ())
        waits = self._deps(eng, reads, writes)
        if tile.dsem is None:
            tile.dsem = self.dfree.pop()
        i = tile.dsem
        self.dcnt[i] += 16 * len(pairs)
        ev = (('d', i), self.dcnt[i])
        self._commit(ev, reads, writes)
        sem = self.dsems[i]
        kw = dict(allow_slow_non_contiguous=True) if slow else {}

        def run(e):
            for s, v in waits:
                e.wait_ge(s, v)
            for (o, a) in pairs:
                e.dma_start(out=o, in_=a, **kw).then_inc(sem, 16)
        self.q[eng].append(run)

    def load(self, tile, out_ap, in_ap, eng='sp', slow=False):
        self.dma(eng, tile, [(out_ap, in_ap)], True, slow)

    def store(self, tile, out_ap, in_ap, eng='sp', slow=False):
        self.dma(eng, tile, [(out_ap, in_ap)], False, slow)

    def barrier(self):
        for e in self.ENG:
            waits = []
            for e2 in self.ENG:
                v = self.cnt[e2]
                k = ('c', e2)
                if v and self.seen[e].get(k, 0) < v:
                    self.seen[e][k] = v
                    waits.append((self.csem[e2], v))
            for i in range(NDS):
                v = self.dcnt[i]
                k = ('d', i)
                if v and self.seen[e].get(k, 0) < v:
                    self.seen[e][k] = v
                    waits.append((self.dsems[i], v))
            if waits:
                self.q[e].append(lambda eng, waits=waits: [eng.wait_ge(s, v) for s, v in waits])

    def emit(self, block):
        m = {'pe': block.tensor, 'act': block.scalar, 'dve': block.vector, 'pool': block.gpsimd, 'sp': block.sync}
        for en in self.ENG:
            def f(eng, en=en):
                for run in self.q[en]:
                    run(eng)
            m[en](f)

    def mm(self, pt, out, pairs, reads):
        n = len(pairs)

        def fn(e):
            ins = None
            for i, (l, r) in enumerate(pairs):
                ins = e.matmul(out, l, r, start=(i == 0), stop=(i == n - 1))
            return ins
        self.op('pe', fn, reads, [pt])

    def tr(self, pt, out, in_, ident, reads):
        self.op('pe', lambda e: e.transpose(out, in_, ident), reads, [pt])

    def act(self, out, in_, func, reads, writes, eng='act', **kw):
        self.op('act', lambda e: e.activation(out=out, in_=in_, func=func, **kw), reads, writes)

    def tt(self, eng, out, in0, in1, op, reads, writes):
        self.op(eng, lambda e: e.tensor_tensor(out=out, in0=in0, in1=in1, op=op), reads, writes)

    def ts(self, eng, out, in0, s1, s2, op0, op1, reads, writes):
        if s2 is None:
            self.op(eng, lambda e: e.tensor_scalar(out=out, in0=in0, scalar1=s1, scalar2=None, op0=op0), reads, writes)
        else:
            self.op(eng, lambda e: e.tensor_scalar(out=out, in0=in0, scalar1=s1, scalar2=s2, op0=op0, op1=op1), reads, writes)

    def stt(self, out, in0, scalar, in1, op0, op1, reads, writes):
        self.op('dve', lambda e: e.scalar_tensor_tensor(out=out, in0=in0, scalar=scalar, in1=in1, op0=op0, op1=op1),
                reads, writes)

    def copy(self, eng, out, in_, reads, writes):
        if eng == 'act':
            self.op('act', lambda e: e.activation(out=out, in_=in_, func=AF.Copy), reads, writes)
        else:
            self.op(eng, lambda e: e.tensor_copy(out=out, in_=in_), reads, writes)


def bc(ap, shape):
    return ap.to_broadcast(list(shape))


def build(cfg):
    SEQS = cfg['seqs']
    DEPTH = cfg.get('depth', 2)
    DBG = cfg.get('debug', False)
    NT = sum(l for l, _ in SEQS)
    LS = max([l for l, k in SEQS if k == 0] + [128])
    NPR = sum(1 for _, k in SEQS if k == 1)
    NPROW = NT + 2 * len(SEQS)
    chunks = []
    t0 = 0
    pb = 0
    seqinfo = []
    pri = 0
    for s, (l, k) in enumerate(SEQS):
        nci = l // 128
        first = len(chunks)
        for ci in range(nci):
            chunks.append(dict(s=s, ci=ci, t0=t0 + ci * 128, pr=pb + 1 + ci * 128, kind=k, nci=nci, p0=ci * 128))
        seqinfo.append(dict(first=first, n=nci, kind=k, pb=pb, len=l, t0=t0, pri=(pri if k == 1 else -1)))
        if k == 1:
            pri += 1
        t0 += l
        pb += l + 2
    NCH = len(chunks)

    nc = bass.Bass("TRN2", target_bir_lowering=False)

    def din(name, shape, dt=F32):
        return nc.dram_tensor(name, list(shape), dt, kind="ExternalInput").ap()

    def dscr(name, shape):
        return nc.dram_tensor(name, list(shape), F32, kind=("ExternalOutput" if DBG else "Internal")).ap()

    x_in = din("x_in", [NT, D])
    c2 = din("c2", [2, D])
    h0 = din("h0", [2, 2, H * HP, NS])
    posT = din("posT", [D, LS])
    consts = din("consts", [128, 6, 128])
    w_ada = din("w_ada", [2, D, ADA], F32R)
    b_ada = din("b_ada", [2, ADA])
    w_in = din("w_in", [2, D, IN_DIM], F32R)
    norm_mix_w = din("norm_mix_w", [2, D])
    norm_ffn_w = din("norm_ffn_w", [2, D])
    conv_a_w = din("conv_a_w", [2, 3, 1024])
    w_out_a = din("w_out_a", [2, 1024, D], F32R)
    ssd_conv_w = din("ssd_conv_w", [2, 3, 3072])
    ssd_conv_b = din("ssd_conv_b", [2, 3072])
    ssd_a_log = din("ssd_a_log", [2, 2, H])
    ssd_dt_bias = din("ssd_dt_bias", [2, 2 * H])
    ssd_d = din("ssd_d", [2, H])
    ssd_norm_w = din("ssd_norm_w", [2, D])
    w_out_b = din("w_out_b", [2, D, D], F32R)
    sgu_ln_g = din("sgu_ln_g", [2, 1024])
    sgu_ln_b = din("sgu_ln_b", [2, 1024])
    sgu_w = din("sgu_w", [2, 8, 128, 128])
    sgu_b = din("sgu_b", [2, 128, 8])
    w_out_c = din("w_out_c", [2, 1024, D], F32R)
    w_o = din("w_o", [2, D, D], F32R)
    router_w = din("router_w", [2, D, 20])
    router_b = din("router_b", [2, 20])
    exp_w_gate = din("exp_w_gate", [2, NE, D, DE], F32R)
    exp_w_up = din("exp_w_up", [2, NE, D, DE], F32R)
    exp_w_down = din("exp_w_down", [2, NE, DE, D], F32R)
    final_norm_w = din("final_norm_w", [1, D])

    y_out = nc.dram_tensor("y_out", [NT, D], F32, kind="ExternalOutput").ap()
    st_out = nc.dram_tensor("st_out", [max(NPR, 1), 2, 2, H, HP, NS], F32, kind="ExternalOutput").ap()

    PROJ_A = dscr("PROJ", [NPROW, 8192])
    PROJ_B = dscr("PROJB", [NPROW, IN_DIM - 8192])

    class _Proj:
        def __getitem__(self, key):
            rs, cs = key
            if cs.start >= 8192:
                return PROJ_B[rs, cs.start - 8192:cs.stop - 8192]
            assert cs.stop <= 8192
            return PROJ_A[rs, cs]
    PROJ = _Proj()
    XBC = dscr("XBC", [NT, 3072])
    DT = dscr("DT", [NT, 64])
    U = dscr("U", [NT, 4096])
    YP = dscr("YP", [NT, D])
    XR = dscr("XR", [NT, D])
    MOD = dscr("MOD", [2, ADA])
    XM = dscr("XM", [NT, D])
    COMB = dscr("COMB", [NT, 16])

    es = ExitStack()
    with es:
        arena_t = es.enter_context(nc.sbuf_tensor("arena", [128, ARENA], F32))
        arena_r = es.enter_context(nc.sbuf_tensor("arena_r", [128, ARENA], F32R))
        psum = [es.enter_context(nc.psum_tensor("pb%d" % i, [128, 512], F32)) for i in range(8)]
        block = es.enter_context(nc.Block())
        P = Prog(nc, es, arena_t, arena_r, psum)

        cst = P.alloc(6, 128)
        P.load(cst, cst.f, consts)
        ident, Umat, UTmat, ones, maskF, maskB = [cst.f[:, i, :] for i in range(6)]
        modT = P.alloc(96, 2)
        AB = P.alloc(4, KD, 2)
        nwT = P.alloc(2, KD)
        mk = P.mark()
        zrow = P.alloc(2048)
        P.op('pool', lambda e: e.memset(zrow.f, 0.0), [], [zrow])
        for si in seqinfo:
            for rr in (si['pb'], si['pb'] + si['len'] + 1):
                P.store(zrow, PROJ_A[rr:rr + 1, :].rearrange("o (a b) -> (o a) b", a=4), zrow.f[0:4, :])
        P.reset(mk)

        def wtile_load(t, dst, src):
            P.load(t, dst, src, eng='pool')

        def make_hT(xt, which, ch, hT, col0, tmp, posB=None, hT32=None):
            r = 1 if ch['kind'] == 0 else 0
            Aap = AB.f[:, 2 * which, :, r]
            Bap = AB.f[:, 2 * which + 1, :, r]
            ss = tmp['ss']
            junk = tmp['junk']
            P.op('act', lambda e: e.activation(out=junk.f, in_=xt.f, func=AF.Square, accum_out=ss.f[:, 0:1]),
                 [xt], [junk, ss])
            P.ts('dve', ss.f[:, 1:2], ss.f[:, 0:1], 1.0 / D, EPS, ALU.mult, ALU.add, [ss], [ss])
            P.act(ss.f[:, 2:3], ss.f[:, 1:2], AF.Sqrt, [ss], [ss])
            P.op('dve', lambda e: e.reciprocal(out=ss.f[:, 3:4], in_=ss.f[:, 2:3]), [ss], [ss])
            P.op('act', lambda e: e.activation(out=junk.f, in_=xt.f, func=AF.Copy, scale=ss.f[:, 3:4]), [xt, ss], [junk])
            for q4 in range(4):
                pt = P.ps()
                for q in range(4):
                    k = q4 * 4 + q
                    P.tr(pt, pt.f[:, q * 128:(q + 1) * 128], junk.f[:, k * 128:(k + 1) * 128], ident, [junk, cst])
                for q in range(4):
                    k = q4 * 4 + q
                    in1 = posB.f[:, k, :] if posB is not None else bc(Bap[:, k:k + 1], [128, 128])
                    rds = [pt, AB] + ([posB] if posB is not None else [])
                    P.stt(hT.r[:, k, col0:col0 + 128], pt.f[:, q * 128:(q + 1) * 128], Aap[:, k:k + 1], in1,
                          ALU.mult, ALU.add, rds, [hT])
                    if hT32 is not None:
                        P.stt(hT32.f[:, k, :], pt.f[:, q * 128:(q + 1) * 128], Aap[:, k:k + 1], in1,
                              ALU.mult, ALU.add, rds, [hT32])

        for l in range(DEPTH):
            xsrc = x_in if l == 0 else XR
            mk = P.mark()
            sc = P.alloc(D)
            scT = P.allocr(KD, 2)
            modrow = P.alloc(ADA)
            badats = [P.alloc(512) for _ in range(2)]
            wr = [P.allocr(KD, 512) for _ in range(2)]
            P.load(sc, sc.f[0:2, :], c2)
            P.act(sc.f[0:2, :], sc.f[0:2, :], AF.Silu, [sc], [sc])
            pt = P.ps()
            for k in range(KD):
                P.tr(pt, pt.f[:, 2 * k:2 * k + 2], sc.f[0:2, k * 128:(k + 1) * 128], ident[0:2, 0:2], [sc, cst])
            P.copy('dve', scT.r, pt.f[:, 0:32].rearrange("p (k r) -> p k r", r=2), [pt], [scT])
            for nt in range(ADA // 512):
                w = wr[nt % 2]
                wtile_load(w, w.r, w_ada[l, :, nt * 512:(nt + 1) * 512].rearrange("(k p) n -> p k n", p=128))
                badat = badats[nt % 2]
                P.dma('sp', badat, [(badat.f[0:1, :], b_ada[l:l + 1, nt * 512:(nt + 1) * 512]),
                                    (badat.f[1:2, :], b_ada[l:l + 1, nt * 512:(nt + 1) * 512])], True)
                pt = P.ps()
                P.mm(pt, pt.f[0:2, :], [(scT.r[:, k, :], w.r[:, k, :]) for k in range(KD)], [scT, w])
                P.tt('dve', modrow.f[0:2, nt * 512:(nt + 1) * 512], pt.f[0:2, :], badat.f[0:2, :],
                     ALU.add, [pt, badat], [modrow])
            P.store(modrow, MOD, modrow.f[0:2, :])
            pt = P.ps()
            for c in range(96):
                P.tr(pt, pt.f[:, 2 * c:2 * c + 2], modrow.f[0:2, c * 128:(c + 1) * 128], ident[0:2, 0:2], [modrow, cst])
            P.copy('dve', modT.f, pt.f[:, 0:192].rearrange("p (c r) -> p c r", r=2), [pt], [modT])
            P.load(nwT, nwT.f[:, 0, :], norm_mix_w[l].rearrange("(k p) -> p k", p=128), slow=True)
            P.load(nwT, nwT.f[:, 1, :], norm_ffn_w[l].rearrange("(k p) -> p k", p=128), slow=True)
            for which, (isc, ish) in enumerate(((1, 0), (4, 3))):
                for r in range(2):
                    P.stt(AB.f[:, 2 * which, :, r], modT.f[:, isc * 16:(isc + 1) * 16, r], 1.0, nwT.f[:, which, :],
                          ALU.add, ALU.mult, [modT, nwT], [AB])
                    P.copy('dve', AB.f[:, 2 * which + 1, :, r], modT.f[:, ish * 16:(ish + 1) * 16, r], [modT], [AB])
            P.reset(mk)

            mk = P.mark()
            TS = 4
            hT = P.allocr(KD, TS * 128)
            xts = [P.alloc(D) for _ in range(2)]
            tmp = dict(ss=P.alloc(8), junk=P.alloc(D))
            posb = P.alloc(KD, 128)
            wts = [P.allocr(KD, 512) for _ in range(2)]
            outs = [P.alloc(512) for _ in range(4)]
            oi = 0
            wi = 0
            for g0 in range(0, NCH, TS):
                grp = chunks[g0:g0 + TS]
                for j, ch in enumerate(grp):
                    xt = xts[j % 2]
                    P.load(xt, xt.f, xsrc[ch['t0']:ch['t0'] + 128, :])
                    pB = None
                    if ch['kind'] == 0:
                        P.load(posb, posb.f, posT[:, ch['p0']:ch['p0'] + 128].rearrange("(k p) t -> p k t", p=128))
                        P.tt('pool', posb.f, posb.f, bc(AB.f[:, 1, :, 1:2], [128, KD, 128]), ALU.add, [posb, AB], [posb])
                        pB = posb
                    make_hT(xt, 0, ch, hT, j * 128, tmp, posB=pB)
                ntile = (IN_DIM + 511) // 512
                for nt in range(ntile):
                    ncol = min(512, IN_DIM - nt * 512)
                    w = wts[wi % 2]
                    wi += 1
                    wtile_load(w, w.r[:, :, 0:ncol], w_in[l, :, nt * 512:nt * 512 + ncol].rearrange("(k p) n -> p k n", p=128))
                    for j, ch in enumerate(grp):
                        pt = P.ps()
                        P.mm(pt, pt.f[:, 0:ncol], [(hT.r[:, k, j * 128:(j + 1) * 128], w.r[:, k, 0:ncol]) for k in range(KD)],
                             [hT, w])
                        o = outs[oi % 4]
                        oi += 1
                        P.copy('act' if oi % 2 else 'dve', o.f[:, 0:ncol], pt.f[:, 0:ncol], [pt], [o])
                        P.store(o, PROJ[ch['pr']:ch['pr'] + 128, nt * 512:nt * 512 + ncol], o.f[:, 0:ncol])
            P.reset(mk)
            if cfg.get('stop') == 'proj':
                break

            mk = P.mark()
            cw = P.alloc(3, 3072)
            cb = P.alloc(3072)
            dtb = P.alloc(64)
            dtts = [P.alloc(64) for _ in range(2)]
            tins = [[P.alloc(1536) for _ in range(3)] for _ in range(2)]
            P.load(cw, cw.f, ssd_conv_w[l].partition_broadcast(128))
            P.load(cb, cb.f, ssd_conv_b[l].partition_broadcast(128))
            P.load(dtb, dtb.f, ssd_dt_bias[l].partition_broadcast(128))
            ri = 0
            for cidx, ch in enumerate(chunks):
                pr, t0 = ch['pr'], ch['t0']
                for hf in range(2):
                    tin = tins[ri % 2]
                    ri += 1
                    cs = slice(hf * 1536, (hf + 1) * 1536)
                    for s3 in range(3):
                        P.load(tin[s3], tin[s3].f, PROJ[pr - 1 + s3:pr - 1 + s3 + 128, C_XBC + hf * 1536:C_XBC + (hf + 1) * 1536])
                    P.tt('pool', tin[0].f, tin[0].f, cw.f[:, 0, cs], ALU.mult, [tin[0], cw], [tin[0]])
                    P.tt('dve', tin[1].f, tin[1].f, cw.f[:, 1, cs], ALU.mult, [tin[1], cw], [tin[1]])
                    P.tt('pool', tin[2].f, tin[2].f, cw.f[:, 2, cs], ALU.mult, [tin[2], cw], [tin[2]])
                    P.tt('dve', tin[1].f, tin[1].f, cb.f[:, cs], ALU.add, [tin[1], cb], [tin[1]])
                    P.tt('dve', tin[0].f, tin[0].f, tin[1].f, ALU.add, [tin[0], tin[1]], [tin[0]])
                    P.tt('dve', tin[0].f, tin[0].f, tin[2].f, ALU.add, [tin[0], tin[2]], [tin[0]])
                    P.act(tin[1].f, tin[0].f, AF.Silu, [tin[0]], [tin[1]])
                    P.store(tin[1], XBC[t0:t0 + 128, cs], tin[1].f)
                dtt = dtts[cidx % 2]
                P.load(dtt, dtt.f, PROJ[pr:pr + 128, C_DT:C_DT + 64])
                P.tt('dve', dtt.f, dtt.f, dtb.f, ALU.add, [dtt, dtb], [dtt])
                P.act(dtt.f, dtt.f, AF.Exp, [dtt], [dtt])
                P.ts('dve', dtt.f, dtt.f, 1.0, None, ALU.add, None, [dtt], [dtt])
                P.act(dtt.f, dtt.f, AF.Ln, [dtt], [dtt])
                P.store(dtt, DT[t0:t0 + 128, :], dtt.f)
            P.reset(mk)

            mk = P.mark()
            caw = P.alloc(3, 1024)
            tabs = [P.alloc(1024) for _ in range(2)]
            tchs = [[P.alloc(2048) for _ in range(3)] for _ in range(2)]
            P.load(caw, caw.f, conv_a_w[l].partition_broadcast(128))
            for cidx, ch in enumerate(chunks):
                pr, t0 = ch['pr'], ch['t0']
                tab = tabs[cidx % 2]
                tch = tchs[cidx % 2]
                P.load(tab, tab.f, PROJ[pr:pr + 128, C_AB:C_AB + 1024])
                for s3 in range(3):
                    t = tch[s3]
                    P.load(t, t.f, PROJ[pr - 1 + s3:pr - 1 + s3 + 128, C_AC:C_AC + 2048])
                    e1 = 'pool' if s3 != 1 else 'dve'
                    P.tt(e1, t.f[:, 0:1024], t.f[:, 0:1024], t.f[:, 1024:2048], ALU.mult, [t], [t])
                    P.tt(e1, t.f[:, 0:1024], t.f[:, 0:1024], caw.f[:, s3, :], ALU.mult, [t, caw], [t])
                a0 = tch[0]
                P.tt('dve', a0.f[:, 0:1024], a0.f[:, 0:1024], tch[1].f[:, 0:1024], ALU.add, [a0, tch[1]], [a0])
                P.tt('dve', a0.f[:, 0:1024], a0.f[:, 0:1024], tch[2].f[:, 0:1024], ALU.add, [a0, tch[2]], [a0])
                P.tt('dve', a0.f[:, 0:1024], a0.f[:, 0:1024], tab.f, ALU.mult, [a0, tab], [a0])
                P.store(a0, U[t0:t0 + 128, 0:1024], a0.f[:, 0:1024])
            P.reset(mk)

            mk = P.mark()
            lng = P.alloc(1024)
            lnb = P.alloc(1024)
            wT = P.alloc(8, 128)
            wtmp = P.alloc(8, 128)
            sgb = P.alloc(8)
            c3sets = [(P.alloc(2048), P.alloc(2048), P.alloc(1024), P.alloc(8), P.alloc(1024)) for _ in range(2)]
            P.load(lng, lng.f, sgu_ln_g[l].partition_broadcast(128))
            P.load(lnb, lnb.f, sgu_ln_b[l].partition_broadcast(128))
            P.load(sgb, sgb.f, sgu_b[l])
            P.load(wtmp, wtmp.f, sgu_w[l].rearrange("g i j -> i g j"))
            for hf in range(2):
                pt = P.ps()
                for q in range(4):
                    P.tr(pt, pt.f[:, q * 128:(q + 1) * 128], wtmp.f[:, hf * 4 + q, :], ident, [wtmp, cst])
                P.copy('dve', wT.f[:, hf * 4:(hf + 1) * 4, :], pt.f.rearrange("p (a b) -> p a b", a=4), [pt], [wT])
            for cidx, ch in enumerate(chunks):
                pr, t0 = ch['pr'], ch['t0']
                suv, gt_, junkc3, stc3, uc = c3sets[cidx % 2]
                P.load(suv, suv.f, PROJ[pr:pr + 128, C_SU:C_SU + 2048])
                P.tt('pool', gt_.f, suv.f, suv.f, ALU.mult, [suv], [gt_])
                P.ts('dve', gt_.f, gt_.f, 0.044715, 1.0, ALU.mult, ALU.add, [gt_], [gt_])
                P.tt('dve', gt_.f, gt_.f, suv.f, ALU.mult, [gt_, suv], [gt_])
                P.act(gt_.f, gt_.f, AF.Sigmoid, [gt_], [gt_], scale=1.5957691216057308)
                P.tt('dve', suv.f, suv.f, gt_.f, ALU.mult, [suv, gt_], [suv])
                u_ = suv.f[:, 0:1024]
                v_ = suv.f[:, 1024:2048]
                P.op('dve', lambda e, v_=v_, stc3=stc3: e.tensor_reduce(out=stc3.f[:, 0:1], in_=v_, axis=mybir.AxisListType.X, op=ALU.add),
                     [suv], [stc3])
                P.ts('dve', stc3.f[:, 1:2], stc3.f[:, 0:1], -1.0 / 1024, None, ALU.mult, None, [stc3], [stc3])
                P.ts('dve', v_, v_, stc3.f[:, 1:2], None, ALU.add, None, [suv, stc3], [suv])
                P.op('act', lambda e, v_=v_, stc3=stc3, junkc3=junkc3: e.activation(out=junkc3.f, in_=v_, func=AF.Square, accum_out=stc3.f[:, 2:3]),
                     [suv], [junkc3, stc3])
                P.ts('dve', stc3.f[:, 3:4], stc3.f[:, 2:3], 1.0 / 1024, EPS, ALU.mult, ALU.add, [stc3], [stc3])
                P.act(stc3.f[:, 4:5], stc3.f[:, 3:4], AF.Sqrt, [stc3], [stc3])
                P.op('dve', lambda e, stc3=stc3: e.reciprocal(out=stc3.f[:, 5:6], in_=stc3.f[:, 4:5]), [stc3], [stc3])
                P.stt(v_, v_, stc3.f[:, 5:6], lng.f, ALU.mult, ALU.mult, [suv, stc3, lng], [suv])
                P.tt('dve', v_, v_, lnb.f, ALU.add, [suv, lnb], [suv])
                for hf in range(2):
                    pt = P.ps()
                    for q in range(4):
                        g = hf * 4 + q
                        P.mm(pt, pt.f[:, q * 128:(q + 1) * 128], [(wT.f[:, g, :], suv.f[:, 1024 + g * 128:1024 + (g + 1) * 128])],
                             [wT, suv])
                    for q in range(4):
                        g = hf * 4 + q
                        P.stt(uc.f[:, g * 128:(g + 1) * 128], pt.f[:, q * 128:(q + 1) * 128], sgb.f[:, g:g + 1],
                              suv.f[:, g * 128:(g + 1) * 128], ALU.add, ALU.mult, [pt, sgb, suv], [uc])
                P.store(uc, U[t0:t0 + 128, 3072:4096], uc.f)
            P.reset(mk)
            if cfg.get('stop') == 'c3':
                break

            li = 0
            for dr in range(2):
                mk = P.mark()
                NB = 2 if dr == 0 else 1
                S = P.alloc(2048)
                xw = P.alloc(2048)
                yt = P.alloc(2048)
                if dr == 1:
                    zt = P.alloc(2048)
                    ypt = P.alloc(2048)
                    nwbc = P.alloc(2048)
                    P.load(nwbc, nwbc.f, ssd_norm_w[l].partition_broadcast(128))
                Rs = [P.alloc(4, 128) for _ in range(2)]
                Qs = [P.alloc(4, 128) for _ in range(2)]
                Lts = [P.alloc(4, 128) for _ in range(2)]
                WTs = [P.alloc(4, 128) for _ in range(2)]
                csets = [dict(xbc=P.alloc(3072), BT=P.alloc(4, 128), CT=P.alloc(4, 128), ST=P.alloc(4, 128), dts=P.alloc(32),
                              da=P.alloc(32), PT=P.alloc(64), lnw=P.alloc(32), biasL=P.alloc(32), eP=P.alloc(32),
                              etot=P.alloc(32), wdec=P.alloc(32)) for _ in range(NB)]
                abc = P.alloc(2, 32)
                Dbc = P.alloc(32)
                st4 = P.alloc(16)
                P.load(abc, abc.f, ssd_a_log[l].partition_broadcast(128))
                P.act(abc.f, abc.f, AF.Exp, [abc], [abc])
                P.ts('dve', abc.f, abc.f, -1.0, None, ALU.mult, None, [abc], [abc])
                P.load(Dbc, Dbc.f, ssd_d[l].partition_broadcast(128))
                cnum = 0
                cum = Umat if dr == 0 else UTmat
                msk = maskF if dr == 0 else maskB
                for si in seqinfo:
                    if si['kind'] == 0:
                        P.load(xw, xw.f.rearrange("p (k n) -> p k n", k=16), h0[l, dr].rearrange("(k p) n -> p k n", p=128))
                        for q4 in range(4):
                            pt = P.ps()
                            for q in range(4):
                                k = q4 * 4 + q
                                P.tr(pt, pt.f[:, q * 128:(q + 1) * 128], xw.f[:, k * 128:(k + 1) * 128], ident, [xw, cst])
                            P.copy('dve', S.f[:, q4 * 512:(q4 + 1) * 512], pt.f, [pt], [S])
                    else:
                        P.op('pool', lambda e, S=S: e.memset(S.f, 0.0), [], [S])
                    order = list(range(si['n'])) if dr == 0 else list(range(si['n'] - 1, -1, -1))
                    for ci in order:
                        ch = chunks[si['first'] + ci]
                        pr, t0 = ch['pr'], ch['t0']
                        cs_ = csets[cnum % NB]
                        cnum += 1
                        xbc, BT, CT, ST, dts, da, PT = cs_['xbc'], cs_['BT'], cs_['CT'], cs_['ST'], cs_['dts'], cs_['da'], cs_['PT']
                        lnw, biasL, eP, etot, wdec = cs_['lnw'], cs_['biasL'], cs_['eP'], cs_['etot'], cs_['wdec']
                        P.load(xbc, xbc.f, XBC[t0:t0 + 128, :])
                        P.load(dts, dts.f, DT[t0:t0 + 128, dr * 32:(dr + 1) * 32])
                        if dr == 1:
                            P.load(zt, zt.f, PROJ[pr:pr + 128, C_Z:C_Z + 2048])
                            P.load(ypt, ypt.f, YP[t0:t0 + 128, :])
                        P.tt('dve', da.f, dts.f, abc.f[:, dr, :], ALU.mult, [dts, abc], [da])
                        pt0 = P.ps()
                        P.mm(pt0, pt0.f[:, 0:32], [(cum, da.f)], [cst, da])
                        P.mm(pt0, pt0.f[:, 32:64], [(ones, da.f)], [cst, da])
                        P.copy('dve', PT.f, pt0.f[:, 0:64], [pt0], [PT])
                        P.act(lnw.f, dts.f, AF.Ln, [dts], [lnw])
                        P.tt('dve', biasL.f, lnw.f, PT.f[:, 0:32], ALU.subtract, [lnw, PT], [biasL])
                        P.act(eP.f, PT.f[:, 0:32], AF.Exp, [PT], [eP])
                        P.act(etot.f, PT.f[:, 32:64], AF.Exp, [PT], [etot])
                        P.tt('dve', wdec.f, biasL.f, PT.f[:, 32:64], ALU.add, [biasL, PT], [wdec])
                        P.act(wdec.f, wdec.f, AF.Exp, [wdec], [wdec])
                        ptb = P.ps()
                        ptc = P.ps()
                        for g in range(4):
                            P.tr(ptb, ptb.f[:, g * 128:(g + 1) * 128], xbc.f[:, 2048 + g * 128:2048 + (g + 1) * 128], ident, [xbc, cst])
                            P.tr(ptc, ptc.f[:, g * 128:(g + 1) * 128], xbc.f[:, 2560 + g * 128:2560 + (g + 1) * 128], ident, [xbc, cst])
                        P.copy('act', BT.f, ptb.f.rearrange("p (a b) -> p a b", a=4), [ptb], [BT])
                        P.copy('dve', CT.f, ptc.f.rearrange("p (a b) -> p a b", a=4), [ptc], [CT])
                        pts = P.ps()
                        for g in range(4):
                            P.mm(pts, pts.f[:, g * 128:(g + 1) * 128], [(BT.f[:, g, :], CT.f[:, g, :])], [BT, CT])
                        P.copy('act', ST.f, pts.f.rearrange("p (a b) -> p a b", a=4), [pts], [ST])
                        ptys = {}
                        ptos = {}
                        stageA = {}

                        def emitA(b):
                            nonlocal li
                            g, hh = b // 2, b % 2
                            if hh == 0:
                                ptys[g] = P.ps()
                                ptos[g] = P.ps()
                                P.mm(ptos[g], ptos[g].f, [(CT.f[:, g, :], S.f[:, g * 512:(g + 1) * 512])], [CT, S])
                            hb = g * 8 + hh * 4
                            R_, Q_, Lt, WT = Rs[li % 2], Qs[li % 2], Lts[li % 2], WTs[li % 2]
                            li += 1
                            P.tt('pool', R_.f, bc(cum.unsqueeze(1), [128, 4, 128]), bc(da.f[:, hb:hb + 4].unsqueeze(2), [128, 4, 128]),
                                 ALU.mult, [cst, da], [R_])
                            P.tt('pool', Q_.f, bc(msk.unsqueeze(1), [128, 4, 128]), bc(biasL.f[:, hb:hb + 4].unsqueeze(2), [128, 4, 128]),
                                 ALU.add, [cst, biasL], [Q_])
                            ptl = P.ps()
                            P.mm(ptl, ptl.f, [(ones, R_.f.rearrange("p a b -> p (a b)")), (ident, Q_.f.rearrange("p a b -> p (a b)"))],
                                 [cst, R_, Q_])
                            P.act(Lt.f, ptl.f.rearrange("p (a b) -> p a b", a=4), AF.Exp, [ptl], [Lt])
                            P.tt('dve', WT.f, Lt.f, bc(ST.f[:, g:g + 1, :], [128, 4, 128]), ALU.mult, [Lt, ST], [WT])
                            stageA[b] = WT

                        def emitB(b):
                            g, hh = b // 2, b % 2
                            hb = g * 8 + hh * 4
                            WT = stageA[b]
                            pty = ptys[g]
                            for h4 in range(4):
                                hd = hb + h4
                                c0 = (hh * 4 + h4) * 64
                                P.mm(pty, pty.f[:, c0:c0 + 64], [(WT.f[:, h4, :], xbc.f[:, hd * 64:(hd + 1) * 64])], [WT, xbc])
                            if hh == 1:
                                pto = ptos[g]
                                yg = yt.f[:, g * 512:(g + 1) * 512]
                                P.tt('dve', yg.rearrange("p (a b) -> p a b", a=8), pto.f.rearrange("p (a b) -> p a b", a=8),
                                     bc(eP.f[:, g * 8:(g + 1) * 8].unsqueeze(2), [128, 8, 64]), ALU.mult, [pto, eP], [yt])
                                P.tt('dve', yg, yg, pty.f, ALU.add, [yt, pty], [yt])
                        emitA(0)
                        for b in range(8):
                            if b + 1 < 8:
                                emitA(b + 1)
                            emitB(b)
                        P.tt('pool', xw.f.rearrange("p (a b) -> p a b", a=32), xbc.f[:, 0:2048].rearrange("p (a b) -> p a b", a=32),
                             bc(wdec.f.unsqueeze(2), [128, 32, 64]), ALU.mult, [xbc, wdec], [xw])
                        for g in range(4):
                            ptg = P.ps()
                            P.mm(ptg, ptg.f, [(xbc.f[:, 2048 + g * 128:2048 + (g + 1) * 128], xw.f[:, g * 512:(g + 1) * 512])], [xbc, xw])
                            Sg = S.f[:, g * 512:(g + 1) * 512]
                            P.tt('dve', Sg.rearrange("p (a b) -> p a b", a=8), Sg.rearrange("p (a b) -> p a b", a=8),
                                 bc(etot.f[:, g * 8:(g + 1) * 8].unsqueeze(2), [128, 8, 64]), ALU.mult, [S, etot], [S])
                            P.tt('dve', Sg, Sg, ptg.f, ALU.add, [S, ptg], [S])
                        if dr == 0:
                            P.store(yt, YP[t0:t0 + 128, :], yt.f)
                        else:
                            P.tt('dve', yt.f, yt.f, ypt.f, ALU.add, [yt, ypt], [yt])
                            P.tt('pool', ypt.f.rearrange("p (a b) -> p a b", a=32), xbc.f[:, 0:2048].rearrange("p (a b) -> p a b", a=32),
                                 bc(Dbc.f.unsqueeze(2), [128, 32, 64]), ALU.mult, [xbc, Dbc], [ypt])
                            P.tt('dve', yt.f, yt.f, ypt.f, ALU.add, [yt, ypt], [yt])
                            P.act(zt.f, zt.f, AF.Silu, [zt], [zt])
                            P.tt('dve', yt.f, yt.f, zt.f, ALU.mult, [yt, zt], [yt])
                            for g in range(4):
                                P.op('act', lambda e, g=g, zt=zt, yt=yt, st4=st4: e.activation(out=zt.f[:, g * 512:(g + 1) * 512], in_=yt.f[:, g * 512:(g + 1) * 512],
                                                                       func=AF.Square, accum_out=st4.f[:, g:g + 1]), [yt], [zt, st4])
                            P.ts('dve', st4.f[:, 4:8], st4.f[:, 0:4], 1.0 / 512, EPS, ALU.mult, ALU.add, [st4], [st4])
                            P.act(st4.f[:, 8:12], st4.f[:, 4:8], AF.Sqrt, [st4], [st4])
                            P.op('dve', lambda e, st4=st4: e.reciprocal(out=st4.f[:, 12:16], in_=st4.f[:, 8:12]), [st4], [st4])
                            for g in range(4):
                                P.stt(yt.f[:, g * 512:(g + 1) * 512], yt.f[:, g * 512:(g + 1) * 512], st4.f[:, 12 + g:13 + g],
                                      nwbc.f[:, g * 512:(g + 1) * 512], ALU.mult, ALU.mult, [yt, st4, nwbc], [yt])
                            P.store(yt, U[t0:t0 + 128, 1024:3072], yt.f)
                    if si['kind'] == 1:
                        for half in range(2):
                            for q in range(4):
                                pt = P.ps()
                                for h4 in range(4):
                                    hd = half * 16 + q * 4 + h4
                                    P.tr(pt, pt.f[0:64, h4 * 128:(h4 + 1) * 128], S.f[:, hd * 64:(hd + 1) * 64], ident, [S, cst])
                                P.copy('dve', xw.f[0:64, q * 512:(q + 1) * 512], pt.f[0:64, :], [pt], [xw])
                            P.store(xw, st_out[si['pri'], l, dr, half * 16:(half + 1) * 16].rearrange("h p n -> p h n"),
                                    xw.f[0:64, :].rearrange("p (h n) -> p h n", h=16))
                P.reset(mk)
            if cfg.get('stop') == 'ssd':
                break

            mk = P.mark()
            TDN = 4
            UT = P.allocr(32, TDN * 128)
            mT = UT
            wds = [P.allocr(KD, 256) for _ in range(2)]
            utile = P.alloc(4096)
            merged = [P.alloc(2048) for _ in range(TDN)]
            xch = [P.alloc(2048) for _ in range(TDN)]
            gts = [P.alloc(256) for _ in range(2)]
            g1s = [P.alloc(256) for _ in range(2)]
            wi = 0
            gi_ = 0
            ci_ = 0
            for g0 in range(0, NCH, TDN):
                grp = chunks[g0:g0 + TDN]
                for j, ch in enumerate(grp):
                    P.load(utile, utile.f, U[ch['t0']:ch['t0'] + 128, :])
                    P.load(xch[j], xch[j].f, xsrc[ch['t0']:ch['t0'] + 128, :])
                    for q4 in range(8):
                        pt = P.ps()
                        for q in range(4):
                            k = q4 * 4 + q
                            P.tr(pt, pt.f[:, q * 128:(q + 1) * 128], utile.f[:, k * 128:(k + 1) * 128], ident, [utile, cst])
                        ci_ += 1
                        P.copy('act' if ci_ % 2 else 'dve', UT.r[:, q4 * 4:(q4 + 1) * 4, j * 128:(j + 1) * 128],
                               pt.f.rearrange("p (a b) -> p a b", a=4), [pt], [UT])
                for bi, (wsrc, k0, KC, gcol) in enumerate(((w_out_a, 0, 8, 0), (w_out_b, 8, 16, 2048), (w_out_c, 24, 8, 4096))):
                    for nt in range(8):
                        w = wds[wi % 2]
                        wi += 1
                        wtile_load(w, w.r[:, 0:KC, :], wsrc[l, :, nt * 256:(nt + 1) * 256].rearrange("(k p) n -> p k n", p=128))
                        for j, ch in enumerate(grp):
                            pt = P.ps()
                            P.mm(pt, pt.f[:, 0:256], [(UT.r[:, k0 + k, j * 128:(j + 1) * 128], w.r[:, k, :]) for k in range(KC)], [UT, w])
                            gt = gts[gi_ % 2]
                            gi_ += 1
                            c0 = C_GATE + gcol + nt * 256
                            P.load(gt, gt.f, PROJ[ch['pr']:ch['pr'] + 128, c0:c0 + 256])
                            P.act(gt.f, gt.f, AF.Sigmoid, [gt], [gt])
                            mg = merged[j].f[:, nt * 256:(nt + 1) * 256]
                            if bi == 0:
                                P.tt('dve', mg, gt.f, pt.f[:, 0:256], ALU.mult, [gt, pt], [merged[j]])
                            else:
                                P.tt('dve', gt.f, gt.f, pt.f[:, 0:256], ALU.mult, [gt, pt], [gt])
                                P.tt('dve', mg, mg, gt.f, ALU.add, [merged[j], gt], [merged[j]])
                for j, ch in enumerate(grp):
                    for q4 in range(4):
                        pt = P.ps()
                        for q in range(4):
                            k = q4 * 4 + q
                            P.tr(pt, pt.f[:, q * 128:(q + 1) * 128], merged[j].f[:, k * 128:(k + 1) * 128], ident, [merged[j], cst])
                        ci_ += 1
                        P.copy('act' if ci_ % 2 else 'dve', mT.r[:, q4 * 4:(q4 + 1) * 4, j * 128:(j + 1) * 128],
                               pt.f.rearrange("p (a b) -> p a b", a=4), [pt], [mT])
                for nt in range(8):
                    w = wds[wi % 2]
                    wi += 1
                    wtile_load(w, w.r, w_o[l, :, nt * 256:(nt + 1) * 256].rearrange("(k p) n -> p k n", p=128))
                    for j, ch in enumerate(grp):
                        r = 1 if ch['kind'] == 0 else 0
                        pt = P.ps()
                        P.mm(pt, pt.f[:, 0:256], [(mT.r[:, k, j * 128:(j + 1) * 128], w.r[:, k, :]) for k in range(KD)], [mT, w])
                        g1 = g1s[gi_ % 2]
                        gi_ += 1
                        P.load(g1, g1.f, MOD[r, 2 * D + nt * 256:2 * D + (nt + 1) * 256].partition_broadcast(128))
                        P.tt('dve', g1.f, g1.f, pt.f[:, 0:256], ALU.mult, [g1, pt], [g1])
                        xs_ = xch[j].f[:, nt * 256:(nt + 1) * 256]
                        P.tt('dve', xs_, xs_, g1.f, ALU.add, [xch[j], g1], [xch[j]])
                for j, ch in enumerate(grp):
                    P.store(xch[j], XM[ch['t0']:ch['t0'] + 128, :], xch[j].f)
            P.reset(mk)
            if cfg.get('stop') == 'mix':
                break

            mk = P.mark()
            TE = 4
            last = (l == DEPTH - 1)
            h2T = P.allocr(KD, TE * 128)
            actT = P.allocr(4, TE * 128)
            wgu = [[P.allocr(KD, 128) for _ in range(2)] for _ in range(2)]
            wdn = [P.allocr(4, 512) for _ in range(2)]
            xe = [P.alloc(2048) for _ in range(TE)]
            acc = [P.alloc(2048) for _ in range(TE)]
            tmp = dict(ss=P.alloc(8), junk=P.alloc(D))
            h32 = P.alloc(KD, 128)
            sgs = [P.alloc(TE * 128) for _ in range(2)]
            rw = P.alloc(KD, 20)
            rbb = P.alloc(20)
            lg = P.alloc(20)
            rs = P.alloc(64)
            combs = [P.alloc(16) for _ in range(TE)]
            g2s = [P.alloc(512) for _ in range(2)]
            P.load(rw, rw.f, router_w[l].rearrange("(k p) n -> p k n", p=128))
            P.load(rbb, rbb.f, router_b[l].partition_broadcast(128))
            ui = 0
            di = 0
            si_ = 0
            g2i = 0
            for g0 in range(0, NCH, TE):
                grp = chunks[g0:g0 + TE]
                nj = len(grp)
                NTK = nj * 128
                for j, ch in enumerate(grp):
                    P.load(xe[j], xe[j].f, XM[ch['t0']:ch['t0'] + 128, :])
                    make_hT(xe[j], 1, ch, h2T, j * 128, tmp, posB=None, hT32=h32)
                    pt = P.ps()
                    P.mm(pt, pt.f[:, 0:20], [(h32.f[:, k, :], rw.f[:, k, :]) for k in range(KD)], [h32, rw])
                    P.tt('dve', lg.f, pt.f[:, 0:20], rbb.f, ALU.add, [pt, rbb], [lg])
                    R_ = rs.f
                    cb_ = combs[j]
                    P.op('dve', lambda e, R_=R_: e.tensor_reduce(out=R_[:, 0:1], in_=lg.f[:, 0:4], axis=mybir.AxisListType.X, op=ALU.max), [lg], [rs])
                    P.ts('dve', R_[:, 1:2], R_[:, 0:1], -1.0, None, ALU.mult, None, [rs], [rs])
                    P.op('act', lambda e, R_=R_: e.activation(out=R_[:, 4:8], in_=lg.f[:, 0:4], func=AF.Exp, bias=R_[:, 1:2], scale=1.0,
                                                            accum_out=R_[:, 2:3]), [lg, rs], [rs])
                    P.op('dve', lambda e, R_=R_: e.reciprocal(out=R_[:, 3:4], in_=R_[:, 2:3]), [rs], [rs])
                    P.ts('dve', R_[:, 8:12], lg.f[:, 0:4], R_[:, 0:1], None, ALU.is_equal, None, [lg, rs], [rs])
                    P.tt('dve', R_[:, 16:32].rearrange("p (g k) -> p g k", g=4), lg.f[:, 4:20].rearrange("p (g k) -> p g k", g=4),
                         bc(R_[:, 8:12].unsqueeze(2), [128, 4, 4]), ALU.mult, [lg, rs], [rs])
                    P.op('dve', lambda e, R_=R_: e.tensor_reduce(out=R_[:, 12:16], in_=R_[:, 16:32].rearrange("p (g k) -> p k g", g=4),
                                                               axis=mybir.AxisListType.X, op=ALU.add), [rs], [rs])
                    P.op('dve', lambda e, R_=R_: e.tensor_reduce(out=R_[:, 32:33], in_=R_[:, 12:16], axis=mybir.AxisListType.X, op=ALU.max), [rs], [rs])
                    P.ts('dve', R_[:, 33:34], R_[:, 32:33], -1.0, None, ALU.mult, None, [rs], [rs])
                    P.op('act', lambda e, R_=R_: e.activation(out=R_[:, 36:40], in_=R_[:, 12:16], func=AF.Exp, bias=R_[:, 33:34], scale=1.0), [rs], [rs])
                    P.ts('dve', R_[:, 40:44], R_[:, 12:16], R_[:, 32:33], None, ALU.is_equal, None, [rs], [rs])
                    P.stt(R_[:, 44:48], R_[:, 40:44], -2.0, R_[:, 36:40], ALU.mult, ALU.add, [rs], [rs])
                    P.op('dve', lambda e, R_=R_: e.tensor_reduce(out=R_[:, 34:35], in_=R_[:, 44:48], axis=mybir.AxisListType.X, op=ALU.max), [rs], [rs])
                    P.ts('dve', R_[:, 48:52], R_[:, 44:48], R_[:, 34:35], None, ALU.is_equal, None, [rs], [rs])
                    P.ts('dve', R_[:, 35:36], R_[:, 34:35], 1.0, None, ALU.add, None, [rs], [rs])
                    P.op('dve', lambda e, R_=R_: e.reciprocal(out=R_[:, 52:53], in_=R_[:, 35:36]), [rs], [rs])
                    P.tt('dve', R_[:, 53:54], R_[:, 52:53], R_[:, 3:4], ALU.mult, [rs], [rs])
                    P.tt('dve', R_[:, 54:55], R_[:, 53:54], R_[:, 34:35], ALU.mult, [rs], [rs])
                    P.ts('dve', R_[:, 56:60], R_[:, 40:44], R_[:, 53:54], None, ALU.mult, None, [rs], [rs])
                    P.stt(R_[:, 56:60], R_[:, 48:52], R_[:, 54:55], R_[:, 56:60], ALU.mult, ALU.add, [rs], [rs])
                    P.tt('dve', cb_.f.rearrange("p (g k) -> p g k", g=4), bc(R_[:, 8:12].unsqueeze(2), [128, 4, 4]),
                         bc(R_[:, 56:60].unsqueeze(1), [128, 4, 4]), ALU.mult, [rs], [cb_])
                    if DBG:
                        P.store(cb_, COMB[ch['t0']:ch['t0'] + 128, :], cb_.f)
                for ex in range(NE):
                    for fc in range(4):
                        wg, wu = wgu[ui % 2]
                        ui += 1
                        wtile_load(wg, wg.r, exp_w_gate[l, ex, :, fc * 128:(fc + 1) * 128].rearrange("(k p) f -> p k f", p=128))
                        wtile_load(wu, wu.r, exp_w_up[l, ex, :, fc * 128:(fc + 1) * 128].rearrange("(k p) f -> p k f", p=128))
                        ptg = P.ps()
                        P.mm(ptg, ptg.f[:, 0:NTK], [(wg.r[:, k, :], h2T.r[:, k, 0:NTK]) for k in range(KD)], [wg, h2T])
                        ptu = P.ps()
                        P.mm(ptu, ptu.f[:, 0:NTK], [(wu.r[:, k, :], h2T.r[:, k, 0:NTK]) for k in range(KD)], [wu, h2T])
                        sg = sgs[si_ % 2]
                        si_ += 1
                        P.act(sg.f[:, 0:NTK], ptg.f[:, 0:NTK], AF.Silu, [ptg], [sg])
                        P.tt('dve', actT.r[:, fc, 0:NTK], sg.f[:, 0:NTK], ptu.f[:, 0:NTK], ALU.mult, [sg, ptu], [actT])
                    for dq in range(4):
                        wd = wdn[di % 2]
                        di += 1
                        wtile_load(wd, wd.r, exp_w_down[l, ex, :, dq * 512:(dq + 1) * 512].rearrange("(c p) n -> p c n", p=128))
                        for j in range(nj):
                            pt = P.ps()
                            P.mm(pt, pt.f, [(actT.r[:, fc, j * 128:(j + 1) * 128], wd.r[:, fc, :]) for fc in range(4)], [actT, wd])
                            dst = acc[j].f[:, dq * 512:(dq + 1) * 512]
                            if ex == 0:
                                P.ts('dve', dst, pt.f, combs[j].f[:, ex:ex + 1], None, ALU.mult, None, [pt, combs[j]], [acc[j]])
                            else:
                                P.stt(dst, pt.f, combs[j].f[:, ex:ex + 1], dst, ALU.mult, ALU.add, [pt, combs[j], acc[j]], [acc[j]])
                for j, ch in enumerate(grp):
                    r = 1 if ch['kind'] == 0 else 0
                    for d4 in range(4):
                        g2 = g2s[g2i % 2]
                        g2i += 1
                        P.load(g2, g2.f, MOD[r, 5 * D + d4 * 512:5 * D + (d4 + 1) * 512].partition_broadcast(128))
                        sl = slice(d4 * 512, (d4 + 1) * 512)
                        P.tt('dve', g2.f, g2.f, acc[j].f[:, sl], ALU.mult, [g2, acc[j]], [g2])
                        P.tt('dve', xe[j].f[:, sl], xe[j].f[:, sl], g2.f, ALU.add, [xe[j], g2], [xe[j]])
                    if not last:
                        P.store(xe[j], XR[ch['t0']:ch['t0'] + 128, :], xe[j].f)
                    else:
                        if DBG:
                            P.store(xe[j], XR[ch['t0']:ch['t0'] + 128, :], xe[j].f)
                        ss = tmp['ss']
                        junk = tmp['junk']
                        P.op('act', lambda e, j=j: e.activation(out=junk.f, in_=xe[j].f, func=AF.Square, accum_out=ss.f[:, 0:1]),
                             [xe[j]], [junk, ss])
                        P.ts('dve', ss.f[:, 1:2], ss.f[:, 0:1], 1.0 / D, EPS, ALU.mult, ALU.add, [ss], [ss])
                        P.act(ss.f[:, 2:3], ss.f[:, 1:2], AF.Sqrt, [ss], [ss])
                        P.op('dve', lambda e: e.reciprocal(out=ss.f[:, 3:4], in_=ss.f[:, 2:3]), [ss], [ss])
                        for d4 in range(4):
                            g2 = g2s[g2i % 2]
                            g2i += 1
                            sl = slice(d4 * 512, (d4 + 1) * 512)
                            P.load(g2, g2.f, final_norm_w[0, sl].partition_broadcast(128))
                            P.stt(junk.f[:, sl], xe[j].f[:, sl], ss.f[:, 3:4], g2.f, ALU.mult, ALU.mult, [xe[j], ss, g2], [junk])
                        P.store(junk, y_out[ch['t0']:ch['t0'] + 128, :], junk.f)
            P.reset(mk)

        P.barrier()
        P.emit(block)
    return nc


def make_consts():
    k = np.arange(128)[:, None]
    i = np.arange(128)[None, :]
    c = np.zeros((128, 6, 128), np.float32)
    c[:, 0] = (k == i)
    c[:, 1] = (k <= i)
    c[:, 2] = (k >= i)
    c[:, 3] = 1.0
    c[:, 4] = np.where(k > i, -BIG, 0.0)
    c[:, 5] = np.where(k < i, -BIG, 0.0)
    return c


def make_posT(n_tok):
    rows = n_tok // 64
    rr, cc = np.meshgrid(np.arange(rows, dtype=np.float32), np.arange(64, dtype=np.float32), indexing='ij')
    quarter = D // 4
    omega = (1.0 / (np.float32(10000.0) ** (np.arange(quarter, dtype=np.float32) / np.float32(quarter)))).astype(np.float32)
    ar = rr.reshape(-1)[:, None] * omega
    ac = cc.reshape(-1)[:, None] * omega
    pos = np.concatenate([np.sin(ar), np.cos(ar), np.sin(ac), np.cos(ac)], axis=-1).astype(np.float32)
    return np.ascontiguousarray(pos.T)


def make_in_map(inp, sample_idx, prompt_idxs, sample_len=None):
    f = lambda a: np.ascontiguousarray(np.asarray(a, dtype=np.float32))
    xs = f(inp['x_sample'])[sample_idx]
    if sample_len is not None:
        xs = xs[:sample_len]
    xp = [f(inp['x_prompt'])[i] for i in prompt_idxs]
    m = {}
    m['x_in'] = np.ascontiguousarray(np.concatenate([xs] + xp, axis=0))
    m['c2'] = np.ascontiguousarray(np.stack([f(inp['c_ctx']), f(inp['c'])[sample_idx]], axis=0))
    m['h0'] = np.ascontiguousarray(f(inp['state_ssd'])[sample_idx].reshape(2, 2, H * HP, NS))
    m['posT'] = make_posT(xs.shape[0])
    m['consts'] = make_consts()
    for k in ['w_ada', 'b_ada', 'w_in', 'norm_mix_w', 'norm_ffn_w', 'conv_a_w', 'w_out_a', 'ssd_conv_w', 'ssd_conv_b',
              'ssd_a_log', 'ssd_d', 'ssd_norm_w', 'w_out_b', 'sgu_ln_g', 'sgu_ln_b', 'sgu_w', 'sgu_b', 'w_out_c', 'w_o',
              'exp_w_gate', 'exp_w_up', 'exp_w_down']:
        m[k] = f(inp[k])
    m['ssd_dt_bias'] = f(inp['ssd_dt_bias']).reshape(2, 2 * H)
    m['router_w'] = np.ascontiguousarray(np.concatenate([f(inp['router_g_w']), f(inp['router_e_w'])], axis=-1))
    m['router_b'] = np.ascontiguousarray(np.concatenate([f(inp['router_g_b']), f(inp['router_e_b'])], axis=-1))
    m['final_norm_w'] = f(inp['final_norm_w']).reshape(1, D)
    return m


_NC_CACHE = {}


def kernel(**inputs):
    seqs = [(4096, 0), (256, 1), (256, 1)]
    key = 'full'
    if key not in _NC_CACHE:
        _NC_CACHE[key] = build(dict(seqs=seqs, depth=2, debug=False))
    nc = _NC_CACHE[key]
    base = make_in_map(inputs, 0, [0, 1])
    in_maps = []
    xs_all = np.asarray(inputs['x_sample'], dtype=np.float32)
    xp_all = np.asarray(inputs['x_prompt'], dtype=np.float32)
    c_all = np.asarray(inputs['c'], dtype=np.float32)
    cctx = np.asarray(inputs['c_ctx'], dtype=np.float32)
    st_all = np.asarray(inputs['state_ssd'], dtype=np.float32)
    for core in range(8):
        si = core % 2
        m = dict(base)
        m['x_in'] = np.ascontiguousarray(np.concatenate([xs_all[si], xp_all[2 * core], xp_all[2 * core + 1]], axis=0))
        m['c2'] = np.ascontiguousarray(np.stack([cctx, c_all[si]], axis=0))
        m['h0'] = np.ascontiguousarray(st_all[si].reshape(2, 2, H * HP, NS))
        in_maps.append(m)
    res = run_bass_kernel_spmd(nc, in_maps, core_ids=list(range(8)))
    y_prompt = np.zeros((16, 256, D), np.float32)
    y_sample = np.zeros((2, 4096, D), np.float32)
    new_state = np.zeros((16, 2, 2, H, HP, NS), np.float32)
    for core in range(8):
        r = res.results[core]
        yo = np.asarray(r['y_out'])
        if core < 2:
            y_sample[core] = yo[0:4096]
        y_prompt[2 * core] = yo[4096:4352]
        y_prompt[2 * core + 1] = yo[4352:4608]
        so = np.asarray(r['st_out'])
        new_state[2 * core] = so[0]
        new_state[2 * core + 1] = so[1]
    return (y_prompt, y_sample, new_state)
```

```python
import numpy as np
from contextlib import ExitStack
import concourse.bass as bass
import concourse.mybir as mybir
from concourse.bass_utils import run_bass_kernel_spmd

F32 = mybir.dt.float32
F32R = mybir.dt.float32r
AF = mybir.ActivationFunctionType
ALU = mybir.AluOpType

D = 2048
KD = 16
IN_DIM = 16448
ADA = 6 * D
H = 32
HP = 64
NS = 128
G = 4
NE = 16
DE = 512
EPS = 1e-6
BIG = 30000.0
C_AB, C_AC, C_AH, C_Z, C_XBC, C_DT, C_SU, C_SV, C_GATE = 0, 1024, 2048, 3072, 5120, 8192, 8256, 9280, 10304
ARENA = 24 * 1024
NDS = 96


class Tile:
    def __init__(self, r, f):
        self.r = r
        self.f = f
        self.w = None
        self.rd = {}
        self.dsem = None


class Prog:
    ENG = ['pe', 'act', 'dve', 'pool', 'sp']

    def __init__(self, nc, es, arena, arena_r, psum):
        self.nc = nc
        self.q = {e: [] for e in self.ENG}
        self.csem = {e: es.enter_context(nc.semaphore('c_' + e)) for e in self.ENG}
        self.cnt = {e: 0 for e in self.ENG}
        self.dsems = [es.enter_context(nc.semaphore('d%d' % i)) for i in range(NDS)]
        self.dcnt = [0] * NDS
        self.dfree = list(range(NDS))
        self.seen = {e: {} for e in self.ENG}
        self.arena = arena
        self.arena_r = arena_r
        self.off = 0
        self.off_r = 0
        self.phase_tiles = []
        self.pbanks = [Tile(None, p[:]) for p in psum]
        self.pidx = 0

    def alloc(self, *shape, r=False):
        n = int(np.prod(shape))
        if r:
            assert self.off_r + n <= ARENA, ("arena_r overflow", self.off_r, n)
            r = self.arena_r[:, self.off_r:self.off_r + n]
            f = r.bitcast(F32)
            self.off_r += n
        else:
            assert self.off + n <= ARENA, ("arena overflow", self.off, n)
            f = self.arena[:, self.off:self.off + n]
            r = f
            self.off += n
        if len(shape) == 2:
            r = r.rearrange("p (a b) -> p a b", a=shape[0])
            f = f.rearrange("p (a b) -> p a b", a=shape[0])
        elif len(shape) == 3:
            r = r.rearrange("p (a b c) -> p a b c", a=shape[0], b=shape[1])
            f = f.rearrange("p (a b c) -> p a b c", a=shape[0], b=shape[1])
        t = Tile(r, f)
        self.phase_tiles.append(t)
        return t

    def allocr(self, *shape):
        return self.alloc(*shape, r=True)

    def mark(self):
        return (self.off, len(self.phase_tiles), self.off_r)

    def reset(self, mark):
        self.barrier()
        off, nt, off_r = mark
        for t in self.phase_tiles[nt:]:
            if t.dsem is not None:
                self.dfree.append(t.dsem)
        del self.phase_tiles[nt:]
        self.off = off
        self.off_r = off_r

    def ps(self):
        t = self.pbanks[self.pidx]
        self.pidx = (self.pidx + 1) % len(self.pbanks)
        return t

    def _semobj(self, k):
        return self.csem[k[1]] if k[0] == 'c' else self.dsems[k[1]]

    def _deps(self, eng, reads, writes):
        need = {}

        def add(ev):
            if ev is None:
                return
            k, v = ev
            if k == ('c', 'pe') and eng == 'pe':
                return
            if need.get(k, 0) < v:
                need[k] = v
        for t in reads:
            add(t.w)
        for t in writes:
            add(t.w)
            for k, v in t.rd.items():
                add((k, v))
        waits = []
        for k, v in need.items():
            if self.seen[eng].get(k, 0) >= v:
                continue
            self.seen[eng][k] = v
            waits.append((self._semobj(k), v))
        return waits

    def _commit(self, ev, reads, writes):
        k, v = ev
        for t in reads:
            if t.rd.get(k, 0) < v:
                t.rd[k] = v
        for t in writes:
            t.w = ev
            t.rd = {}

    def op(self, eng, fn, reads=(), writes=()):
        waits = self._deps(eng, reads, writes)
        self.cnt[eng] += 1
        ev = (('c', eng), self.cnt[eng])
        self._commit(ev, reads, writes)
        sem = self.csem[eng]

        def run(e):
            for s, v in waits:
                e.wait_ge(s, v)
            ins = fn(e)
            ins.then_inc(sem, 1)
        self.q[eng].append(run)

    def dma(self, eng, tile, pairs, write, slow=False):
        reads, writes = ((), (tile,)) if write else ((tile,), ())
        waits = self._deps(eng, reads, writes)
        if tile.dsem is None:
            tile.dsem = self.dfree.pop()
        i = tile.dsem
        self.dcnt[i] += 16 * len(pairs)
        ev = (('d', i), self.dcnt[i])
        self._commit(ev, reads, writes)
        sem = self.dsems[i]
        kw = dict(allow_slow_non_contiguous=True) if slow else {}

        def run(e):
            for s, v in waits:
                e.wait_ge(s, v)
            for (o, a) in pairs:
                e.dma_start(out=o, in_=a, **kw).then_inc(sem, 16)
        self.q[eng].append(run)

    def load(self, tile, out_ap, in_ap, eng='sp', slow=False):
        self.dma(eng, tile, [(out_ap, in_ap)], True, slow)

    def store(self, tile, out_ap, in_ap, eng='sp', slow=False):
        self.dma(eng, tile, [(out_ap, in_ap)], False, slow)

    def barrier(self):
        for e in self.ENG:
            waits = []
            for e2 in self.ENG:
                v = self.cnt[e2]
                k = ('c', e2)
                if v and self.seen[e].get(k, 0) < v:
                    self.seen[e][k] = v
                    waits.append((self.csem[e2], v))
            for i in range(NDS):
                v = self.dcnt[i]
                k = ('d', i)
                if v and self.seen[e].get(k, 0) < v:
                    self.seen[e][k] = v
                    waits.append((self.dsems[i], v))
            if waits:
                self.q[e].append(lambda eng, waits=waits: [eng.wait_ge(s, v) for s, v in waits])

    def emit(self, block):
        m = {'pe': block.tensor, 'act': block.scalar, 'dve': block.vector, 'pool': block.gpsimd, 'sp': block.sync}
        for en in self.ENG:
            def f(eng, en=en):
                for run in self.q[en]:
                    run(eng)
            m[en](f)

    def mm(self, pt, out, pairs, reads):
        n = len(pairs)

        def fn(e):
            ins = None
            for i, (l, r) in enumerate(pairs):
                ins = e.matmul(out, l, r, start=(i == 0), stop=(i == n - 1))
            return ins
        self.op('pe', fn, reads, [pt])

    def tr(self, pt, out, in_, ident, reads):
        self.op('pe', lambda e: e.transpose(out, in_, ident), reads, [pt])

    def act(self, out, in_, func, reads, writes, eng='act', **kw):
        self.op('act', lambda e: e.activation(out=out, in_=in_, func=func, **kw), reads, writes)

    def tt(self, eng, out, in0, in1, op, reads, writes):
        self.op(eng, lambda e: e.tensor_tensor(out=out, in0=in0, in1=in1, op=op), reads, writes)

    def ts(self, eng, out, in0, s1, s2, op0, op1, reads, writes):
        if s2 is None:
            self.op(eng, lambda e: e.tensor_scalar(out=out, in0=in0, scalar1=s1, scalar2=None, op0=op0), reads, writes)
        else:
            self.op(eng, lambda e: e.tensor_scalar(out=out, in0=in0, scalar1=s1, scalar2=s2, op0=op0, op1=op1), reads, writes)

    def stt(self, out, in0, scalar, in1, op0, op1, reads, writes):
        self.op('dve', lambda e: e.scalar_tensor_tensor(out=out, in0=in0, scalar=scalar, in1=in1, op0=op0, op1=op1),
                reads, writes)

    def copy(self, eng, out, in_, reads, writes):
        if eng == 'act':
            self.op('act', lambda e: e.activation(out=out, in_=in_, func=AF.Copy), reads, writes)
        else:
            self.op(eng, lambda e: e.tensor_copy(out=out, in_=in_), reads, writes)


def bc(ap, shape):
    return ap.to_broadcast(list(shape))


def build(cfg):
    SEQS = cfg['seqs']
    DEPTH = cfg.get('depth', 2)
    DBG = cfg.get('debug', False)
    NT = sum(l for l, _ in SEQS)
    LS = max([l for l, k in SEQS if k == 0] + [128])
    NPR = sum(1 for _, k in SEQS if k == 1)
    NPROW = NT + 2 * len(SEQS)
    chunks = []
    t0 = 0
    pb = 0
    seqinfo = []
    pri = 0
    for s, (l, k) in enumerate(SEQS):
        nci = l // 128
        first = len(chunks)
        for ci in range(nci):
            chunks.append(dict(s=s, ci=ci, t0=t0 + ci * 128, pr=pb + 1 + ci * 128, kind=k, nci=nci, p0=ci * 128))
        seqinfo.append(dict(first=first, n=nci, kind=k, pb=pb, len=l, t0=t0, pri=(pri if k == 1 else -1)))
        if k == 1:
            pri += 1
        t0 += l
        pb += l + 2
    NCH = len(chunks)

    nc = bass.Bass("TRN2", target_bir_lowering=False)

    def din(name, shape, dt=F32):
        return nc.dram_tensor(name, list(shape), dt, kind="ExternalInput").ap()

    def dscr(name, shape):
        return nc.dram_tensor(name, list(shape), F32, kind=("ExternalOutput" if DBG else "Internal")).ap()

    x_in = din("x_in", [NT, D])
    c2 = din("c2", [2, D])
    h0 = din("h0", [2, 2, H * HP, NS])
    posT = din("posT", [D, LS])
    consts = din("consts", [128, 6, 128])
    w_ada = din("w_ada", [2, D, ADA], F32R)
    b_ada = din("b_ada", [2, ADA])
    w_in = din("w_in", [2, D, IN_DIM], F32R)
    norm_mix_w = din("norm_mix_w", [2, D])
    norm_ffn_w = din("norm_ffn_w", [2, D])
    conv_a_w = din("conv_a_w", [2, 3, 1024])
    w_out_a = din("w_out_a", [2, 1024, D], F32R)
    ssd_conv_w = din("ssd_conv_w", [2, 3, 3072])
    ssd_conv_b = din("ssd_conv_b", [2, 3072])
    ssd_a_log = din("ssd_a_log", [2, 2, H])
    ssd_dt_bias = din("ssd_dt_bias", [2, 2 * H])
    ssd_d = din("ssd_d", [2, H])
    ssd_norm_w = din("ssd_norm_w", [2, D])
    w_out_b = din("w_out_b", [2, D, D], F32R)
    sgu_ln_g = din("sgu_ln_g", [2, 1024])
    sgu_ln_b = din("sgu_ln_b", [2, 1024])
    sgu_w = din("sgu_w", [2, 8, 128, 128])
    sgu_b = din("sgu_b", [2, 128, 8])
    w_out_c = din("w_out_c", [2, 1024, D], F32R)
    w_o = din("w_o", [2, D, D], F32R)
    router_w = din("router_w", [2, D, 20])
    router_b = din("router_b", [2, 20])
    exp_w_gate = din("exp_w_gate", [2, NE, D, DE], F32R)
    exp_w_up = din("exp_w_up", [2, NE, D, DE], F32R)
    exp_w_down = din("exp_w_down", [2, NE, DE, D], F32R)
    final_norm_w = din("final_norm_w", [1, D])

    y_out = nc.dram_tensor("y_out", [NT, D], F32, kind="ExternalOutput").ap()
    st_out = nc.dram_tensor("st_out", [max(NPR, 1), 2, 2, H, HP, NS], F32, kind="ExternalOutput").ap()

    PROJ_A = dscr("PROJ", [NPROW, 8192])
    PROJ_B = dscr("PROJB", [NPROW, IN_DIM - 8192])

    class _Proj:
        def __getitem__(self, key):
            rs, cs = key
            if cs.start >= 8192:
                return PROJ_B[rs, cs.start - 8192:cs.stop - 8192]
            assert cs.stop <= 8192
            return PROJ_A[rs, cs]
    PROJ = _Proj()
    XBC = dscr("XBC", [NT, 3072])
    DT = dscr("DT", [NT, 64])
    U = dscr("U", [NT, 4096])
    YP = dscr("YP", [NT, D])
    XR = dscr("XR", [NT, D])
    MOD = dscr("MOD", [2, ADA])
    XM = dscr("XM", [NT, D])
    COMB = dscr("COMB", [NT, 16])

    es = ExitStack()
    with es:
        arena_t = es.enter_context(nc.sbuf_tensor("arena", [128, ARENA], F32))
        arena_r = es.enter_context(nc.sbuf_tensor("arena_r", [128, ARENA], F32R))
        psum = [es.enter_context(nc.psum_tensor("pb%d" % i, [128, 512], F32)) for i in range(8)]
        block = es.enter_context(nc.Block())
        P = Prog(nc, es, arena_t, arena_r, psum)

        cst = P.alloc(6, 128)
        P.load(cst, cst.f, consts)
        ident, Umat, UTmat, ones, maskF, maskB = [cst.f[:, i, :] for i in range(6)]
        modT = P.alloc(96, 2)
        AB = P.alloc(4, KD, 2)
        nwT = P.alloc(2, KD)
        mk = P.mark()
        zrow = P.alloc(2048)
        P.op('pool', lambda e: e.memset(zrow.f, 0.0), [], [zrow])
        for si in seqinfo:
            for rr in (si['pb'], si['pb'] + si['len'] + 1):
                P.store(zrow, PROJ_A[rr:rr + 1, :].rearrange("o (a b) -> (o a) b", a=4), zrow.f[0:4, :])
        P.reset(mk)

        def wtile_load(t, dst, src):
            P.load(t, dst, src, eng='pool')

        def make_hT(xt, which, ch, hT, col0, tmp, posB=None, hT32=None):
            r = 1 if ch['kind'] == 0 else 0
            Aap = AB.f[:, 2 * which, :, r]
            Bap = AB.f[:, 2 * which + 1, :, r]
            ss = tmp['ss']
            junk = tmp['junk']
            P.op('act', lambda e: e.activation(out=junk.f, in_=xt.f, func=AF.Square, accum_out=ss.f[:, 0:1]),
                 [xt], [junk, ss])
            P.ts('dve', ss.f[:, 1:2], ss.f[:, 0:1], 1.0 / D, EPS, ALU.mult, ALU.add, [ss], [ss])
            P.act(ss.f[:, 2:3], ss.f[:, 1:2], AF.Sqrt, [ss], [ss])
            P.op('dve', lambda e: e.reciprocal(out=ss.f[:, 3:4], in_=ss.f[:, 2:3]), [ss], [ss])
            P.op('act', lambda e: e.activation(out=junk.f, in_=xt.f, func=AF.Copy, scale=ss.f[:, 3:4]), [xt, ss], [junk])
            for q4 in range(4):
                pt = P.ps()
                for q in range(4):
                    k = q4 * 4 + q
                    P.tr(pt, pt.f[:, q * 128:(q + 1) * 128], junk.f[:, k * 128:(k + 1) * 128], ident, [junk, cst])
                for q in range(4):
                    k = q4 * 4 + q
                    in1 = posB.f[:, k, :] if posB is not None else bc(Bap[:, k:k + 1], [128, 128])
                    rds = [pt, AB] + ([posB] if posB is not None else [])
                    P.stt(hT.r[:, k, col0:col0 + 128], pt.f[:, q * 128:(q + 1) * 128], Aap[:, k:k + 1], in1,
                          ALU.mult, ALU.add, rds, [hT])
                    if hT32 is not None:
                        P.stt(hT32.f[:, k, :], pt.f[:, q * 128:(q + 1) * 128], Aap[:, k:k + 1], in1,
                              ALU.mult, ALU.add, rds, [hT32])

        for l in range(DEPTH):
            xsrc = x_in if l == 0 else XR
            mk = P.mark()
            sc = P.alloc(D)
            scT = P.allocr(KD, 2)
            modrow = P.alloc(ADA)
            badats = [P.alloc(512) for _ in range(2)]
            wr = [P.allocr(KD, 512) for _ in range(2)]
            P.load(sc, sc.f[0:2, :], c2)
            P.act(sc.f[0:2, :], sc.f[0:2, :], AF.Silu, [sc], [sc])
            pt = P.ps()
            for k in range(KD):
                P.tr(pt, pt.f[:, 2 * k:2 * k + 2], sc.f[0:2, k * 128:(k + 1) * 128], ident[0:2, 0:2], [sc, cst])
            P.copy('dve', scT.r, pt.f[:, 0:32].rearrange("p (k r) -> p k r", r=2), [pt], [scT])
            for nt in range(ADA // 512):
                w = wr[nt % 2]
                wtile_load(w, w.r, w_ada[l, :, nt * 512:(nt + 1) * 512].rearrange("(k p) n -> p k n", p=128))
                badat = badats[nt % 2]
                P.dma('sp', badat, [(badat.f[0:1, :], b_ada[l:l + 1, nt * 512:(nt + 1) * 512]),
                                    (badat.f[1:2, :], b_ada[l:l + 1, nt * 512:(nt + 1) * 512])], True)
                pt = P.ps()
                P.mm(pt, pt.f[0:2, :], [(scT.r[:, k, :], w.r[:, k, :]) for k in range(KD)], [scT, w])
                P.tt('dve', modrow.f[0:2, nt * 512:(nt + 1) * 512], pt.f[0:2, :], badat.f[0:2, :],
                     ALU.add, [pt, badat], [modrow])
            P.store(modrow, MOD, modrow.f[0:2, :])
            pt = P.ps()
            for c in range(96):
                P.tr(pt, pt.f[:, 2 * c:2 * c + 2], modrow.f[0:2, c * 128:(c + 1) * 128], ident[0:2, 0:2], [modrow, cst])
            P.copy('dve', modT.f, pt.f[:, 0:192].rearrange("p (c r) -> p c r", r=2), [pt], [modT])
            P.load(nwT, nwT.f[:, 0, :], norm_mix_w[l].rearrange("(k p) -> p k", p=128), slow=True)
            P.load(nwT, nwT.f[:, 1, :], norm_ffn_w[l].rearrange("(k p) -> p k", p=128), slow=True)
            for which, (isc, ish) in enumerate(((1, 0), (4, 3))):
                for r in range(2):
                    P.stt(AB.f[:, 2 * which, :, r], modT.f[:, isc * 16:(isc + 1) * 16, r], 1.0, nwT.f[:, which, :],
                          ALU.add, ALU.mult, [modT, nwT], [AB])
                    P.copy('dve', AB.f[:, 2 * which + 1, :, r], modT.f[:, ish * 16:(ish + 1) * 16, r], [modT], [AB])
            P.reset(mk)

            mk = P.mark()
            TS = 4
            hT = P.allocr(KD, TS * 128)
            xts = [P.alloc(D) for _ in range(2)]
            tmp = dict(ss=P.alloc(8), junk=P.alloc(D))
            posb = P.alloc(KD, 128)
            wts = [P.allocr(KD, 512) for _ in range(2)]
            outs = [P.alloc(512) for _ in range(4)]
            oi = 0
            wi = 0
            for g0 in range(0, NCH, TS):
                grp = chunks[g0:g0 + TS]
                for j, ch in enumerate(grp):
                    xt = xts[j % 2]
                    P.load(xt, xt.f, xsrc[ch['t0']:ch['t0'] + 128, :])
                    pB = None
                    if ch['kind'] == 0:
                        P.load(posb, posb.f, posT[:, ch['p0']:ch['p0'] + 128].rearrange("(k p) t -> p k t", p=128))
                        P.tt('pool', posb.f, posb.f, bc(AB.f[:, 1, :, 1:2], [128, KD, 128]), ALU.add, [posb, AB], [posb])
                        pB = posb
                    make_hT(xt, 0, ch, hT, j * 128, tmp, posB=pB)
                ntile = (IN_DIM + 511) // 512
                for nt in range(ntile):
                    ncol = min(512, IN_DIM - nt * 512)
                    w = wts[wi % 2]
                    wi += 1
                    wtile_load(w, w.r[:, :, 0:ncol], w_in[l, :, nt * 512:nt * 512 + ncol].rearrange("(k p) n -> p k n", p=128))
                    for j, ch in enumerate(grp):
                        pt = P.ps()
                        P.mm(pt, pt.f[:, 0:ncol], [(hT.r[:, k, j * 128:(j + 1) * 128], w.r[:, k, 0:ncol]) for k in range(KD)],
                             [hT, w])
                        o = outs[oi % 4]
                        oi += 1
                        P.copy('act' if oi % 2 else 'dve', o.f[:, 0:ncol], pt.f[:, 0:ncol], [pt], [o])
                        P.store(o, PROJ[ch['pr']:ch['pr'] + 128, nt * 512:nt * 512 + ncol], o.f[:, 0:ncol])
            P.reset(mk)
            if cfg.get('stop') == 'proj':
                break

            mk = P.mark()
            cw = P.alloc(3, 3072)
            cb = P.alloc(3072)
            dtb = P.alloc(64)
            dtts = [P.alloc(64) for _ in range(2)]
            tins = [[P.alloc(1536) for _ in range(3)] for _ in range(2)]
            P.load(cw, cw.f, ssd_conv_w[l].partition_broadcast(128))
            P.load(cb, cb.f, ssd_conv_b[l].partition_broadcast(128))
            P.load(dtb, dtb.f, ssd_dt_bias[l].partition_broadcast(128))
            ri = 0
            for cidx, ch in enumerate(chunks):
                pr, t0 = ch['pr'], ch['t0']
                for hf in range(2):
                    tin = tins[ri % 2]
                    ri += 1
                    cs = slice(hf * 1536, (hf + 1) * 1536)
                    for s3 in range(3):
                        P.load(tin[s3], tin[s3].f, PROJ[pr - 1 + s3:pr - 1 + s3 + 128, C_XBC + hf * 1536:C_XBC + (hf + 1) * 1536])
                    P.tt('pool', tin[0].f, tin[0].f, cw.f[:, 0, cs], ALU.mult, [tin[0], cw], [tin[0]])
                    P.tt('dve', tin[1].f, tin[1].f, cw.f[:, 1, cs], ALU.mult, [tin[1], cw], [tin[1]])
                    P.tt('pool', tin[2].f, tin[2].f, cw.f[:, 2, cs], ALU.mult, [tin[2], cw], [tin[2]])
                    P.tt('dve', tin[1].f, tin[1].f, cb.f[:, cs], ALU.add, [tin[1], cb], [tin[1]])
                    P.tt('dve', tin[0].f, tin[0].f, tin[1].f, ALU.add, [tin[0], tin[1]], [tin[0]])
                    P.tt('dve', tin[0].f, tin[0].f, tin[2].f, ALU.add, [tin[0], tin[2]], [tin[0]])
                    P.act(tin[1].f, tin[0].f, AF.Silu, [tin[0]], [tin[1]])
                    P.store(tin[1], XBC[t0:t0 + 128, cs], tin[1].f)
                dtt = dtts[cidx % 2]
                P.load(dtt, dtt.f, PROJ[pr:pr + 128, C_DT:C_DT + 64])
                P.tt('dve', dtt.f, dtt.f, dtb.f, ALU.add, [dtt, dtb], [dtt])
                P.act(dtt.f, dtt.f, AF.Exp, [dtt], [dtt])
                P.ts('dve', dtt.f, dtt.f, 1.0, None, ALU.add, None, [dtt], [dtt])
                P.act(dtt.f, dtt.f, AF.Ln, [dtt], [dtt])
                P.store(dtt, DT[t0:t0 + 128, :], dtt.f)
            P.reset(mk)

            mk = P.mark()
            caw = P.alloc(3, 1024)
            tabs = [P.alloc(1024) for _ in range(2)]
            tchs = [[P.alloc(2048) for _ in range(3)] for _ in range(2)]
            P.load(caw, caw.f, conv_a_w[l].partition_broadcast(128))
            for cidx, ch in enumerate(chunks):
                pr, t0 = ch['pr'], ch['t0']
                tab = tabs[cidx % 2]
                tch = tchs[cidx % 2]
                P.load(tab, tab.f, PROJ[pr:pr + 128, C_AB:C_AB + 1024])
                for s3 in range(3):
                    t = tch[s3]
                    P.load(t, t.f, PROJ[pr - 1 + s3:pr - 1 + s3 + 128, C_AC:C_AC + 2048])
                    e1 = 'pool' if s3 != 1 else 'dve'
                    P.tt(e1, t.f[:, 0:1024], t.f[:, 0:1024], t.f[:, 1024:2048], ALU.mult, [t], [t])
                    P.tt(e1, t.f[:, 0:1024], t.f[:, 0:1024], caw.f[:, s3, :], ALU.mult, [t, caw], [t])
                a0 = tch[0]
                P.tt('dve', a0.f[:, 0:1024], a0.f[:, 0:1024], tch[1].f[:, 0:1024], ALU.add, [a0, tch[1]], [a0])
                P.tt('dve', a0.f[:, 0:1024], a0.f[:, 0:1024], tch[2].f[:, 0:1024], ALU.add, [a0, tch[2]], [a0])
                P.tt('dve', a0.f[:, 0:1024], a0.f[:, 0:1024], tab.f, ALU.mult, [a0, tab], [a0])
                P.store(a0, U[t0:t0 + 128, 0:1024], a0.f[:, 0:1024])
            P.reset(mk)

            mk = P.mark()
            lng = P.alloc(1024)
            lnb = P.alloc(1024)
            wT = P.alloc(8, 128)
            wtmp = P.alloc(8, 128)
            sgb = P.alloc(8)
            c3sets = [(P.alloc(2048), P.alloc(2048), P.alloc(1024), P.alloc(8), P.alloc(1024)) for _ in range(2)]
            P.load(lng, lng.f, sgu_ln_g[l].partition_broadcast(128))
            P.load(lnb, lnb.f, sgu_ln_b[l].partition_broadcast(128))
            P.load(sgb, sgb.f, sgu_b[l])
            P.load(wtmp, wtmp.f, sgu_w[l].rearrange("g i j -> i g j"))
            for hf in range(2):
                pt = P.ps()
                for q in range(4):
                    P.tr(pt, pt.f[:, q * 128:(q + 1) * 128], wtmp.f[:, hf * 4 + q, :], ident, [wtmp, cst])
                P.copy('dve', wT.f[:, hf * 4:(hf + 1) * 4, :], pt.f.rearrange("p (a b) -> p a b", a=4), [pt], [wT])
            for cidx, ch in enumerate(chunks):
                pr, t0 = ch['pr'], ch['t0']
                suv, gt_, junkc3, stc3, uc = c3sets[cidx % 2]
                P.load(suv, suv.f, PROJ[pr:pr + 128, C_SU:C_SU + 2048])
                P.tt('pool', gt_.f, suv.f, suv.f, ALU.mult, [suv], [gt_])
                P.ts('dve', gt_.f, gt_.f, 0.044715, 1.0, ALU.mult, ALU.add, [gt_], [gt_])
                P.tt('dve', gt_.f, gt_.f, suv.f, ALU.mult, [gt_, suv], [gt_])
                P.act(gt_.f, gt_.f, AF.Sigmoid, [gt_], [gt_], scale=1.5957691216057308)
                P.tt('dve', suv.f, suv.f, gt_.f, ALU.mult, [suv, gt_], [suv])
                u_ = suv.f[:, 0:1024]
                v_ = suv.f[:, 1024:2048]
                P.op('dve', lambda e, v_=v_, stc3=stc3: e.tensor_reduce(out=stc3.f[:, 0:1], in_=v_, axis=mybir.AxisListType.X, op=ALU.add),
                     [suv], [stc3])
                P.ts('dve', stc3.f[:, 1:2], stc3.f[:, 0:1], -1.0 / 1024, None, ALU.mult, None, [stc3], [stc3])
                P.ts('dve', v_, v_, stc3.f[:, 1:2], None, ALU.add, None, [suv, stc3], [suv])
                P.op('act', lambda e, v_=v_, stc3=stc3, junkc3=junkc3: e.activation(out=junkc3.f, in_=v_, func=AF.Square, accum_out=stc3.f[:, 2:3]),
                     [suv], [junkc3, stc3])
                P.ts('dve', stc3.f[:, 3:4], stc3.f[:, 2:3], 1.0 / 1024, EPS, ALU.mult, ALU.add, [stc3], [stc3])
                P.act(stc3.f[:, 4:5], stc3.f[:, 3:4], AF.Sqrt, [stc3], [stc3])
                P.op('dve', lambda e, stc3=stc3: e.reciprocal(out=stc3.f[:, 5:6], in_=stc3.f[:, 4:5]), [stc3], [stc3])
                P.stt(v_, v_, stc3.f[:, 5:6], lng.f, ALU.mult, ALU.mult, [suv, stc3, lng], [suv])
                P.tt('dve', v_, v_, lnb.f, ALU.add, [suv, lnb], [suv])
                for hf in range(2):
                    pt = P.ps()
                    for q in range(4):
                        g = hf * 4 + q
                        P.mm(pt, pt.f[:, q * 128:(q + 1) * 128], [(wT.f[:, g, :], suv.f[:, 1024 + g * 128:1024 + (g + 1) * 128])],
                             [wT, suv])
                    for q in range(4):
                        g = hf * 4 + q
                        P.stt(uc.f[:, g * 128:(g + 1) * 128], pt.f[:, q * 128:(q + 1) * 128], sgb.f[:, g:g + 1],
                              suv.f[:, g * 128:(g + 1) * 128], ALU.add, ALU.mult, [pt, sgb, suv], [uc])
                P.store(uc, U[t0:t0 + 128, 3072:4096], uc.f)
            P.reset(mk)
            if cfg.get('stop') == 'c3':
                break

            li = 0
            for dr in range(2):
                mk = P.mark()
                NB = 2 if dr == 0 else 1
                S = P.alloc(2048)
                xw = P.alloc(2048)
                yt = P.alloc(2048)
                if dr == 1:
                    zt = P.alloc(2048)
                    ypt = P.alloc(2048)
                    nwbc = P.alloc(2048)
                    P.load(nwbc, nwbc.f, ssd_norm_w[l].partition_broadcast(128))
                Rs = [P.alloc(4, 128) for _ in range(2)]
                Qs = [P.alloc(4, 128) for _ in range(2)]
                Lts = [P.alloc(4, 128) for _ in range(2)]
                WTs = [P.alloc(4, 128) for _ in range(2)]
                csets = [dict(xbc=P.alloc(3072), BT=P.alloc(4, 128), CT=P.alloc(4, 128), ST=P.alloc(4, 128), dts=P.alloc(32),
                              da=P.alloc(32), PT=P.alloc(64), lnw=P.alloc(32), biasL=P.alloc(32), eP=P.alloc(32),
                              etot=P.alloc(32), wdec=P.alloc(32)) for _ in range(NB)]
                abc = P.alloc(2, 32)
                Dbc = P.alloc(32)
                st4 = P.alloc(16)
                P.load(abc, abc.f, ssd_a_log[l].partition_broadcast(128))
                P.act(abc.f, abc.f, AF.Exp, [abc], [abc])
                P.ts('dve', abc.f, abc.f, -1.0, None, ALU.mult, None, [abc], [abc])
                P.load(Dbc, Dbc.f, ssd_d[l].partition_broadcast(128))
                cnum = 0
                cum = Umat if dr == 0 else UTmat
                msk = maskF if dr == 0 else maskB
                for si in seqinfo:
                    if si['kind'] == 0:
                        P.load(xw, xw.f.rearrange("p (k n) -> p k n", k=16), h0[l, dr].rearrange("(k p) n -> p k n", p=128))
                        for q4 in range(4):
                            pt = P.ps()
                            for q in range(4):
                                k = q4 * 4 + q
                                P.tr(pt, pt.f[:, q * 128:(q + 1) * 128], xw.f[:, k * 128:(k + 1) * 128], ident, [xw, cst])
                            P.copy('dve', S.f[:, q4 * 512:(q4 + 1) * 512], pt.f, [pt], [S])
                    else:
                        P.op('pool', lambda e, S=S: e.memset(S.f, 0.0), [], [S])
                    order = list(range(si['n'])) if dr == 0 else list(range(si['n'] - 1, -1, -1))
                    cbase = cnum
                    cnum += len(order)

                    def ssd_front(oi_):
                        ci = order[oi_]
                        ch = chunks[si['first'] + ci]
                        pr, t0 = ch['pr'], ch['t0']
                        cs_ = csets[(cbase + oi_) % NB]
                        xbc, BT, CT, ST, dts, da, PT = cs_['xbc'], cs_['BT'], cs_['CT'], cs_['ST'], cs_['dts'], cs_['da'], cs_['PT']
                        lnw, biasL, eP, etot, wdec = cs_['lnw'], cs_['biasL'], cs_['eP'], cs_['etot'], cs_['wdec']
                        P.load(xbc, xbc.f, XBC[t0:t0 + 128, :])
                        P.load(dts, dts.f, DT[t0:t0 + 128, dr * 32:(dr + 1) * 32])
                        if dr == 1:
                            P.load(zt, zt.f, PROJ[pr:pr + 128, C_Z:C_Z + 2048])
                            P.load(ypt, ypt.f, YP[t0:t0 + 128, :])
                        P.tt('dve', da.f, dts.f, abc.f[:, dr, :], ALU.mult, [dts, abc], [da])
                        pt0 = P.ps()
                        P.mm(pt0, pt0.f[:, 0:32], [(cum, da.f)], [cst, da])
                        P.mm(pt0, pt0.f[:, 32:64], [(ones, da.f)], [cst, da])
                        P.copy('dve', PT.f, pt0.f[:, 0:64], [pt0], [PT])
                        P.act(lnw.f, dts.f, AF.Ln, [dts], [lnw])
                        P.tt('dve', biasL.f, lnw.f, PT.f[:, 0:32], ALU.subtract, [lnw, PT], [biasL])
                        P.act(eP.f, PT.f[:, 0:32], AF.Exp, [PT], [eP])
                        P.act(etot.f, PT.f[:, 32:64], AF.Exp, [PT], [etot])
                        P.tt('dve', wdec.f, biasL.f, PT.f[:, 32:64], ALU.add, [biasL, PT], [wdec])
                        P.act(wdec.f, wdec.f, AF.Exp, [wdec], [wdec])
                        ptb = P.ps()
                        ptc = P.ps()
                        for g in range(4):
                            P.tr(ptb, ptb.f[:, g * 128:(g + 1) * 128], xbc.f[:, 2048 + g * 128:2048 + (g + 1) * 128], ident, [xbc, cst])
                            P.tr(ptc, ptc.f[:, g * 128:(g + 1) * 128], xbc.f[:, 2560 + g * 128:2560 + (g + 1) * 128], ident, [xbc, cst])
                        P.copy('act', BT.f, ptb.f.rearrange("p (a b) -> p a b", a=4), [ptb], [BT])
                        P.copy('dve', CT.f, ptc.f.rearrange("p (a b) -> p a b", a=4), [ptc], [CT])
                        pts = P.ps()
                        for g in range(4):
                            P.mm(pts, pts.f[:, g * 128:(g + 1) * 128], [(BT.f[:, g, :], CT.f[:, g, :])], [BT, CT])
                        P.copy('act', ST.f, pts.f.rearrange("p (a b) -> p a b", a=4), [pts], [ST])

                    def ssd_rest(oi_):
                        nonlocal li
                        ci = order[oi_]
                        ch = chunks[si['first'] + ci]
                        pr, t0 = ch['pr'], ch['t0']
                        cs_ = csets[(cbase + oi_) % NB]
                        xbc, BT, CT, ST, dts, da, PT = cs_['xbc'], cs_['BT'], cs_['CT'], cs_['ST'], cs_['dts'], cs_['da'], cs_['PT']
                        lnw, biasL, eP, etot, wdec = cs_['lnw'], cs_['biasL'], cs_['eP'], cs_['etot'], cs_['wdec']
                        ptys = {}
                        ptos = {}
                        stageA = {}

                        def emitA(b):
                            nonlocal li
                            g, hh = b // 2, b % 2
                            if hh == 0:
                                ptys[g] = P.ps()
                                ptos[g] = P.ps()
                                P.mm(ptos[g], ptos[g].f, [(CT.f[:, g, :], S.f[:, g * 512:(g + 1) * 512])], [CT, S])
                            hb = g * 8 + hh * 4
                            R_, Q_, Lt, WT = Rs[li % 2], Qs[li % 2], Lts[li % 2], WTs[li % 2]
                            li += 1
                            P.tt('pool', R_.f, bc(cum.unsqueeze(1), [128, 4, 128]), bc(da.f[:, hb:hb + 4].unsqueeze(2), [128, 4, 128]),
                                 ALU.mult, [cst, da], [R_])
                            P.tt('pool', Q_.f, bc(msk.unsqueeze(1), [128, 4, 128]), bc(biasL.f[:, hb:hb + 4].unsqueeze(2), [128, 4, 128]),
                                 ALU.add, [cst, biasL], [Q_])
                            ptl = P.ps()
                            P.mm(ptl, ptl.f, [(ones, R_.f.rearrange("p a b -> p (a b)")), (ident, Q_.f.rearrange("p a b -> p (a b)"))],
                                 [cst, R_, Q_])
                            P.act(Lt.f, ptl.f.rearrange("p (a b) -> p a b", a=4), AF.Exp, [ptl], [Lt])
                            P.tt('dve', WT.f, Lt.f, bc(ST.f[:, g:g + 1, :], [128, 4, 128]), ALU.mult, [Lt, ST], [WT])
                            stageA[b] = WT

                        def emitB(b):
                            g, hh = b // 2, b % 2
                            hb = g * 8 + hh * 4
                            WT = stageA[b]
                            pty = ptys[g]
                            for h4 in range(4):
                                hd = hb + h4
                                c0 = (hh * 4 + h4) * 64
                                P.mm(pty, pty.f[:, c0:c0 + 64], [(WT.f[:, h4, :], xbc.f[:, hd * 64:(hd + 1) * 64])], [WT, xbc])
                            if hh == 1:
                                pto = ptos[g]
                                yg = yt.f[:, g * 512:(g + 1) * 512]
                                P.tt('dve', yg.rearrange("p (a b) -> p a b", a=8), pto.f.rearrange("p (a b) -> p a b", a=8),
                                     bc(eP.f[:, g * 8:(g + 1) * 8].unsqueeze(2), [128, 8, 64]), ALU.mult, [pto, eP], [yt])
                                P.tt('dve', yg, yg, pty.f, ALU.add, [yt, pty], [yt])
                        emitA(0)
                        for b in range(8):
                            if b + 1 < 8:
                                emitA(b + 1)
                            emitB(b)
                        P.tt('pool', xw.f.rearrange("p (a b) -> p a b", a=32), xbc.f[:, 0:2048].rearrange("p (a b) -> p a b", a=32),
                             bc(wdec.f.unsqueeze(2), [128, 32, 64]), ALU.mult, [xbc, wdec], [xw])
                        for g in range(4):
                            ptg = P.ps()
                            P.mm(ptg, ptg.f, [(xbc.f[:, 2048 + g * 128:2048 + (g + 1) * 128], xw.f[:, g * 512:(g + 1) * 512])], [xbc, xw])
                            Sg = S.f[:, g * 512:(g + 1) * 512]
                            P.tt('dve', Sg.rearrange("p (a b) -> p a b", a=8), Sg.rearrange("p (a b) -> p a b", a=8),
                                 bc(etot.f[:, g * 8:(g + 1) * 8].unsqueeze(2), [128, 8, 64]), ALU.mult, [S, etot], [S])
                            P.tt('dve', Sg, Sg, ptg.f, ALU.add, [S, ptg], [S])
                        if dr == 0:
                            P.store(yt, YP[t0:t0 + 128, :], yt.f)
                        else:
                            P.tt('dve', yt.f, yt.f, ypt.f, ALU.add, [yt, ypt], [yt])
                            P.tt('pool', ypt.f.rearrange("p (a b) -> p a b", a=32), xbc.f[:, 0:2048].rearrange("p (a b) -> p a b", a=32),
                                 bc(Dbc.f.unsqueeze(2), [128, 32, 64]), ALU.mult, [xbc, Dbc], [ypt])
                            P.tt('dve', yt.f, yt.f, ypt.f, ALU.add, [yt, ypt], [yt])
                            P.act(zt.f, zt.f, AF.Silu, [zt], [zt])
                            P.tt('dve', yt.f, yt.f, zt.f, ALU.mult, [yt, zt], [yt])
                            for g in range(4):
                                P.op('act', lambda e, g=g, zt=zt, yt=yt, st4=st4: e.activation(out=zt.f[:, g * 512:(g + 1) * 512], in_=yt.f[:, g * 512:(g + 1) * 512],
                                                                       func=AF.Square, accum_out=st4.f[:, g:g + 1]), [yt], [zt, st4])
                            P.ts('dve', st4.f[:, 4:8], st4.f[:, 0:4], 1.0 / 512, EPS, ALU.mult, ALU.add, [st4], [st4])
                            P.act(st4.f[:, 8:12], st4.f[:, 4:8], AF.Sqrt, [st4], [st4])
                            P.op('dve', lambda e, st4=st4: e.reciprocal(out=st4.f[:, 12:16], in_=st4.f[:, 8:12]), [st4], [st4])
                            for g in range(4):
                                P.stt(yt.f[:, g * 512:(g + 1) * 512], yt.f[:, g * 512:(g + 1) * 512], st4.f[:, 12 + g:13 + g],
                                      nwbc.f[:, g * 512:(g + 1) * 512], ALU.mult, ALU.mult, [yt, st4, nwbc], [yt])
                            P.store(yt, U[t0:t0 + 128, 1024:3072], yt.f)

                    if NB == 2:
                        ssd_front(0)
                        for oi_ in range(len(order)):
                            if oi_ + 1 < len(order):
                                ssd_front(oi_ + 1)
                            ssd_rest(oi_)
                    else:
                        for oi_ in range(len(order)):
                            ssd_front(oi_)
                            ssd_rest(oi_)
                    if si['kind'] == 1:
                        for half in range(2):
                            for q in range(4):
                                pt = P.ps()
                                for h4 in range(4):
                                    hd = half * 16 + q * 4 + h4
                                    P.tr(pt, pt.f[0:64, h4 * 128:(h4 + 1) * 128], S.f[:, hd * 64:(hd + 1) * 64], ident, [S, cst])
                                P.copy('dve', xw.f[0:64, q * 512:(q + 1) * 512], pt.f[0:64, :], [pt], [xw])
                            P.store(xw, st_out[si['pri'], l, dr, half * 16:(half + 1) * 16].rearrange("h p n -> p h n"),
                                    xw.f[0:64, :].rearrange("p (h n) -> p h n", h=16))
                P.reset(mk)
            if cfg.get('stop') == 'ssd':
                break

            mk = P.mark()
            TDN = 4
            UT = P.allocr(32, TDN * 128)
            mT = UT
            wds = [P.allocr(KD, 256) for _ in range(2)]
            utile = P.alloc(4096)
            merged = [P.alloc(2048) for _ in range(TDN)]
            xch = [P.alloc(2048) for _ in range(TDN)]
            gts = [P.alloc(256) for _ in range(2)]
            g1s = [P.alloc(256) for _ in range(2)]
            wi = 0
            gi_ = 0
            ci_ = 0
            for g0 in range(0, NCH, TDN):
                grp = chunks[g0:g0 + TDN]
                for j, ch in enumerate(grp):
                    P.load(utile, utile.f, U[ch['t0']:ch['t0'] + 128, :])
                    P.load(xch[j], xch[j].f, xsrc[ch['t0']:ch['t0'] + 128, :])
                    for q4 in range(8):
                        pt = P.ps()
                        for q in range(4):
                            k = q4 * 4 + q
                            P.tr(pt, pt.f[:, q * 128:(q + 1) * 128], utile.f[:, k * 128:(k + 1) * 128], ident, [utile, cst])
                        ci_ += 1
                        P.copy('act' if ci_ % 2 else 'dve', UT.r[:, q4 * 4:(q4 + 1) * 4, j * 128:(j + 1) * 128],
                               pt.f.rearrange("p (a b) -> p a b", a=4), [pt], [UT])
                for bi, (wsrc, k0, KC, gcol) in enumerate(((w_out_a, 0, 8, 0), (w_out_b, 8, 16, 2048), (w_out_c, 24, 8, 4096))):
                    for nt in range(8):
                        w = wds[wi % 2]
                        wi += 1
                        wtile_load(w, w.r[:, 0:KC, :], wsrc[l, :, nt * 256:(nt + 1) * 256].rearrange("(k p) n -> p k n", p=128))
                        for j, ch in enumerate(grp):
                            pt = P.ps()
                            P.mm(pt, pt.f[:, 0:256], [(UT.r[:, k0 + k, j * 128:(j + 1) * 128], w.r[:, k, :]) for k in range(KC)], [UT, w])
                            gt = gts[gi_ % 2]
                            gi_ += 1
                            c0 = C_GATE + gcol + nt * 256
                            P.load(gt, gt.f, PROJ[ch['pr']:ch['pr'] + 128, c0:c0 + 256])
                            P.act(gt.f, gt.f, AF.Sigmoid, [gt], [gt])
                            mg = merged[j].f[:, nt * 256:(nt + 1) * 256]
                            if bi == 0:
                                P.tt('dve', mg, gt.f, pt.f[:, 0:256], ALU.mult, [gt, pt], [merged[j]])
                            else:
                                P.tt('dve', gt.f, gt.f, pt.f[:, 0:256], ALU.mult, [gt, pt], [gt])
                                P.tt('dve', mg, mg, gt.f, ALU.add, [merged[j], gt], [merged[j]])
                for j, ch in enumerate(grp):
                    for q4 in range(4):
                        pt = P.ps()
                        for q in range(4):
                            k = q4 * 4 + q
                            P.tr(pt, pt.f[:, q * 128:(q + 1) * 128], merged[j].f[:, k * 128:(k + 1) * 128], ident, [merged[j], cst])
                        ci_ += 1
                        P.copy('act' if ci_ % 2 else 'dve', mT.r[:, q4 * 4:(q4 + 1) * 4, j * 128:(j + 1) * 128],
                               pt.f.rearrange("p (a b) -> p a b", a=4), [pt], [mT])
                for nt in range(8):
                    w = wds[wi % 2]
                    wi += 1
                    wtile_load(w, w.r, w_o[l, :, nt * 256:(nt + 1) * 256].rearrange("(k p) n -> p k n", p=128))
                    for j, ch in enumerate(grp):
                        r = 1 if ch['kind'] == 0 else 0
                        pt = P.ps()
                        P.mm(pt, pt.f[:, 0:256], [(mT.r[:, k, j * 128:(j + 1) * 128], w.r[:, k, :]) for k in range(KD)], [mT, w])
                        g1 = g1s[gi_ % 2]
                        gi_ += 1
                        P.load(g1, g1.f, MOD[r, 2 * D + nt * 256:2 * D + (nt + 1) * 256].partition_broadcast(128))
                        P.tt('dve', g1.f, g1.f, pt.f[:, 0:256], ALU.mult, [g1, pt], [g1])
                        xs_ = xch[j].f[:, nt * 256:(nt + 1) * 256]
                        P.tt('dve', xs_, xs_, g1.f, ALU.add, [xch[j], g1], [xch[j]])
                for j, ch in enumerate(grp):
                    P.store(xch[j], XM[ch['t0']:ch['t0'] + 128, :], xch[j].f)
            P.reset(mk)
            if cfg.get('stop') == 'mix':
                break

            mk = P.mark()
            TE = 4
            last = (l == DEPTH - 1)
            h2T = P.allocr(KD, TE * 128)
            actTs = [P.allocr(4, TE * 128) for _ in range(2)]
            wgu = [[P.allocr(KD, 128) for _ in range(2)] for _ in range(2)]
            wdn = [P.allocr(4, 512) for _ in range(2)]
            xe = [P.alloc(2048) for _ in range(TE)]
            acc = [P.alloc(2048) for _ in range(TE)]
            tmp = dict(ss=P.alloc(8), junk=P.alloc(D))
            h32 = P.alloc(KD, 128)
            sgs = [P.alloc(TE * 128) for _ in range(2)]
            rw = P.alloc(KD, 20)
            rbb = P.alloc(20)
            lg = P.alloc(20)
            rs = P.alloc(64)
            combs = [P.alloc(16) for _ in range(TE)]
            g2s = [P.alloc(512) for _ in range(2)]
            P.load(rw, rw.f, router_w[l].rearrange("(k p) n -> p k n", p=128))
            P.load(rbb, rbb.f, router_b[l].partition_broadcast(128))
            ui = 0
            di = 0
            si_ = 0
            g2i = 0
            for g0 in range(0, NCH, TE):
                grp = chunks[g0:g0 + TE]
                nj = len(grp)
                NTK = nj * 128
                for j, ch in enumerate(grp):
                    P.load(xe[j], xe[j].f, XM[ch['t0']:ch['t0'] + 128, :])
                    make_hT(xe[j], 1, ch, h2T, j * 128, tmp, posB=None, hT32=h32)
                    pt = P.ps()
                    P.mm(pt, pt.f[:, 0:20], [(h32.f[:, k, :], rw.f[:, k, :]) for k in range(KD)], [h32, rw])
                    P.tt('dve', lg.f, pt.f[:, 0:20], rbb.f, ALU.add, [pt, rbb], [lg])
                    R_ = rs.f
                    cb_ = combs[j]
                    P.op('dve', lambda e, R_=R_: e.tensor_reduce(out=R_[:, 0:1], in_=lg.f[:, 0:4], axis=mybir.AxisListType.X, op=ALU.max), [lg], [rs])
                    P.ts('dve', R_[:, 1:2], R_[:, 0:1], -1.0, None, ALU.mult, None, [rs], [rs])
                    P.op('act', lambda e, R_=R_: e.activation(out=R_[:, 4:8], in_=lg.f[:, 0:4], func=AF.Exp, bias=R_[:, 1:2], scale=1.0,
                                                            accum_out=R_[:, 2:3]), [lg, rs], [rs])
                    P.op('dve', lambda e, R_=R_: e.reciprocal(out=R_[:, 3:4], in_=R_[:, 2:3]), [rs], [rs])
                    P.ts('dve', R_[:, 8:12], lg.f[:, 0:4], R_[:, 0:1], None, ALU.is_equal, None, [lg, rs], [rs])
                    P.tt('dve', R_[:, 16:32].rearrange("p (g k) -> p g k", g=4), lg.f[:, 4:20].rearrange("p (g k) -> p g k", g=4),
                         bc(R_[:, 8:12].unsqueeze(2), [128, 4, 4]), ALU.mult, [lg, rs], [rs])
                    P.op('dve', lambda e, R_=R_: e.tensor_reduce(out=R_[:, 12:16], in_=R_[:, 16:32].rearrange("p (g k) -> p k g", g=4),
                                                               axis=mybir.AxisListType.X, op=ALU.add), [rs], [rs])
                    P.op('dve', lambda e, R_=R_: e.tensor_reduce(out=R_[:, 32:33], in_=R_[:, 12:16], axis=mybir.AxisListType.X, op=ALU.max), [rs], [rs])
                    P.ts('dve', R_[:, 33:34], R_[:, 32:33], -1.0, None, ALU.mult, None, [rs], [rs])
                    P.op('act', lambda e, R_=R_: e.activation(out=R_[:, 36:40], in_=R_[:, 12:16], func=AF.Exp, bias=R_[:, 33:34], scale=1.0), [rs], [rs])
                    P.ts('dve', R_[:, 40:44], R_[:, 12:16], R_[:, 32:33], None, ALU.is_equal, None, [rs], [rs])
                    P.stt(R_[:, 44:48], R_[:, 40:44], -2.0, R_[:, 36:40], ALU.mult, ALU.add, [rs], [rs])
                    P.op('dve', lambda e, R_=R_: e.tensor_reduce(out=R_[:, 34:35], in_=R_[:, 44:48], axis=mybir.AxisListType.X, op=ALU.max), [rs], [rs])
                    P.ts('dve', R_[:, 48:52], R_[:, 44:48], R_[:, 34:35], None, ALU.is_equal, None, [rs], [rs])
                    P.ts('dve', R_[:, 35:36], R_[:, 34:35], 1.0, None, ALU.add, None, [rs], [rs])
                    P.op('dve', lambda e, R_=R_: e.reciprocal(out=R_[:, 52:53], in_=R_[:, 35:36]), [rs], [rs])
                    P.tt('dve', R_[:, 53:54], R_[:, 52:53], R_[:, 3:4], ALU.mult, [rs], [rs])
                    P.tt('dve', R_[:, 54:55], R_[:, 53:54], R_[:, 34:35], ALU.mult, [rs], [rs])
                    P.ts('dve', R_[:, 56:60], R_[:, 40:44], R_[:, 53:54], None, ALU.mult, None, [rs], [rs])
                    P.stt(R_[:, 56:60], R_[:, 48:52], R_[:, 54:55], R_[:, 56:60], ALU.mult, ALU.add, [rs], [rs])
                    P.tt('dve', cb_.f.rearrange("p (g k) -> p g k", g=4), bc(R_[:, 8:12].unsqueeze(2), [128, 4, 4]),
                         bc(R_[:, 56:60].unsqueeze(1), [128, 4, 4]), ALU.mult, [rs], [cb_])
                    if DBG:
                        P.store(cb_, COMB[ch['t0']:ch['t0'] + 128, :], cb_.f)
                def emit_gu(ex):
                    nonlocal ui, si_
                    actT = actTs[ex % 2]
                    for fc in range(4):
                        wg, wu = wgu[ui % 2]
                        ui += 1
                        wtile_load(wg, wg.r, exp_w_gate[l, ex, :, fc * 128:(fc + 1) * 128].rearrange("(k p) f -> p k f", p=128))
                        wtile_load(wu, wu.r, exp_w_up[l, ex, :, fc * 128:(fc + 1) * 128].rearrange("(k p) f -> p k f", p=128))
                        ptg = P.ps()
                        P.mm(ptg, ptg.f[:, 0:NTK], [(wg.r[:, k, :], h2T.r[:, k, 0:NTK]) for k in range(KD)], [wg, h2T])
                        ptu = P.ps()
                        P.mm(ptu, ptu.f[:, 0:NTK], [(wu.r[:, k, :], h2T.r[:, k, 0:NTK]) for k in range(KD)], [wu, h2T])
                        sg = sgs[si_ % 2]
                        si_ += 1
                        P.act(sg.f[:, 0:NTK], ptg.f[:, 0:NTK], AF.Silu, [ptg], [sg])
                        P.tt('dve', actT.r[:, fc, 0:NTK], sg.f[:, 0:NTK], ptu.f[:, 0:NTK], ALU.mult, [sg, ptu], [actT])

                def emit_down(ex):
                    nonlocal di
                    actT = actTs[ex % 2]
                    for dq in range(4):
                        wd = wdn[di % 2]
                        di += 1
                        wtile_load(wd, wd.r, exp_w_down[l, ex, :, dq * 512:(dq + 1) * 512].rearrange("(c p) n -> p c n", p=128))
                        for j in range(nj):
                            pt = P.ps()
                            P.mm(pt, pt.f, [(actT.r[:, fc, j * 128:(j + 1) * 128], wd.r[:, fc, :]) for fc in range(4)], [actT, wd])
                            dst = acc[j].f[:, dq * 512:(dq + 1) * 512]
                            if ex == 0:
                                P.ts('dve', dst, pt.f, combs[j].f[:, ex:ex + 1], None, ALU.mult, None, [pt, combs[j]], [acc[j]])
                            else:
                                P.stt(dst, pt.f, combs[j].f[:, ex:ex + 1], dst, ALU.mult, ALU.add, [pt, combs[j], acc[j]], [acc[j]])
                emit_gu(0)
                for ex in range(NE):
                    if ex + 1 < NE:
                        emit_gu(ex + 1)
                    emit_down(ex)
                for j, ch in enumerate(grp):
                    r = 1 if ch['kind'] == 0 else 0
                    for d4 in range(4):
                        g2 = g2s[g2i % 2]
                        g2i += 1
                        P.load(g2, g2.f, MOD[r, 5 * D + d4 * 512:5 * D + (d4 + 1) * 512].partition_broadcast(128))
                        sl = slice(d4 * 512, (d4 + 1) * 512)
                        P.tt('dve', g2.f, g2.f, acc[j].f[:, sl], ALU.mult, [g2, acc[j]], [g2])
                        P.tt('dve', xe[j].f[:, sl], xe[j].f[:, sl], g2.f, ALU.add, [xe[j], g2], [xe[j]])
                    if not last:
                        P.store(xe[j], XR[ch['t0']:ch['t0'] + 128, :], xe[j].f)
                    else:
                        if DBG:
                            P.store(xe[j], XR[ch['t0']:ch['t0'] + 128, :], xe[j].f)
                        ss = tmp['ss']
                        junk = tmp['junk']
                        P.op('act', lambda e, j=j: e.activation(out=junk.f, in_=xe[j].f, func=AF.Square, accum_out=ss.f[:, 0:1]),
                             [xe[j]], [junk, ss])
                        P.ts('dve', ss.f[:, 1:2], ss.f[:, 0:1], 1.0 / D, EPS, ALU.mult, ALU.add, [ss], [ss])
                        P.act(ss.f[:, 2:3], ss.f[:, 1:2], AF.Sqrt, [ss], [ss])
                        P.op('dve', lambda e: e.reciprocal(out=ss.f[:, 3:4], in_=ss.f[:, 2:3]), [ss], [ss])
                        for d4 in range(4):
                            g2 = g2s[g2i % 2]
                            g2i += 1
                            sl = slice(d4 * 512, (d4 + 1) * 512)
                            P.load(g2, g2.f, final_norm_w[0, sl].partition_broadcast(128))
                            P.stt(junk.f[:, sl], xe[j].f[:, sl], ss.f[:, 3:4], g2.f, ALU.mult, ALU.mult, [xe[j], ss, g2], [junk])
                        P.store(junk, y_out[ch['t0']:ch['t0'] + 128, :], junk.f)
            P.reset(mk)

        P.barrier()
        P.emit(block)
    return nc


def make_consts():
    k = np.arange(128)[:, None]
    i = np.arange(128)[None, :]
    c = np.zeros((128, 6, 128), np.float32)
    c[:, 0] = (k == i)
    c[:, 1] = (k <= i)
    c[:, 2] = (k >= i)
    c[:, 3] = 1.0
    c[:, 4] = np.where(k > i, -BIG, 0.0)
    c[:, 5] = np.where(k < i, -BIG, 0.0)
    return c


def make_posT(n_tok):
    rows = n_tok // 64
    rr, cc = np.meshgrid(np.arange(rows, dtype=np.float32), np.arange(64, dtype=np.float32), indexing='ij')
    quarter = D // 4
    omega = (1.0 / (np.float32(10000.0) ** (np.arange(quarter, dtype=np.float32) / np.float32(quarter)))).astype(np.float32)
    ar = rr.reshape(-1)[:, None] * omega
    ac = cc.reshape(-1)[:, None] * omega
    pos = np.concatenate([np.sin(ar), np.cos(ar), np.sin(ac), np.cos(ac)], axis=-1).astype(np.float32)
    return np.ascontiguousarray(pos.T)


def make_in_map(inp, sample_idx, prompt_idxs, sample_len=None):
    f = lambda a: np.ascontiguousarray(np.asarray(a, dtype=np.float32))
    xs = f(inp['x_sample'])[sample_idx]
    if sample_len is not None:
        xs = xs[:sample_len]
    xp = [f(inp['x_prompt'])[i] for i in prompt_idxs]
    m = {}
    m['x_in'] = np.ascontiguousarray(np.concatenate([xs] + xp, axis=0))
    m['c2'] = np.ascontiguousarray(np.stack([f(inp['c_ctx']), f(inp['c'])[sample_idx]], axis=0))
    m['h0'] = np.ascontiguousarray(f(inp['state_ssd'])[sample_idx].reshape(2, 2, H * HP, NS))
    m['posT'] = make_posT(xs.shape[0])
    m['consts'] = make_consts()
    for k in ['w_ada', 'b_ada', 'w_in', 'norm_mix_w', 'norm_ffn_w', 'conv_a_w', 'w_out_a', 'ssd_conv_w', 'ssd_conv_b',
              'ssd_a_log', 'ssd_d', 'ssd_norm_w', 'w_out_b', 'sgu_ln_g', 'sgu_ln_b', 'sgu_w', 'sgu_b', 'w_out_c', 'w_o',
              'exp_w_gate', 'exp_w_up', 'exp_w_down']:
        m[k] = f(inp[k])
    m['ssd_dt_bias'] = f(inp['ssd_dt_bias']).reshape(2, 2 * H)
    m['router_w'] = np.ascontiguousarray(np.concatenate([f(inp['router_g_w']), f(inp['router_e_w'])], axis=-1))
    m['router_b'] = np.ascontiguousarray(np.concatenate([f(inp['router_g_b']), f(inp['router_e_b'])], axis=-1))
    m['final_norm_w'] = f(inp['final_norm_w']).reshape(1, D)
    return m


_NC_CACHE = {}


def kernel(**inputs):
    seqs = [(4096, 0), (256, 1), (256, 1)]
    key = 'full'
    if key not in _NC_CACHE:
        _NC_CACHE[key] = build(dict(seqs=seqs, depth=2, debug=False))
    nc = _NC_CACHE[key]
    base = make_in_map(inputs, 0, [0, 1])
    in_maps = []
    xs_all = np.asarray(inputs['x_sample'], dtype=np.float32)
    xp_all = np.asarray(inputs['x_prompt'], dtype=np.float32)
    c_all = np.asarray(inputs['c'], dtype=np.float32)
    cctx = np.asarray(inputs['c_ctx'], dtype=np.float32)
    st_all = np.asarray(inputs['state_ssd'], dtype=np.float32)
    for core in range(8):
        si = core % 2
        m = dict(base)
        m['x_in'] = np.ascontiguousarray(np.concatenate([xs_all[si], xp_all[2 * core], xp_all[2 * core + 1]], axis=0))
        m['c2'] = np.ascontiguousarray(np.stack([cctx, c_all[si]], axis=0))
        m['h0'] = np.ascontiguousarray(st_all[si].reshape(2, 2, H * HP, NS))
        in_maps.append(m)
    res = run_bass_kernel_spmd(nc, in_maps, core_ids=list(range(8)))
    y_prompt = np.zeros((16, 256, D), np.float32)
    y_sample = np.zeros((2, 4096, D), np.float32)
    new_state = np.zeros((16, 2, 2, H, HP, NS), np.float32)
    for core in range(8):
        r = res.results[core]
        yo = np.asarray(r['y_out'])
        if core < 2:
            y_sample[core] = yo[0:4096]
        y_prompt[2 * core] = yo[4096:4352]
        y_prompt[2 * core + 1] = yo[4352:4608]
        so = np.asarray(r['st_out'])
        new_state[2 * core] = so[0]
        new_state[2 * core + 1] = so[1]
    return (y_prompt, y_sample, new_state)
```
